# Optimizing a Trainium2 kernel written in Bass

```python
import math
import jax, jax.numpy as jnp
from jax import lax
import numpy as np

D_MODEL = 1024
BATCH = 2
SEQ = 8192
DEPTH = 2

CHUNK = 64
D_MIX = D_MODEL
A_HEADS = 6
A_HEAD_DIM = 64
A_WIDTH = A_HEADS * A_HEAD_DIM
A_W_LORA = 32
A_A_LORA = 32
A_G_LORA = 64
A_V_LORA = 16
RWKV_GN_EPS = 64e-5
B_HEADS = 6
B_NOPE = 64
B_ROPE = 32
B_QK = B_NOPE + B_ROPE
B_V = 64
B_WIDTH = B_HEADS * B_V
B_Q_LORA = 256
B_KV_LORA = 128
ROPE_THETA = 10000.0
Q_BLOCK = 128
C_GROUPS = 4
C_GROUP_DIM = 64
C_WIDTH = C_GROUPS * C_GROUP_DIM
C_BLOCK = 128
A_SPLITS = (A_WIDTH, 2 * A_WIDTH, 3 * A_WIDTH, 3 * A_WIDTH + A_W_LORA, 3 * A_WIDTH + A_W_LORA + A_A_LORA)
A_COLS = 3 * A_WIDTH + A_W_LORA + A_A_LORA + A_G_LORA
B_COLS = B_Q_LORA + B_KV_LORA + B_ROPE
C_COLS = 2 * C_WIDTH
IN_COLS = A_COLS + B_COLS + C_COLS
D_FF_DENSE = 2816
N_EXPERTS = 8
TOP_K = 2
D_FF_EXPERT = 3584
N_DENSE = (DEPTH + 1) // 2
N_MOE = DEPTH // 2
N_VRES = DEPTH - 1
EPS = 1e-6

kernel_name = 'hybrid_rwkv7_mla_gmlp_moe_trunk'


def rms_norm(x, g, eps=EPS):
    xf = x.astype(jnp.float32)
    y = xf * lax.rsqrt(jnp.mean(xf * xf, axis=-1, keepdims=True) + eps)
    return (y * g.astype(jnp.float32)).astype(x.dtype)


def layer_norm(x, g, b, eps=EPS):
    xf = x.astype(jnp.float32)
    mu = jnp.mean(xf, axis=-1, keepdims=True)
    var = jnp.mean(jnp.square(xf - mu), axis=-1, keepdims=True)
    y = (xf - mu) * lax.rsqrt(var + eps) * g.astype(jnp.float32) + b.astype(jnp.float32)
    return y.astype(x.dtype)


def token_shift(y, mu):
    prev = jnp.pad(y, ((0, 0), (1, 0), (0, 0)))[:, :-1]
    return y + (prev - y) * mu


def rwkv7_time_mix(za, shift_mu, w0, w_up, a0, a_up, g_up, k_k, k_a, r_k, ln_g, ln_b,
                   v_first, v0, v_down, v_up):
    B_, S_, _ = za.shape
    za = token_shift(za, shift_mu)
    r, k, v, wd, ad, gd = jnp.split(za, A_SPLITS, axis=-1)
    w_log = -jax.nn.softplus(-(w0 + jnp.tanh(wd) @ w_up)) - 0.5
    decay = jnp.exp(-jnp.exp(w_log.astype(jnp.float32)))
    a = jax.nn.sigmoid(a0 + ad @ a_up)
    g = jax.nn.sigmoid(gd) @ g_up
    if v_first is not None:
        v = v + (v_first - v) * jax.nn.sigmoid(v0 + (v @ v_down) @ v_up)
    heads = lambda t: t.reshape(B_, S_, A_HEADS, A_HEAD_DIM).astype(jnp.float32)
    kk = heads(k * k_k)
    kk = kk / jnp.maximum(jnp.sqrt(jnp.sum(kk * kk, axis=-1, keepdims=True)), 1e-12)
    k = k * (1 + (a - 1) * k_a)
    rh, kh, vh, ah, wh = heads(r), heads(k), heads(v), heads(a), heads(decay)

    def step(state, inp):
        r_t, w_t, k_t, v_t, kk_t, a_t = inp
        sa = jnp.einsum('bhvk,bhk->bhv', state, -kk_t)
        state = (state * w_t[:, :, None, :]
                 + sa[..., None] * (kk_t * a_t)[:, :, None, :]
                 + v_t[..., None] * k_t[:, :, None, :])
        return state, jnp.einsum('bhvk,bhk->bhv', state, r_t)

    xs = tuple(jnp.swapaxes(t, 0, 1) for t in (rh, wh, kh, vh, kk, ah))
    state0 = jnp.zeros((B_, A_HEADS, A_HEAD_DIM, A_HEAD_DIM), jnp.float32)
    _, y = lax.scan(step, state0, xs)
    y = jnp.swapaxes(y, 0, 1)
    mu = jnp.mean(y, axis=-1, keepdims=True)
    var = jnp.mean(jnp.square(y - mu), axis=-1, keepdims=True)
    y = (y - mu) * lax.rsqrt(var + RWKV_GN_EPS)
    y = (y * ln_g.reshape(A_HEADS, A_HEAD_DIM).astype(jnp.float32)
         + ln_b.reshape(A_HEADS, A_HEAD_DIM).astype(jnp.float32))
    y = y + jnp.sum(rh * kh * r_k.astype(jnp.float32), axis=-1, keepdims=True) * vh
    y = y.reshape(B_, S_, A_WIDTH).astype(za.dtype) * g
    return y, v


def apply_rope(t, cos, sin):
    half = B_ROPE // 2
    t1, t2 = t[..., :half], t[..., half:]
    return jnp.concatenate([t1 * cos - t2 * sin, t1 * sin + t2 * cos], axis=-1)


def mla_attention(zb, positions, q_norm_g, w_uq, kv_norm_g, w_ukv, q_head_g, k_head_g, out_g):
    B_, S_, _ = zb.shape
    cq, ckv, kr = jnp.split(zb, (B_Q_LORA, B_Q_LORA + B_KV_LORA), axis=-1)
    q = (rms_norm(cq, q_norm_g) @ w_uq).reshape(B_, S_, B_HEADS, B_QK)
    kv = (rms_norm(ckv, kv_norm_g) @ w_ukv).reshape(B_, S_, B_HEADS, B_NOPE + B_V)
    k_nope, v = kv[..., :B_NOPE], kv[..., B_NOPE:]
    k = jnp.concatenate([k_nope, jnp.broadcast_to(kr[:, :, None, :], (B_, S_, B_HEADS, B_ROPE))], axis=-1)
    q = rms_norm(q, q_head_g)
    k = rms_norm(k, k_head_g)
    inv_freq = ROPE_THETA ** (-jnp.arange(0, B_ROPE, 2, dtype=jnp.float32) / B_ROPE)
    ang = positions.astype(jnp.float32)[..., None] * inv_freq
    cos = jnp.cos(ang)[:, :, None, :].astype(q.dtype)
    sin = jnp.sin(ang)[:, :, None, :].astype(q.dtype)
    q = jnp.concatenate([q[..., :B_NOPE], apply_rope(q[..., B_NOPE:], cos, sin)], axis=-1)
    k = jnp.concatenate([k[..., :B_NOPE], apply_rope(k[..., B_NOPE:], cos, sin)], axis=-1)
    q = jnp.transpose(q, (0, 2, 1, 3))
    k = jnp.transpose(k, (0, 2, 1, 3))
    v = jnp.transpose(v, (0, 2, 1, 3))
    n_blocks = S_ // Q_BLOCK
    qb = jnp.transpose(q.reshape(B_, B_HEADS, n_blocks, Q_BLOCK, B_QK), (2, 0, 1, 3, 4))
    key_chunk = jnp.arange(S_) // CHUNK
    scale = 1.0 / math.sqrt(B_QK)

    def block(args):
        q_blk, i = args
        s = jnp.einsum('bhqd,bhkd->bhqk', q_blk, k).astype(jnp.float32) * scale
        q_chunk = (i * Q_BLOCK + jnp.arange(Q_BLOCK)) // CHUNK
        s = jnp.where(key_chunk[None, :] <= q_chunk[:, None], s, -jnp.inf)
        p = jax.nn.softmax(s, axis=-1).astype(v.dtype)
        return jnp.einsum('bhqk,bhkd->bhqd', p, v)

    o = lax.map(block, (qb, jnp.arange(n_blocks)))
    o = jnp.transpose(o, (1, 0, 3, 2, 4)).reshape(B_, S_, B_WIDTH)
    return rms_norm(o, out_g)


def gmlp_spatial_gate(zc, ln_g, ln_b, w_s, b_s, out_g):
    B_, S_, _ = zc.shape
    z = jax.nn.gelu(zc, approximate=False)
    u, v = jnp.split(z, 2, axis=-1)
    v = layer_norm(v, ln_g, ln_b)
    vb = v.reshape(B_, S_ // C_BLOCK, C_BLOCK, C_GROUPS, C_GROUP_DIM)
    tri = jnp.tril(jnp.ones((C_BLOCK, C_BLOCK), dtype=bool))
    w = jnp.where(tri[None], w_s, 0)
    sv = jnp.einsum('gts,bnsgc->bntgc', w, vb) + jnp.transpose(b_s)[None, None, :, :, None]
    y = u * sv.reshape(B_, S_, C_WIDTH)
    return rms_norm(y, out_g)


def swiglu(h, wg, wu, wd):
    return (jax.nn.silu(h @ wg) * (h @ wu)) @ wd


def moe_swiglu(h, router, wg, wu, wd):
    B_, S_, D_ = h.shape
    t = h.reshape(B_ * S_, D_)
    logits = (t @ router).astype(jnp.float32)
    top_v, top_i = lax.top_k(logits, TOP_K)
    gates = jax.nn.softmax(top_v, axis=-1)
    gate_full = jnp.sum(jax.nn.one_hot(top_i, N_EXPERTS, dtype=jnp.float32) * gates[..., None], axis=1)
    out = jnp.zeros_like(t)
    for e in range(N_EXPERTS):
        out = out + gate_full[:, e:e + 1].astype(t.dtype) * swiglu(t, wg[e], wu[e], wd[e])
    return out.reshape(B_, S_, D_)


def setup_inputs(seed: int = 0) -> dict:
    key = jax.random.key(seed)
    ks = iter(jax.random.split(key, 64))
    nrm = lambda shape, scale: jax.random.normal(next(ks), shape, jnp.float32) * scale
    gain = lambda shape: 1.0 + nrm(shape, 0.05)
    offs = jax.random.randint(next(ks), (BATCH, 1), 0, 64) * CHUNK
    positions = (offs + jnp.arange(SEQ, dtype=jnp.int32)[None, :]).astype(jnp.int32)
    return {
        'x': nrm((BATCH, SEQ, D_MODEL), 1.0),
        'positions': positions,
        'mix_norm_g': gain((DEPTH, D_MODEL)),
        'w_in': nrm((DEPTH, D_MODEL, IN_COLS), D_MODEL ** -0.5),
        'shift_mu': jax.random.uniform(next(ks), (DEPTH, A_COLS), jnp.float32, 0.1, 0.9),
        'a_w0': jax.random.uniform(next(ks), (DEPTH, A_WIDTH), jnp.float32, -6.0, 1.0),
        'a_w_up': nrm((DEPTH, A_W_LORA, A_WIDTH), 0.1 * A_W_LORA ** -0.5),
        'a_a0': nrm((DEPTH, A_WIDTH), 0.5),
        'a_a_up': nrm((DEPTH, A_A_LORA, A_WIDTH), 0.1 * A_A_LORA ** -0.5),
        'a_g_up': nrm((DEPTH, A_G_LORA, A_WIDTH), A_G_LORA ** -0.5),
        'a_k_k': 0.85 + nrm((DEPTH, A_WIDTH), 0.1),
        'a_k_a': 1.0 + nrm((DEPTH, A_WIDTH), 0.1),
        'a_r_k': nrm((DEPTH, A_HEADS, A_HEAD_DIM), 0.1),
        'a_ln_g': gain((DEPTH, A_WIDTH)),
        'a_ln_b': nrm((DEPTH, A_WIDTH), 0.02),
        'a_v0': nrm((N_VRES, A_WIDTH), 0.5),
        'a_v_down': nrm((N_VRES, A_WIDTH, A_V_LORA), A_WIDTH ** -0.5),
        'a_v_up': nrm((N_VRES, A_V_LORA, A_WIDTH), 0.1 * A_V_LORA ** -0.5),
        'b_q_norm_g': gain((DEPTH, B_Q_LORA)),
        'b_w_uq': nrm((DEPTH, B_Q_LORA, B_HEADS * B_QK), B_Q_LORA ** -0.5),
        'b_kv_norm_g': gain((DEPTH, B_KV_LORA)),
        'b_w_ukv': nrm((DEPTH, B_KV_LORA, B_HEADS * (B_NOPE + B_V)), B_KV_LORA ** -0.5),
        'b_q_head_g': gain((DEPTH, B_QK)),
        'b_k_head_g': gain((DEPTH, B_QK)),
        'b_out_g': gain((DEPTH, B_WIDTH)),
        'c_ln_g': gain((DEPTH, C_WIDTH)),
        'c_ln_b': nrm((DEPTH, C_WIDTH), 0.02),
        'c_w_s': nrm((DEPTH, C_GROUPS, C_BLOCK, C_BLOCK), 0.5 * C_BLOCK ** -0.5),
        'c_b_s': 1.0 + nrm((DEPTH, C_GROUPS, C_BLOCK), 0.1),
        'c_out_g': gain((DEPTH, C_WIDTH)),
        'w_out': nrm((DEPTH, D_MIX, D_MODEL), D_MIX ** -0.5),
        'ffn_norm_g': gain((DEPTH, D_MODEL)),
        'dense_w_gate': nrm((N_DENSE, D_MODEL, D_FF_DENSE), D_MODEL ** -0.5),
        'dense_w_up': nrm((N_DENSE, D_MODEL, D_FF_DENSE), D_MODEL ** -0.5),
        'dense_w_down': nrm((N_DENSE, D_FF_DENSE, D_MODEL), D_FF_DENSE ** -0.5),
        'moe_router': nrm((N_MOE, D_MODEL, N_EXPERTS), D_MODEL ** -0.5),
        'moe_w_gate': nrm((N_MOE, N_EXPERTS, D_MODEL, D_FF_EXPERT), D_MODEL ** -0.5),
        'moe_w_up': nrm((N_MOE, N_EXPERTS, D_MODEL, D_FF_EXPERT), D_MODEL ** -0.5),
        'moe_w_down': nrm((N_MOE, N_EXPERTS, D_FF_EXPERT, D_MODEL), D_FF_EXPERT ** -0.5),
    }


def reference(x, positions, mix_norm_g, w_in, shift_mu, a_w0, a_w_up, a_a0, a_a_up, a_g_up,
              a_k_k, a_k_a, a_r_k, a_ln_g, a_ln_b, a_v0, a_v_down, a_v_up,
              b_q_norm_g, b_w_uq, b_kv_norm_g, b_w_ukv, b_q_head_g, b_k_head_g, b_out_g,
              c_ln_g, c_ln_b, c_w_s, c_b_s, c_out_g, w_out, ffn_norm_g,
              dense_w_gate, dense_w_up, dense_w_down, moe_router, moe_w_gate, moe_w_up, moe_w_down):
    v_first = None
    for l in range(DEPTH):
        h = rms_norm(x, mix_norm_g[l])
        z = h @ w_in[l]
        za, zb, zc = jnp.split(z, (A_COLS, A_COLS + B_COLS), axis=-1)
        if l > 0:
            vres = (a_v0[l - 1], a_v_down[l - 1], a_v_up[l - 1])
        else:
            vres = (None, None, None)
        ya, v_l = rwkv7_time_mix(za, shift_mu[l], a_w0[l], a_w_up[l], a_a0[l], a_a_up[l], a_g_up[l],
                                 a_k_k[l], a_k_a[l], a_r_k[l], a_ln_g[l], a_ln_b[l], v_first, *vres)
        if l == 0:
            v_first = v_l
        yb = mla_attention(zb, positions, b_q_norm_g[l], b_w_uq[l], b_kv_norm_g[l], b_w_ukv[l],
                           b_q_head_g[l], b_k_head_g[l], b_out_g[l])
        yc = gmlp_spatial_gate(zc, c_ln_g[l], c_ln_b[l], c_w_s[l], c_b_s[l], c_out_g[l])
        x = x + jnp.concatenate([ya, yb, yc], axis=-1) @ w_out[l]
        h = rms_norm(x, ffn_norm_g[l])
        if l % 2 == 0:
            i = l // 2
            x = x + swiglu(h, dense_w_gate[i], dense_w_up[i], dense_w_down[i])
        else:
            i = l // 2
            x = x + moe_swiglu(h, moe_router[i], moe_w_gate[i], moe_w_up[i], moe_w_down[i])
    return x
```

```python
import math, time, sys, contextlib
import numpy as np
import ml_dtypes
from concourse.bass_utils import run_bass_kernel_spmd
import contextlib
import numpy as np
import concourse.bass as bass
import concourse.mybir as mybir

F32 = mybir.dt.float32
BF16 = mybir.dt.bfloat16
I32 = mybir.dt.int32
AF = mybir.ActivationFunctionType
ALU = mybir.AluOpType
AX = mybir.AxisListType

EPOCH = 30000


class Buf:
    _n = 0

    def __init__(self, S, t, kind):
        self.S = S
        self.t = t
        self.kind = kind
        Buf._n += 1
        self.id = Buf._n
        self.lw = {}
        self.lr = {}
        self.dsem = None
        self.dcnt = 0

    def __getitem__(self, idx):
        return self.t[idx]

    def view(self, ap):
        b = Buf(self.S, ap, self.kind)
        b.lw = self.lw; b.lr = self.lr; b.id = self.id
        b.parent = self
        return b

    def reset(self):
        self.lw.clear(); self.lr.clear(); self.dsem = None; self.dcnt = 0


class Eng:
    def __init__(self, S, name, h):
        self.S = S
        self.name = name
        self.h = h
        self.seq = 0
        self.known = {}

    def cur_event(self):
        ep = (self.seq - 1) // EPOCH
        return (self.name, ep), (self.seq - 1) % EPOCH + 1


class Sched:
    _phase = 0

    def __init__(self, nc):
        self.nc = nc
        Sched._phase += 1
        self.pid = Sched._phase
        self.es = contextlib.ExitStack()
        self.E = {
            "pe": Eng(self, "pe", nc.tensor),
            "act": Eng(self, "act", nc.scalar),
            "dve": Eng(self, "dve", nc.vector),
            "pool": Eng(self, "pool", nc.gpsimd),
            "sp": Eng(self, "sp", nc.sync),
        }
        self.semtab = {}
        self.semh = []
        self.final = {}
        self.dram_out = []
        self.ncc = 0

    def sem(self, name):
        h = self.nc.alloc_semaphore("p%d_%s" % (self.pid, name))
        self.semh.append(h)
        return h

    def sbuf(self, name, shape, dt=F32):
        t = self.es.enter_context(self.nc.sbuf_tensor("sb%d_%s" % (self.pid, name), list(shape), dt))
        return Buf(self, t, "sbuf")

    def psum(self, name, shape, dt=F32):
        t = self.es.enter_context(self.nc.psum_tensor("ps%d_%s" % (self.pid, name), list(shape), dt))
        return Buf(self, t, "psum")

    def dram(self, name, shape, dt=F32, kind="Internal"):
        t = self.nc.dram_tensor(name, list(shape), dt, kind=kind)
        b = Buf(self, t.ap(), "dram")
        b.io = kind
        if kind == "ExternalOutput":
            self.dram_out.append(b)
        return b

    def _semfor(self, key):
        if key not in self.semtab:
            self.semtab[key] = self.sem("s_%s_%s" % (str(key[0]), str(key[1])))
        return self.semtab[key]

    def _wait(self, eng, key, val):
        if eng.known.get(key, 0) >= val:
            return
        eng.h.wait_ge(self._semfor(key), val)
        eng.known[key] = val
        if key[0] in self.E:
            for ep in range(key[1]):
                eng.known[(key[0], ep)] = EPOCH

    def _deps(self, eng, reads, writes, skipkey=None):
        for b in reads:
            for k, v in b.lw.items():
                if (k[0] == "pe" and eng.name == "pe"):
                    continue
                self._wait(eng, k, v)
        for b in writes:
            for k, v in b.lw.items():
                if (k[0] == "pe" and eng.name == "pe") or k == skipkey:
                    continue
                self._wait(eng, k, v)
            for k, v in b.lr.items():
                if (k[0] == "pe" and eng.name == "pe"):
                    continue
                self._wait(eng, k, v)

    def _commit(self, key, val, reads, writes):
        if self.final.get(key, 0) < val:
            self.final[key] = val
        for b in reads:
            if b.lr.get(key, 0) < val:
                b.lr[key] = val
        for b in writes:
            if b.kind == "dram":
                b.lw[key] = val
                continue
            b.lw.clear(); b.lw[key] = val
            b.lr.clear()

    def op(self, en, fn, reads=(), writes=()):
        eng = self.E[en]
        reads = [b for b in reads if b is not None]
        writes = [b for b in writes if b is not None]
        self._deps(eng, reads, writes)
        ins = fn(eng.h)
        eng.seq += 1
        key, val = eng.cur_event()
        ins.then_inc(self._semfor(key), 1)
        self._commit(key, val, reads, writes)
        return ins

    def _dma_common(self, qn, reads, writes, issue):
        eng = self.E[qn]
        reads = [b for b in reads if b is not None]
        writes = [b for b in writes if b is not None]
        cand = [b for b in writes if b.kind != "dram"] + [b for b in reads if b.kind != "dram"]
        owner = cand[0] if cand else (writes[0] if writes else reads[0])
        if owner.dsem is None:
            owner.dsem = {}
        key = ("ds" if qn == "pool" else "d", owner.id)
        owner.dsem[key] = owner.dsem.get(key, 0) + 16
        self._deps(eng, reads, [b for b in writes if b.kind != "dram"], skipkey=key)
        val = owner.dsem[key]
        ins = issue(eng.h)
        ins.then_inc(self._semfor(key), 16)
        self._commit(key, val, reads, writes)
        return ins

    def dma(self, qn, out, in_, reads=(), writes=(), **kw):
        return self._dma_common(qn, reads, writes, lambda h: h.dma_start(out=out, in_=in_, **kw))

    def idma(self, out, in_view, idx_ap, reads=(), writes=()):
        return self._dma_common("pool", reads, writes, lambda h: h.indirect_dma_start(
            out=out, out_offset=None, in_=in_view, in_offset=bass.IndirectOffsetOnAxis(ap=idx_ap, axis=0)))

    def allgather(self, in_buf, out_buf, groups):
        eng = self.E["pool"]
        for k, v in in_buf.lw.items():
            self._wait(eng, k, v)
        for k, v in list(out_buf.lw.items()) + list(out_buf.lr.items()):
            self._wait(eng, k, v)
        self.ncc += 1
        key = ("cc", self.ncc)
        ins = self.nc.gpsimd.collective_compute("AllGather", ALU.bypass, replica_groups=groups, ins=[in_buf.t], outs=[out_buf.t])
        ins.then_inc(self._semfor(key), 1)
        self._commit(key, 1, [in_buf], [])
        out_buf.lw.clear(); out_buf.lw[key] = 1; out_buf.lr.clear()
        return ins

    def drain(self):
        sp = self.E["sp"]
        for k, v in list(self.final.items()):
            self._wait(sp, k, v)

    def phase_end(self, last=False):
        self.drain()
        self.nc.all_engine_barrier()
        if not last:
            self.nc.clear_and_free_semaphores(self.semh)
            self.nc.all_engine_barrier()
        self.es.close()
BF = ml_dtypes.bfloat16
D = 1024; INC = 2208; EPS = 1e-6; TT = 512
CH = [(s, 128) for s in range(0, 1664, 128)] + [(1664, 32)] + [(s, 128) for s in range(1696, 2208, 128)]
TWO_PI = 2.0 * math.pi
T = 64; NCH = TT // T
LWS = -0.6065306597126334
GN_EPS = 64e-5
QT = 512
SCALE = 1.0 / math.sqrt(96.0)
GROUPS = [[0, 1, 2, 3], [4, 5, 6, 7]]
RAB = 1152
RP = 4 * 128 + 4 * 390

class NS:
    pass

CR_F = 128
CR_B = 192
CR_V = 1024

def gaddr(cr, R, i, r):
    m = r // cr
    nr = np.minimum(cr, R - m * cr)
    return 4 * cr * m + i * nr + (r - m * cr)

def allgather_rows(S, send, gath, R, cr):
    m = 0
    while m * cr < R:
        nr = min(cr, R - m * cr)
        S.allgather(send.view(send.t[m*cr:m*cr+nr, :]), gath.view(gath.t[4*cr*m:4*cr*m + 4*nr, :]), GROUPS)
        m += 1

def a2_consts():
    c = np.zeros((6, 128, 128), np.float32)
    c[0] = 1.0
    c[1] = np.eye(128)
    P = np.zeros((96, 96), np.float32)
    for i in range(16):
        P[64 + i, 80 + i] = -1.0
        P[80 + i, 64 + i] = 1.0
    c[2][:96, :96] = P.T
    s = np.arange(128)[:, None]; t = np.arange(128)[None, :]
    c[3] = (s <= t)
    inv = (10000.0 ** (-np.arange(0, 32, 2, dtype=np.float32) / 32)).astype(np.float32)
    c[4][64:80, 0] = inv; c[4][80:96, 0] = inv
    c[4][0:64, 1] = 1.0
    return np.ascontiguousarray(c.transpose(1, 0, 2))


def rwkv_consts():
    c = np.zeros((8, 128, 128), np.float32)
    bd = np.zeros((128, 128), np.float32); bd[:64, :64] = 1; bd[64:, 64:] = 1
    s = np.arange(128)[:, None]; t = np.arange(128)[None, :]
    c[0] = bd
    c[1] = bd * (s <= t)
    c[2] = bd * (s < t)
    c[3] = bd * (s > t)
    c[4] = bd * (s <= t)
    c[5] = -c[4]
    c[6] = np.eye(128)
    c[7] = bd / 64.0
    return np.ascontiguousarray(c.transpose(1, 0, 2))


def masks_C():
    m = np.zeros((4, 128, QT), np.float32)
    for d in range(4):
        kk = d * 128 + np.arange(128)[:, None]; qq = np.arange(QT)[None, :]
        m[d] = (kk // 64 <= qq // 64)
    return m.astype(BF)


def d_consts():
    c = np.zeros((2, 128, 128), np.float32)
    c[0] = 1.0; c[1] = np.eye(128)
    return np.ascontiguousarray(c.transpose(1, 0, 2))


def phase_A(nc, G, l):
    NT = G.NT
    S = Sched(nc)
    for b_ in G.persist:
        b_.reset()
    xT_d = G.xT_d; w_d = G.inp["w_in%d" % l]; pc_d = G.inp["pcolA%d" % l]; pos_d = G.inp["pos"]
    wuq_d = G.inp["wuq%d" % l]; wukv_d = G.inp["wukv%d" % l]; ws_d = G.inp["ws%d" % l]; bs_d = G.inp["bs%d" % l]; cst_d = G.inp["cstA"]
    sAf = G.sAf[l]; sAb = G.sAb[l]; sAv = G.sAv[l]; ycl = G.ycl[l]
    pc = S.sbuf("pc", [128, 32]); cst = S.sbuf("cst", [128, 6, 128])
    S.dma("sp", pc[:], pc_d[:], reads=[pc_d], writes=[pc])
    S.dma("sp", cst[:], cst_d[:], reads=[cst_d], writes=[cst])
    ONES = cst[:, 0, :]; IDENT = cst[:, 1, :]; PT = cst[0:96, 2, 0:96]; MASK = cst[:, 3, :]
    INVF = cst[0:96, 4, 0:1]; NOPE = cst[0:96, 4, 1:2]
    onesb = S.sbuf("onesb", [128, 128], BF16); identb = S.sbuf("identb", [128, 128], BF16); ptb = S.sbuf("ptb", [96, 96], BF16)
    onesrow = S.sbuf("onesrow", [1, 128], BF16)
    S.op("dve", lambda h: h.tensor_copy(out=onesb[:], in_=ONES), reads=[cst], writes=[onesb])
    S.op("dve", lambda h: h.tensor_copy(out=identb[:], in_=IDENT), reads=[cst], writes=[identb])
    S.op("dve", lambda h: h.tensor_copy(out=ptb[:], in_=PT), reads=[cst], writes=[ptb])
    S.op("pool", lambda h: h.memset(onesrow[:], 1.0), writes=[onesrow])
    epsc = S.sbuf("epsc", [128, 1]); S.op("pool", lambda h: h.memset(epsc[:], EPS), writes=[epsc])
    pic = S.sbuf("pic", [128, 1]); S.op("pool", lambda h: h.memset(pic[:], -math.pi), writes=[pic])

    NPS = 7
    slots = [S.psum(f"slot{i}", [128, TT]) for i in range(NPS)]
    ptr = S.psum("ptr", [128, 1024], BF16)
    psc = [0]
    def getps():
        psc[0] += 1
        return slots[psc[0] % NPS]
    tmpc = [0]
    tmps = [S.sbuf(f"tmp{i}", [128, TT]) for i in range(4)]
    def gettmp():
        tmpc[0] += 1
        return tmps[tmpc[0] % 4]

    xT = [S.sbuf(f"xT{k}", [128, NT], F32) for k in range(8)]
    wb = [S.sbuf(f"wb{k}", [128, INC], BF16) for k in range(8)]
    wst = [S.sbuf(f"wst{i}", [128, INC], F32) for i in range(1)] * 2
    for k in range(8):
        S.dma("sp", xT[k][:], G.xsrc[l][k*128:(k+1)*128, :], reads=[G.xsrc[l]], writes=[xT[k]])
    for k in range(8):
        st = wst[k % 2]
        S.dma("pool", st[:], w_d[k*128:(k+1)*128, :], reads=[w_d], writes=[st])
        S.op("pool", lambda h: h.tensor_copy(out=wb[k][:], in_=st[:]), reads=[st], writes=[wb[k]])
    wuq = [S.sbuf(f"wuq{k}", [128, 576], BF16) for k in range(2)]
    for k in range(2):
        st = wst[k % 2]
        S.dma("pool", st[:, 0:576], wuq_d[k*128:(k+1)*128, :], reads=[wuq_d], writes=[st])
        S.op("pool", lambda h: h.tensor_copy(out=wuq[k][:], in_=st[:, 0:576]), reads=[st], writes=[wuq[k]])
    wukv = S.sbuf("wukv", [128, 768], BF16)
    S.dma("pool", wst[0][:, 0:768], wukv_d[:, :], reads=[wukv_d], writes=[wst[0]])
    S.op("pool", lambda h: h.tensor_copy(out=wukv[:], in_=wst[0][:, 0:768]), reads=[wst[0]], writes=[wukv])
    wukv_v = S.sbuf("wukv_v", [128, 6, 64], BF16)
    S.op("pool", lambda h: h.tensor_copy(out=wukv_v[:], in_=wukv[:].rearrange("p (h c) -> p h c", c=128)[:, :, 64:128]), reads=[wukv], writes=[wukv_v])
    wsT = [S.sbuf(f"wsT{g}", [128, 128], BF16) for g in range(4)]
    wsl = S.sbuf("wsl", [128, 4, 128], F32)
    S.dma("sp", wsl[:], ws_d.t.rearrange("g t s -> t g s"), reads=[ws_d], writes=[wsl])
    for g in range(4):
        p = getps()
        S.op("pe", lambda h: h.transpose(p[:, 0:128], wsl[:, g, :], IDENT), reads=[wsl, cst], writes=[p])
        S.op("dve", lambda h: h.tensor_tensor(out=wsT[g][:], in0=p[:, 0:128], in1=MASK, op=ALU.mult), reads=[p, cst], writes=[wsT[g]])
    bsr = S.sbuf("bsr", [1, 512], BF16); bsf = S.sbuf("bsf", [1, 512], F32)
    S.dma("sp", bsf[:], bs_d[:, :], reads=[bs_d], writes=[bsf])
    S.op("dve", lambda h: h.tensor_copy(out=bsr[:], in_=bsf[:]), reads=[bsf], writes=[bsr])

    posi = S.sbuf("posi", [96, TT], I32); posf = S.sbuf("posf", [96, TT], F32)
    cosf = S.sbuf("cosf", [96, TT], F32); sinf = S.sbuf("sinf", [96, TT], F32)
    def rope_tables(sl):
        S.dma("sp", posi[:], pos_d.t[0:1, sl].partition_broadcast(96), reads=[pos_d], writes=[posi])
        S.op("dve", lambda h: h.tensor_copy(out=posf[:], in_=posi[:]), reads=[posi], writes=[posf])
        S.op("dve", lambda h: h.tensor_scalar(out=posf[:], in0=posf[:], scalar1=INVF, scalar2=None, op0=ALU.mult), reads=[posf, cst], writes=[posf])
        for dst, shift in [(sinf, 0.0), (cosf, 0.5 * math.pi)]:
            S.op("dve", lambda h: h.tensor_scalar(out=dst[:], in0=posf[:], scalar1=shift, scalar2=None, op0=ALU.add), reads=[posf], writes=[dst])
            S.op("dve", lambda h: h.tensor_scalar(out=rrf[:], in0=dst[:], scalar1=1.0 / TWO_PI, scalar2=None, op0=ALU.mult), reads=[dst], writes=[rrf])
            S.op("dve", lambda h: h.tensor_copy(out=rri[:], in_=rrf[:]), reads=[rrf], writes=[rri])
            S.op("dve", lambda h: h.tensor_copy(out=rrf[:], in_=rri[:]), reads=[rri], writes=[rrf])
            S.op("dve", lambda h: h.scalar_tensor_tensor(out=dst[:], in0=rrf[:], scalar=-6.28125, in1=dst[:], op0=ALU.mult, op1=ALU.add), reads=[rrf, dst], writes=[dst])
            S.op("dve", lambda h: h.scalar_tensor_tensor(out=dst[:], in0=rrf[:], scalar=-(TWO_PI - 6.28125), in1=dst[:], op0=ALU.mult, op1=ALU.add), reads=[rrf, dst], writes=[dst])
            S.op("dve", lambda h: h.tensor_scalar(out=rrf[:], in0=dst[:], scalar1=math.pi, scalar2=None, op0=ALU.is_gt), reads=[dst], writes=[rrf])
            S.op("dve", lambda h: h.scalar_tensor_tensor(out=dst[:], in0=rrf[:], scalar=-TWO_PI, in1=dst[:], op0=ALU.mult, op1=ALU.add), reads=[rrf, dst], writes=[dst])
            S.op("dve", lambda h: h.tensor_scalar(out=rrf[:], in0=dst[:], scalar1=-math.pi, scalar2=None, op0=ALU.is_lt), reads=[dst], writes=[rrf])
            S.op("dve", lambda h: h.scalar_tensor_tensor(out=dst[:], in0=rrf[:], scalar=TWO_PI, in1=dst[:], op0=ALU.mult, op1=ALU.add), reads=[rrf, dst], writes=[dst])
            S.op("act", lambda h: h.activation(out=dst[:], in_=dst[:], func=AF.Sin), reads=[dst], writes=[dst])
    rrf = S.sbuf("rrf", [96, TT], F32); rri = S.sbuf("rri", [96, TT], I32)

    sq = [S.sbuf(f"sq{i}", [128, TT], BF16) for i in range(2)]
    hT = [S.sbuf(f"hT{k}", [128, TT], BF16) for k in range(8)]
    rs = S.sbuf("rs", [128, TT], F32)
    zo = [S.sbuf(f"zo{i}", [128, TT], F32) for i in range(3)]
    zB = [S.sbuf(f"zB{i}", [128, TT], F32) for i in range(4)]
    zC = [S.sbuf(f"zC{i}", [128, TT], F32) for i in range(4)]
    cqn = [S.sbuf(f"cqn{i}", [128, TT], BF16) for i in range(2)]
    ckvn = S.sbuf("ckvn", [128, TT], BF16)
    kfull = S.sbuf("kfull", [96, TT], F32)
    sq96 = S.sbuf("sq96", [96, TT], BF16)
    rs96 = S.sbuf("rs96", [96, TT], F32)
    qn = S.sbuf("qn", [96, TT], BF16)
    t1 = S.sbuf("t1", [96, TT], F32); t2 = S.sbuf("t2", [96, TT], F32)
    qo = [S.sbuf(f"qo{i}", [96, TT], BF16) for i in range(3)]
    vo = [S.sbuf(f"vo{i}", [128, 384], BF16) for i in range(2)]
    vnb = [S.sbuf(f"vnb{i}", [128, TT], BF16) for i in range(2)]
    vtok = [S.sbuf(f"vtok{i}", [128, 128], BF16) for i in range(2)]
    yg = [S.sbuf(f"yg{i}", [128, TT], F32) for i in range(2)]
    yco = [S.sbuf(f"yco{i}", [128, TT], F32) for i in range(2)]
    cnt = [0]; qc = [0]

    def headnorm_rope(src_ps_or_sb, srcbuf, gcol, out_d, h, sl, t):
        S.op("act", lambda hh: hh.activation(out=sq96[:], in_=src_ps_or_sb, func=AF.Square), reads=[srcbuf], writes=[sq96])
        p = getps()
        S.op("pe", lambda hh: hh.matmul(p[0:96, :], lhsT=onesb[0:96, 0:96], rhs=sq96[:], start=True, stop=True), reads=[onesb, sq96], writes=[p])
        S.op("act", lambda hh: hh.activation(out=rs96[:], in_=p[0:96, :], func=AF.Sqrt, scale=1.0/96, bias=epsc[0:96, :]), reads=[p, epsc], writes=[rs96])
        S.op("dve", lambda hh: hh.reciprocal(out=rs96[:], in_=rs96[:]), reads=[rs96], writes=[rs96])
        S.op("dve", lambda hh: hh.scalar_tensor_tensor(out=qn[:], in0=src_ps_or_sb, scalar=gcol, in1=rs96[:], op0=ALU.mult, op1=ALU.mult), reads=[srcbuf, pc, rs96], writes=[qn])
        p2 = getps()
        S.op("pe", lambda hh: hh.matmul(p2[0:96, :], lhsT=ptb[:], rhs=qn[:], start=True, stop=True), reads=[ptb, qn], writes=[p2])
        S.op("pool", lambda hh: hh.tensor_tensor(out=t1[:], in0=qn[:], in1=cosf[:], op=ALU.mult), reads=[qn, cosf], writes=[t1])
        S.op("dve", lambda hh: hh.tensor_tensor(out=t2[:], in0=p2[0:96, :], in1=sinf[:], op=ALU.mult), reads=[p2, sinf], writes=[t2])
        o = qo[qc[0] % 3]; qc[0] += 1
        S.op("pool", lambda hh: hh.tensor_tensor(out=o[:], in0=t1[:], in1=t2[:], op=ALU.add), reads=[t1, t2], writes=[o])
        if out_d == "q":
            S.dma("sp", sAb[h*96:(h+1)*96, sl], o[:], reads=[o], writes=[sAb])
        else:
            dst = sAb.t[576 + h*96:576 + (h+1)*96, :].rearrange("d (j x) -> d j x", j=4)[:, :, t*128:(t+1)*128]
            S.dma("sp", dst, o[:].rearrange("d (j c) -> d j c", c=128), reads=[o], writes=[sAb])

    for t in range(NT // TT):
        sl = slice(t*TT, (t+1)*TT)
        rope_tables(sl)
        for k in range(8):
            s = sq[k % 2]
            S.op("act", lambda h: h.activation(out=s[:], in_=xT[k][:, sl], func=AF.Square), reads=[xT[k]], writes=[s])
            ps_ss = getps() if k == 0 else ps_ss
            S.op("pe", lambda h: h.matmul(ps_ss[:], lhsT=onesb[:], rhs=s[:], start=(k == 0), stop=(k == 7)), reads=[onesb, s], writes=[ps_ss])
        S.op("act", lambda h: h.activation(out=rs[:], in_=ps_ss[:], func=AF.Sqrt, scale=1.0/D, bias=epsc[:]), reads=[ps_ss, epsc], writes=[rs])
        S.op("dve", lambda h: h.reciprocal(out=rs[:], in_=rs[:]), reads=[rs], writes=[rs])
        for k in range(8):
            S.op("dve", lambda h: h.scalar_tensor_tensor(out=hT[k][:], in0=xT[k][:, sl], scalar=pc[:, k:k+1], in1=rs[:], op0=ALU.mult, op1=ALU.mult), reads=[xT[k], pc, rs], writes=[hT[k]])
        for m, (c0, mw) in enumerate(CH):
            pz = getps()
            for k in range(8):
                S.op("pe", lambda h: h.matmul(pz[:mw, :], lhsT=wb[k][:, c0:c0+mw], rhs=hT[k][:], start=(k == 0), stop=(k == 7)), reads=[wb[k], hT[k]], writes=[pz])
            if m < 10:
                o = zo[cnt[0] % 3]; cnt[0] += 1
                if m % 2 == 0:
                    S.op("act", lambda h: h.copy(out=o[:mw, :], in_=pz[:mw, :]), reads=[pz], writes=[o])
                else:
                    S.op("dve", lambda h: h.tensor_copy(out=o[:mw, :], in_=pz[:mw, :]), reads=[pz], writes=[o])
                S.dma("sp", sAf[c0:c0+mw, sl], o[:mw, :], reads=[o], writes=[sAf])
            elif m < 14:
                o = zB[m - 10]
                S.op("act", lambda h: h.copy(out=o[:mw, :], in_=pz[:mw, :]), reads=[pz], writes=[o])
            else:
                o = zC[m - 14]
                S.op("act", lambda h: h.activation(out=o[:], in_=pz[:], func=AF.Gelu), reads=[pz], writes=[o])
        ps1 = getps()
        for k in range(2):
            s = sq[k % 2]
            S.op("act", lambda h: h.activation(out=s[:], in_=zB[k][:], func=AF.Square), reads=[zB[k]], writes=[s])
            S.op("pe", lambda h: h.matmul(ps1[:], lhsT=onesb[:], rhs=s[:], start=(k == 0), stop=(k == 1)), reads=[onesb, s], writes=[ps1])
        S.op("act", lambda h: h.activation(out=rs[:], in_=ps1[:], func=AF.Sqrt, scale=1.0/256, bias=epsc[:]), reads=[ps1, epsc], writes=[rs])
        S.op("dve", lambda h: h.reciprocal(out=rs[:], in_=rs[:]), reads=[rs], writes=[rs])
        for k in range(2):
            S.op("dve", lambda h: h.scalar_tensor_tensor(out=cqn[k][:], in0=zB[k][:], scalar=pc[:, 8+k:9+k], in1=rs[:], op0=ALU.mult, op1=ALU.mult), reads=[zB[k], pc, rs], writes=[cqn[k]])
        for hd in range(6):
            pq = getps()
            for k in range(2):
                S.op("pe", lambda h: h.matmul(pq[0:96, :], lhsT=wuq[k][:, hd*96:(hd+1)*96], rhs=cqn[k][:], start=(k == 0), stop=(k == 1)), reads=[wuq[k], cqn[k]], writes=[pq])
            headnorm_rope(pq[0:96, :], pq, pc[0:96, 11:12], "q", hd, sl, t)
        s = sq[0]
        S.op("act", lambda h: h.activation(out=s[:], in_=zB[2][:], func=AF.Square), reads=[zB[2]], writes=[s])
        ps2 = getps()
        S.op("pe", lambda h: h.matmul(ps2[:], lhsT=onesb[:], rhs=s[:], start=True, stop=True), reads=[onesb, s], writes=[ps2])
        S.op("act", lambda h: h.activation(out=rs[:], in_=ps2[:], func=AF.Sqrt, scale=1.0/128, bias=epsc[:]), reads=[ps2, epsc], writes=[rs])
        S.op("dve", lambda h: h.reciprocal(out=rs[:], in_=rs[:]), reads=[rs], writes=[rs])
        S.op("dve", lambda h: h.scalar_tensor_tensor(out=ckvn[:], in0=zB[2][:], scalar=pc[:, 10:11], in1=rs[:], op0=ALU.mult, op1=ALU.mult), reads=[zB[2], pc, rs], writes=[ckvn])
        S.op("pool", lambda h: h.tensor_copy(out=kfull[64:96, :], in_=zB[3][0:32, :]), reads=[zB[3]], writes=[kfull])
        for hd in range(6):
            pk = getps()
            S.op("pe", lambda h: h.matmul(pk[0:64, :], lhsT=wukv[:, hd*128:hd*128+64], rhs=ckvn[:], start=True, stop=True), reads=[wukv, ckvn], writes=[pk])
            S.op("act", lambda h: h.copy(out=kfull[0:64, :], in_=pk[0:64, :]), reads=[pk], writes=[kfull])
            headnorm_rope(kfull[:], kfull, pc[0:96, 12:13], "k", hd, sl, t)
        for j in range(TT // 128):
            pv = getps()
            S.op("pe", lambda h: h.matmul(pv[:, 0:384], lhsT=ckvn[:, j*128:(j+1)*128], rhs=wukv_v[:].rearrange("p h c -> p (h c)"), start=True, stop=True), reads=[ckvn, wukv_v], writes=[pv])
            o = vo[j % 2]
            S.op("act", lambda h: h.copy(out=o[:], in_=pv[:, 0:384]), reads=[pv], writes=[o])
            S.dma("sp", sAv[j*(NT//4) + t*128: j*(NT//4) + (t+1)*128, :], o[:], reads=[o], writes=[sAv])
        pm = getps()
        for k in range(2):
            S.op("pe", lambda h: h.matmul(pm[:], lhsT=ONES, rhs=zC[2+k][:], start=(k == 0), stop=(k == 1)), reads=[cst, zC[2+k]], writes=[pm])
        vc = [gettmp(), gettmp()]
        for k in range(2):
            S.op("dve", lambda h: h.scalar_tensor_tensor(out=vc[k][:], in0=pm[:], scalar=-1.0/256, in1=zC[2+k][:], op0=ALU.mult, op1=ALU.add), reads=[pm, zC[2+k]], writes=[vc[k]])
        pvv = getps()
        for k in range(2):
            s2 = gettmp()
            S.op("pool", lambda h: h.tensor_tensor(out=s2[:], in0=vc[k][:], in1=vc[k][:], op=ALU.mult), reads=[vc[k]], writes=[s2])
            S.op("pe", lambda h: h.matmul(pvv[:], lhsT=ONES, rhs=s2[:], start=(k == 0), stop=(k == 1)), reads=[cst, s2], writes=[pvv])
        S.op("act", lambda h: h.activation(out=rs[:], in_=pvv[:], func=AF.Sqrt, scale=1.0/256, bias=epsc[:]), reads=[pvv, epsc], writes=[rs])
        S.op("dve", lambda h: h.reciprocal(out=rs[:], in_=rs[:]), reads=[rs], writes=[rs])
        for k in range(2):
            S.op("dve", lambda h: h.tensor_tensor(out=vc[k][:], in0=vc[k][:], in1=rs[:], op=ALU.mult), reads=[vc[k], rs], writes=[vc[k]])
            S.op("dve", lambda h: h.tensor_scalar(out=vnb[k][:], in0=vc[k][:], scalar1=pc[:, 13+k:14+k], scalar2=pc[:, 15+k:16+k], op0=ALU.mult, op1=ALU.add), reads=[vc[k], pc], writes=[vnb[k]])
        for j in range(TT // 128):
            bsl = slice(j*128, (j+1)*128)
            for k in range(2):
                S.op("pe", lambda h: h.transpose(ptr[:, k*128:(k+1)*128], vnb[k][:, bsl], identb[:]), reads=[vnb[k], identb], writes=[ptr])
                S.op("act", lambda h: h.copy(out=vtok[k][:], in_=ptr[:, k*128:(k+1)*128]), reads=[ptr], writes=[vtok[k]])
            for g in range(4):
                k = g // 2; hf = slice((g % 2)*64, (g % 2)*64 + 64)
                pg = getps()
                S.op("pe", lambda h: h.matmul(pg[:, 0:128], lhsT=vtok[k][:], rhs=wsT[g][:], start=True, stop=False), reads=[vtok[k], wsT[g]], writes=[pg])
                S.op("pe", lambda h: h.matmul(pg[:, 0:128], lhsT=onesrow[:], rhs=bsr[:, g*128:(g+1)*128], start=False, stop=True), reads=[onesrow, bsr], writes=[pg])
                S.op("dve", lambda h: h.tensor_tensor(out=yg[k][hf, bsl], in0=pg[hf, 0:128], in1=zC[k][hf, bsl], op=ALU.mult), reads=[pg, zC[k]], writes=[yg[k]])
        py = getps()
        for k in range(2):
            s2 = gettmp()
            S.op("pool", lambda h: h.tensor_tensor(out=s2[:], in0=yg[k][:], in1=yg[k][:], op=ALU.mult), reads=[yg[k]], writes=[s2])
            S.op("pe", lambda h: h.matmul(py[:], lhsT=ONES, rhs=s2[:], start=(k == 0), stop=(k == 1)), reads=[cst, s2], writes=[py])
        S.op("act", lambda h: h.activation(out=rs[:], in_=py[:], func=AF.Sqrt, scale=1.0/256, bias=epsc[:]), reads=[py, epsc], writes=[rs])
        S.op("dve", lambda h: h.reciprocal(out=rs[:], in_=rs[:]), reads=[rs], writes=[rs])
        for k in range(2):
            o = yco[k]
            S.op("dve", lambda h: h.scalar_tensor_tensor(out=o[:], in0=yg[k][:], scalar=pc[:, 17+k:18+k], in1=rs[:], op0=ALU.mult, op1=ALU.mult), reads=[yg[k], pc, rs], writes=[o])
            S.dma("sp", ycl[k*128:(k+1)*128, sl], o[:], reads=[o], writes=[ycl])

    allgather_rows(S, sAf, G.gAf[l], 1280, CR_F)
    allgather_rows(S, sAb, G.gAb[l], RAB, CR_B)
    allgather_rows(S, sAv, G.gAv[l], NT, min(CR_V, NT))
    S.phase_end()


def phase_B(nc, G, l):
    NT = G.NT; SEQ = 4 * NT; NTILE = SEQ // TT; NTL = NT // TT
    S = Sched(nc)
    for b_ in G.persist:
        b_.reset()
    gAf = G.gAf[l]; sP = G.sP[l]
    pcol_d = G.inp["pcolB%d" % l]; plo_d = G.inp["plo%d" % l]; wup_d = G.inp["wup%d" % l]; aup_d = G.inp["aup%d" % l]
    gup_d = G.inp["gup%d" % l]; w0row_d = G.inp["w0row%d" % l]; cst_d = G.inp["cstB"]
    if l == 1:
        vdown_d = G.inp["vdown"]; vup_d = G.inp["vup"]
    idxB = S.sbuf("idxB", [128, 3, NTILE], I32); idxBh = S.sbuf("idxBh", [128, 3, NTILE], I32)
    S.dma("sp", idxB[:], G.inp["idxB"].t, reads=[G.inp["idxB"]], writes=[idxB])
    S.dma("sp", idxBh[:], G.inp["idxBh"].t, reads=[G.inp["idxBh"]], writes=[idxBh])
    pcol = S.sbuf("pcol", [128, 16]); plo = S.sbuf("plo", [64, 4])
    wup = S.sbuf("wup", [32, 128]); aup = S.sbuf("aup", [32, 128]); gup = S.sbuf("gup", [64, 128])
    w0row = S.sbuf("w0row", [1, 128]); cst = S.sbuf("cst", [128, 8, 128])
    onesrow = S.sbuf("onesrow", [1, 128])
    omk = S.sbuf("omk", [128, 1])
    for sb, dr in [(pcol, pcol_d), (plo, plo_d), (wup, wup_d), (aup, aup_d), (gup, gup_d), (w0row, w0row_d)]:
        S.dma("sp", sb[:], dr[:], reads=[dr], writes=[sb])
    S.dma("sp", cst[:], cst_d[:], reads=[cst_d], writes=[cst])
    if l == 1:
        vdown = S.sbuf("vdown", [128, 3, 16]); vup = S.sbuf("vup", [16, 128])
        S.dma("sp", vdown[:], vdown_d[:], reads=[vdown_d], writes=[vdown])
        S.dma("sp", vup[:], vup_d[:], reads=[vup_d], writes=[vup])
    S.op("pool", lambda h: h.memset(onesrow[:], 1.0), writes=[onesrow])
    S.op("dve", lambda h: h.tensor_scalar(out=omk[:], in0=pcol[:, 6:7], scalar1=-1.0, scalar2=1.0, op0=ALU.mult, op1=ALU.add), reads=[pcol], writes=[omk])
    ONESB = cst[:, 0, :]; TRI = cst[:, 1, :]; TRIS = cst[:, 2, :]; MSU = cst[:, 2, :]; MSL = cst[:, 3, :]
    MUI = cst[:, 4, :]; MNUI = cst[:, 5, :]; IDENT = cst[:, 6, :]; MEANB = cst[:, 7, :]
    C_MUR, C_MUK, C_MUV, C_W0, C_A0, C_KK, C_KA, C_RK, C_LNG, C_LNB, C_V0, C_MU0V = range(12)

    def col(i, n=128):
        return pcol[0:n, i:i+1]

    NB = 2
    raw = {}
    names = [("zr", 128), ("zk", 128), ("zv", 128), ("zw", 32), ("za", 32), ("zg", 64)]
    if l == 1:
        names += [("zva0", 128), ("zva1", 128), ("zva2", 128), ("zv0", 128)]
    for nm, rows in names:
        raw[nm] = [S.sbuf(f"raw_{nm}{i}", [rows, TT + 1]) for i in range(1)] * NB
        S.op("pool", lambda h: h.memset(raw[nm][0][:, 0:1], 0.0), writes=[raw[nm][0]])
    tmp = [S.sbuf(f"tmp{i}", [128, TT]) for i in range(3)]
    tmpc = [0]
    def gettmp():
        tmpc[0] += 1
        return tmp[tmpc[0] % 3]
    sh = {nm: S.sbuf(f"sh_{nm}", [rows, TT]) for nm, rows in names}
    th = S.sbuf("th", [32, TT])
    sgd = S.sbuf("sgd", [64, TT])
    a_t = S.sbuf("a_t", [128, TT]); g_t = [S.sbuf(f"g_t{i}", [128, TT]) for i in range(NB)]
    kk = S.sbuf("kk", [128, TT]); kap = S.sbuf("kap", [128, TT]); kmod = S.sbuf("kmod", [128, TT])
    b_t = S.sbuf("b_t", [128, TT]); v_t = [S.sbuf(f"v_t{i}", [128, TT]) for i in range(NB)]
    bv = [S.sbuf(f"bv{i}", [128, TT]) for i in range(NB)]
    sq = S.sbuf("sq", [128, TT]); nrm = S.sbuf("nrm", [128, TT])
    sgtok = [S.sbuf(f"sgtok{i}", [128, 128]) for i in range(2)]
    eL = S.sbuf("eL", [128, TT]); eLx = S.sbuf("eLx", [128, TT]); enL = S.sbuf("enL", [128, TT])
    gam = [S.sbuf(f"gam{i}", [128, NCH]) for i in range(NB)]
    vd_sb = S.sbuf("vd_sb", [16, TT]); sv = S.sbuf("sv", [128, TT])
    bd = {nm: [S.sbuf(f"bd_{nm}{i}", [128, NCH, 128]) for i in range(NB)] for nm in ["RT", "KpT", "KT", "BT", "VT"]}
    for nm in bd:
        for i in range(NB):
            S.op("pool", lambda h: h.memset(bd[nm][i][:], 0.0), writes=[bd[nm][i]])
    yT = [S.sbuf(f"yT{i}", [128, TT]) for i in range(NB)]
    yo = [S.sbuf(f"yo{i}", [128, TT]) for i in range(1)] * NB
    pL = S.psum("pL", [128, TT]); pLx = S.psum("pLx", [128, TT])
    NPS = 6
    slots = [S.psum(f"slot{i}", [128, TT]) for i in range(NPS)]
    psc = [0]
    def getps():
        psc[0] += 1
        s_ = slots[psc[0] % NPS]
        return s_.view(s_.t[:, 0:128])
    def getpbig():
        psc[0] += 1
        return slots[psc[0] % NPS]
    def pool_of(name, n, shape=(128, 128)):
        bufs = [S.sbuf(f"{name}{i}", list(shape)) for i in range(n)]
        c = [0]
        def get():
            c[0] += 1
            return bufs[c[0] % n]
        return get
    NPIPE = 3
    g_N = pool_of("cN", NPIPE); g_Q = pool_of("cQ", NPIPE); g_Aak = pool_of("cAak", NPIPE)
    g_nArb = pool_of("cnArb", NPIPE); g_Ark = pool_of("cArk", NPIPE)
    g_nB = pool_of("cnB", NPIPE); g_Kb = pool_of("cKb", NPIPE); g_Vb = pool_of("cVb", NPIPE)
    g_X = pool_of("cX", 4); g_XT = pool_of("cXT", 4); g_WT = pool_of("cWT", 3); g_WTf = pool_of("cWTf", NPIPE + 1)
    g_rhs = pool_of("crhs", 2); g_U = pool_of("cU", 2)
    Mst = [S.sbuf(f"Mst{i}", [128, 128]) for i in range(2)]
    Mg = S.sbuf("Mg", [128, 128])
    S.op("pool", lambda h: h.memset(Mst[0][:], 0.0), writes=[Mst[0]])
    evc = [0]
    def evac_copy(dst, src, scale=None):
        evc[0] += 1
        if evc[0] % 2 == 0:
            if scale is None:
                S.op("act", lambda h: h.copy(out=dst[:], in_=src[:]), reads=[src], writes=[dst])
            else:
                S.op("act", lambda h: h.mul(out=dst[:], in_=src[:], mul=scale), reads=[src], writes=[dst])
        else:
            if scale is None:
                S.op("dve", lambda h: h.tensor_copy(out=dst[:], in_=src[:]), reads=[src], writes=[dst])
            else:
                S.op("dve", lambda h: h.tensor_scalar(out=dst[:], in0=src[:], scalar1=scale, scalar2=None, op0=ALU.mult), reads=[src], writes=[dst])

    def mm(ps, lhsT, rhs, rl, rr, start=True, stop=True, psl=None):
        o = ps[:] if psl is None else psl
        S.op("pe", lambda h: h.matmul(o, lhsT=lhsT, rhs=rhs, start=start, stop=stop), reads=[rl, rr], writes=[ps])

    def pre_steps(ti):
        bi = ti % NB
        t0 = ti * TT
        steps = []
        def loads():
            i_ = ti // NTL; n_ = ti % NTL
            gv = gAf.t.rearrange("a (n c) -> (a n) c", c=TT)
            gh = gAf.t.rearrange("a (c o) -> (a c) o", o=1)
            kinds = [("zr", 0, gAf, gv, gh), ("zk", 1, gAf, gv, gh), ("zv", 2, gAf, gv, gh)]
            if l == 1:
                gv0 = G.gAf[0].t.rearrange("a (n c) -> (a n) c", c=TT)
                gh0 = G.gAf[0].t.rearrange("a (c o) -> (a c) o", o=1)
                kinds.append(("zv0", 2, G.gAf[0], gv0, gh0))
            for nm, kd_, gb, v_, h_ in kinds:
                dst = raw[nm][bi]
                S.idma(dst[:, 1:TT+1], v_, idxB[:, kd_, ti:ti+1], reads=[gb, idxB], writes=[dst])
                if ti > 0:
                    S.idma(dst[:, 0:1], h_, idxBh[:, kd_, ti:ti+1], reads=[gb, idxBh], writes=[dst])
            stat = [("zw", 1152, 32), ("za", 1184, 32), ("zg", 1216, 64)]
            if l == 1:
                stat += [("zva0", 768, 128), ("zva1", 896, 128), ("zva2", 1024, 128)]
            for nm, r0, nr in stat:
                dst = raw[nm][bi]
                base = int(gaddr(CR_F, 1280, i_, r0))
                if n_ > 0:
                    S.dma("sp", dst[:, 0:TT+1], gAf[base:base+nr, n_*TT-1:(n_+1)*TT], reads=[gAf], writes=[dst])
                else:
                    S.dma("sp", dst[:, 1:TT+1], gAf[base:base+nr, 0:TT], reads=[gAf], writes=[dst])
                    if ti > 0:
                        pb = int(gaddr(CR_F, 1280, i_ - 1, r0))
                        S.dma("sp", dst[:, 0:1], gAf[pb:pb+nr, NT-1:NT], reads=[gAf], writes=[dst], allow_slow_non_contiguous=True)
        steps.append(loads)
        def shift(nm, mucol):
            X = raw[nm][bi]; o = sh[nm]; rows = X.t.shape[0]
            tp = gettmp()
            S.op("pool", lambda h: h.tensor_tensor(out=tp[0:rows, :], in0=X[:, 0:TT], in1=X[:, 1:TT+1], op=ALU.subtract), reads=[X], writes=[tp])
            S.op("dve", lambda h: h.scalar_tensor_tensor(out=o[:], in0=tp[0:rows, :], scalar=mucol, in1=X[:, 1:TT+1], op0=ALU.mult, op1=ALU.add), reads=[tp, X, pcol, plo], writes=[o])
        def shifts():
            shift("zr", col(C_MUR)); shift("zk", col(C_MUK)); shift("zv", col(C_MUV))
            shift("zw", plo[0:32, 0:1]); shift("za", plo[0:32, 1:2]); shift("zg", plo[0:64, 2:3])
            if l == 1:
                shift("zva0", col(12)); shift("zva1", col(13)); shift("zva2", col(14)); shift("zv0", col(C_MU0V))
        steps.append(shifts)
        def loras():
            S.op("act", lambda h: h.activation(out=th[:], in_=sh["zw"][:], func=AF.Tanh), reads=[sh["zw"]], writes=[th])
            for j in range(TT // 128):
                pt = getps()
                mm(pt, th[:, j*128:(j+1)*128], wup[:], th, wup, start=True, stop=False)
                mm(pt, onesrow[:], w0row[:], onesrow, w0row, start=False, stop=True)
                st = sgtok[j % 2]
                S.op("act", lambda h: h.activation(out=st[:], in_=pt[:], func=AF.Sigmoid), reads=[pt], writes=[st])
                mm(pL, st[:], TRI, st, cst, psl=pL[:, j*128:(j+1)*128])
                mm(pLx, st[:], TRIS, st, cst, psl=pLx[:, j*128:(j+1)*128])
            S.op("act", lambda h: h.activation(out=eL[:], in_=pL[:], func=AF.Exp, scale=LWS), reads=[pL], writes=[eL])
            S.op("act", lambda h: h.activation(out=enL[:], in_=pL[:], func=AF.Exp, scale=-LWS), reads=[pL], writes=[enL])
            S.op("act", lambda h: h.activation(out=eLx[:], in_=pLx[:], func=AF.Exp, scale=LWS), reads=[pLx], writes=[eLx])
            gm = gam[bi]
            S.op("dve", lambda h: h.tensor_copy(out=gm[:], in_=eL[:, T-1::T]), reads=[eL], writes=[gm])
            p = getpbig()
            mm(p, aup[:], sh["za"][:], aup, sh["za"])
            S.op("act", lambda h: h.activation(out=a_t[:], in_=p[:], func=AF.Sigmoid, bias=col(C_A0)), reads=[p, pcol], writes=[a_t])
            S.op("act", lambda h: h.activation(out=sgd[:], in_=sh["zg"][:], func=AF.Sigmoid), reads=[sh["zg"]], writes=[sgd])
            p2 = getpbig()
            mm(p2, gup[:], sgd[:], gup, sgd)
            S.op("act", lambda h: h.copy(out=g_t[bi][:], in_=p2[:]), reads=[p2], writes=[g_t[bi]])
        steps.append(loras)
        def vres():
            vt = v_t[bi]
            if l == 0:
                S.op("pool", lambda h: h.tensor_copy(out=vt[:], in_=sh["zv"][:]), reads=[sh["zv"]], writes=[vt])
                return
            p = getps()
            for c in range(3):
                mm(p, vdown[:, c, :], sh[f"zva{c}"][:], vdown, sh[f"zva{c}"], start=(c == 0), stop=(c == 2), psl=None) if False else \
                    S.op("pe", lambda h: h.matmul(pbig_v[0:16, :], lhsT=vdown[:, c, :], rhs=sh[f"zva{c}"][:], start=(c == 0), stop=(c == 2)), reads=[vdown, sh[f"zva{c}"]], writes=[pbig_vb])
            S.op("act", lambda h: h.copy(out=vd_sb[:], in_=pbig_v[0:16, :]), reads=[pbig_vb], writes=[vd_sb])
            p3 = getpbig()
            mm(p3, vup[:], vd_sb[:], vup, vd_sb)
            S.op("act", lambda h: h.activation(out=sv[:], in_=p3[:], func=AF.Sigmoid, bias=col(C_V0)), reads=[p3, pcol], writes=[sv])
            tp = gettmp()
            S.op("pool", lambda h: h.tensor_tensor(out=tp[:], in0=sh["zv0"][:], in1=sh["zv"][:], op=ALU.subtract), reads=[sh["zv0"], sh["zv"]], writes=[tp])
            S.op("dve", lambda h: h.tensor_tensor(out=tp[:], in0=tp[:], in1=sv[:], op=ALU.mult), reads=[tp, sv], writes=[tp])
            S.op("pool", lambda h: h.tensor_tensor(out=vt[:], in0=tp[:], in1=sh["zv"][:], op=ALU.add), reads=[tp, sh["zv"]], writes=[vt])
        if l == 1:
            pbig_vb = getpbig(); pbig_v = pbig_vb.t
        steps.append(vres)
        def kstuff():
            S.op("dve", lambda h: h.tensor_scalar(out=kk[:], in0=sh["zk"][:], scalar1=col(C_KK), scalar2=None, op0=ALU.mult), reads=[sh["zk"], pcol], writes=[kk])
            S.op("pool", lambda h: h.tensor_tensor(out=sq[:], in0=kk[:], in1=kk[:], op=ALU.mult), reads=[kk], writes=[sq])
            p = getpbig()
            mm(p, ONESB, sq[:], cst, sq)
            S.op("act", lambda h: h.activation(out=nrm[:], in_=p[:], func=AF.Sqrt), reads=[p], writes=[nrm])
            S.op("dve", lambda h: h.tensor_scalar(out=nrm[:], in0=nrm[:], scalar1=1e-12, scalar2=None, op0=ALU.max), reads=[nrm], writes=[nrm])
            S.op("dve", lambda h: h.reciprocal(out=nrm[:], in_=nrm[:]), reads=[nrm], writes=[nrm])
            S.op("dve", lambda h: h.tensor_tensor(out=kap[:], in0=kk[:], in1=nrm[:], op=ALU.mult), reads=[kk, nrm], writes=[kap])
            tp = gettmp()
            S.op("dve", lambda h: h.tensor_scalar(out=tp[:], in0=a_t[:], scalar1=col(C_KA), scalar2=omk[:, 0:1], op0=ALU.mult, op1=ALU.add), reads=[a_t, pcol, omk], writes=[tp])
            S.op("pool", lambda h: h.tensor_tensor(out=kmod[:], in0=sh["zk"][:], in1=tp[:], op=ALU.mult), reads=[sh["zk"], tp], writes=[kmod])
            S.op("pool", lambda h: h.tensor_tensor(out=b_t[:], in0=kap[:], in1=a_t[:], op=ALU.mult), reads=[kap, a_t], writes=[b_t])
            tp2 = gettmp()
            S.op("dve", lambda h: h.scalar_tensor_tensor(out=tp2[:], in0=sh["zr"][:], scalar=col(C_RK), in1=kmod[:], op0=ALU.mult, op1=ALU.mult), reads=[sh["zr"], pcol, kmod], writes=[tp2])
            p2 = getpbig()
            mm(p2, ONESB, tp2[:], cst, tp2)
            S.op("dve", lambda h: h.tensor_tensor(out=bv[bi][:], in0=p2[:], in1=v_t[bi][:], op=ALU.mult), reads=[p2, v_t[bi]], writes=[bv[bi]])
        steps.append(kstuff)
        def expand():
            for nm, src, ee in [("RT", sh["zr"], eL), ("KpT", kap, eLx), ("KT", kmod, enL), ("BT", b_t, enL), ("VT", v_t[bi], None)]:
                dst = bd[nm][bi]
                for hh in range(2):
                    ps_ = slice(hh*64, (hh+1)*64)
                    o = dst[ps_, :, hh*64:(hh+1)*64]
                    i0 = src[ps_, :].rearrange("p (c t) -> p c t", t=T)
                    eng = "dve" if hh == 0 else "pool"
                    if ee is None:
                        S.op(eng, lambda h: h.tensor_copy(out=o, in_=i0), reads=[src], writes=[dst])
                    else:
                        i1 = ee[ps_, :].rearrange("p (c t) -> p c t", t=T)
                        S.op(eng, lambda h: h.tensor_tensor(out=o, in0=i0, in1=i1, op=ALU.mult), reads=[src, ee], writes=[dst])
        steps.append(expand)
        return steps

    def chunk_par(ti, c):
        bi = ti % NB
        RT = bd["RT"][bi]; KpT = bd["KpT"][bi]; KT = bd["KT"][bi]; BT = bd["BT"][bi]; VT = bd["VT"][bi]
        rt = RT[:, c, :]; kpt = KpT[:, c, :]; kt = KT[:, c, :]; bt = BT[:, c, :]; vt = VT[:, c, :]
        st = {}
        steps = []
        def amats():
            N = g_N(); Q = g_Q(); Aak = g_Aak(); nArb = g_nArb(); Ark = g_Ark()
            for dst, l, ll, r, rr, mask in [(N, bt, BT, kpt, KpT, MSU), (Q, kpt, KpT, bt, BT, MSL), (Aak, kt, KT, kpt, KpT, MSU),
                                            (nArb, bt, BT, rt, RT, MNUI), (Ark, kt, KT, rt, RT, MUI)]:
                p = getps()
                mm(p, l, r, ll, rr)
                S.op("dve", lambda h: h.tensor_tensor(out=dst[:], in0=p[:], in1=mask, op=ALU.mult), reads=[p, cst], writes=[dst])
            st.update(N=N, Q=Q, Aak=Aak, nArb=nArb, Ark=Ark)
        steps.append(amats)
        def transposes():
            nB = g_nB(); Kb = g_Kb(); Vb = g_Vb()
            for dst, src, sb, scale in [(nB, bt, BT, -1.0), (Kb, kt, KT, None), (Vb, vt, VT, None)]:
                p = getps()
                S.op("pe", lambda h: h.transpose(p[:], src, IDENT), reads=[sb, cst], writes=[p])
                evac_copy(dst, p, scale)
            st.update(nB=nB, Kb=Kb, Vb=Vb)
        steps.append(transposes)
        def inv0():
            WT = g_WT()
            S.op("pool", lambda h: h.tensor_tensor(out=WT[:], in0=IDENT, in1=st["N"][:], op=ALU.subtract), reads=[cst, st["N"]], writes=[WT])
            st.update(WT=WT, X=st["N"], XT=st["Q"])
        steps.append(inv0)
        def invj(j):
            def f():
                X = st["X"]; XT = st["XT"]; WT = st["WT"]
                last = (j == 4)
                XTn = g_XT()
                p2 = getps(); mm(p2, X[:], XT[:], X, XT)
                if not last:
                    Xn = g_X()
                    p1 = getps(); mm(p1, XT[:], X[:], XT, X)
                    S.op("act", lambda h: h.copy(out=Xn[:], in_=p1[:]), reads=[p1], writes=[Xn])
                S.op("dve", lambda h: h.tensor_copy(out=XTn[:], in_=p2[:]), reads=[p2], writes=[XTn])
                p3 = getps(); mm(p3, XTn[:], WT[:], XTn, WT)
                WTn = g_WTf() if last else g_WT()
                S.op("dve", lambda h: h.tensor_tensor(out=WTn[:], in0=p3[:], in1=WT[:], op=ALU.add), reads=[p3, WT], writes=[WTn])
                st["XT"] = XTn; st["WT"] = WTn
                if not last:
                    st["X"] = Xn
            return f
        for j in range(5):
            steps.append(invj(j))
        return steps, st

    mcur = [0]
    def chunk_seq(ti, c, st):
        bi = ti % NB
        RT = bd["RT"][bi]; KpT = bd["KpT"][bi]
        rt = RT[:, c, :]; kpt = KpT[:, c, :]
        steps = []
        def s1():
            M0 = Mst[mcur[0] % 2]
            p = getps()
            mm(p, kpt, M0[:], KpT, M0, start=True, stop=False)
            mm(p, st["Aak"][:], st["Vb"][:], st["Aak"], st["Vb"], start=False, stop=True)
            rhs = g_rhs()
            S.op("act", lambda h: h.copy(out=rhs[:], in_=p[:]), reads=[p], writes=[rhs])
            st["rhs"] = rhs
            S.op("pool", lambda h: h.tensor_scalar(out=Mg[:], in0=M0[:], scalar1=gam[bi][:, c:c+1], scalar2=None, op0=ALU.mult), reads=[M0, gam[bi]], writes=[Mg])
        def s2():
            p = getps()
            mm(p, st["WT"][:], st["rhs"][:], st["WT"], st["rhs"])
            U = g_U()
            S.op("dve", lambda h: h.tensor_copy(out=U[:], in_=p[:]), reads=[p], writes=[U])
            st["U"] = U
        def s3():
            M0 = Mst[mcur[0] % 2]; M1 = Mst[(mcur[0] + 1) % 2]
            U = st["U"]
            pm = getps()
            mm(pm, st["Kb"][:], st["Vb"][:], st["Kb"], st["Vb"], start=True, stop=False)
            mm(pm, st["nB"][:], U[:], st["nB"], U, start=False, stop=True)
            S.op("dve", lambda h: h.scalar_tensor_tensor(out=M1[:], in0=pm[:], scalar=gam[bi][:, c:c+1], in1=Mg[:], op0=ALU.mult, op1=ALU.add), reads=[pm, gam[bi], Mg], writes=[M1])
            py = getps()
            mm(py, M0[:], rt, M0, RT, start=True, stop=False)
            mm(py, U[:], st["nArb"][:], U, st["nArb"], start=False, stop=False)
            mm(py, st["Vb"][:], st["Ark"][:], st["Vb"], st["Ark"], start=False, stop=True)
            y = yT[bi]
            S.op("act", lambda h: h.copy(out=y[0:64, c*T:(c+1)*T], in_=py[0:64, 0:64]), reads=[py], writes=[y])
            S.op("act", lambda h: h.copy(out=y[64:128, c*T:(c+1)*T], in_=py[64:128, 64:128]), reads=[py], writes=[y])
            mcur[0] += 1
        return [s1, s2, s3]

    def post_steps(ti):
        bi = ti % NB
        t0 = ti * TT
        def f():
            y = yT[bi]
            p = getpbig()
            mm(p, MEANB, y[:], cst, y)
            yc = gettmp()
            S.op("dve", lambda h: h.tensor_tensor(out=yc[:], in0=y[:], in1=p[:], op=ALU.subtract), reads=[y, p], writes=[yc])
            s2 = gettmp()
            S.op("pool", lambda h: h.tensor_tensor(out=s2[:], in0=yc[:], in1=yc[:], op=ALU.mult), reads=[yc], writes=[s2])
            p2 = getpbig()
            mm(p2, MEANB, s2[:], cst, s2)
            S.op("act", lambda h: h.activation(out=s2[:], in_=p2[:], func=AF.Sqrt, bias=epsc[:, 0:1]), reads=[p2, epsc], writes=[s2])
            S.op("dve", lambda h: h.reciprocal(out=s2[:], in_=s2[:]), reads=[s2], writes=[s2])
            S.op("dve", lambda h: h.tensor_tensor(out=yc[:], in0=yc[:], in1=s2[:], op=ALU.mult), reads=[yc, s2], writes=[yc])
            S.op("dve", lambda h: h.tensor_scalar(out=yc[:], in0=yc[:], scalar1=col(C_LNG), scalar2=col(C_LNB), op0=ALU.mult, op1=ALU.add), reads=[yc, pcol], writes=[yc])
            S.op("pool", lambda h: h.tensor_tensor(out=yc[:], in0=yc[:], in1=bv[bi][:], op=ALU.add), reads=[yc, bv[bi]], writes=[yc])
            o = yo[bi]
            S.op("dve", lambda h: h.tensor_tensor(out=o[:], in0=yc[:], in1=g_t[bi][:], op=ALU.mult), reads=[yc, g_t[bi]], writes=[o])
            S.dma("sp", sP[(ti // NTL)*128:(ti // NTL + 1)*128, (ti % NTL)*TT:(ti % NTL + 1)*TT], o[:], reads=[o], writes=[sP])
        return [f]
    epsc = S.sbuf("epsc", [128, 1])
    S.op("pool", lambda h: h.memset(epsc[:], GN_EPS), writes=[epsc])

    for s in pre_steps(0):
        s()
    LAG = 2
    pend = []
    nextpre = []
    for ti in range(NTILE):
        if ti + 1 < NTILE:
            nextpre = pre_steps(ti + 1)
        else:
            nextpre = []
        for c in range(NCH):
            psteps, st = chunk_par(ti, c)
            seqs = []
            if len(pend) >= LAG:
                pti, pc, pst = pend.pop(0)
                seqs = chunk_seq(pti, pc, pst)
                if pc == NCH - 1:
                    seqs = seqs + post_steps(pti)
            n = max(len(psteps), len(seqs))
            qi = 0
            for i in range(len(psteps)):
                psteps[i]()
                while qi < len(seqs) and (qi + 1) * len(psteps) <= (i + 1) * max(1, len(seqs)):
                    seqs[qi](); qi += 1
            while qi < len(seqs):
                seqs[qi](); qi += 1
            pend.append((ti, c, st))
            if nextpre and c < len(nextpre):
                nextpre[c]()
        for s in nextpre[NCH:]:
            s()
    while pend:
        pti, pc, pst = pend.pop(0)
        for s in chunk_seq(pti, pc, pst):
            s()
        if pc == NCH - 1:
            for s in post_steps(pti):
                s()

    S.phase_end()


def phase_C(nc, G, l):
    NT = G.NT; SEQ = 4 * NT; NTL = NT // TT
    NQ = SEQ // QT; NKL = SEQ // 4
    S = Sched(nc)
    for b_ in G.persist:
        b_.reset()
    gAb = G.gAb[l]; gAv = G.gAv[l]; sP = G.sP[l]
    idxCk = S.sbuf("idxCk", [128, 6, 4], I32); idxCv = S.sbuf("idxCv", [128, NKL // 128], I32)
    S.dma("sp", idxCk[:], G.inp["idxCk"].t, reads=[G.inp["idxCk"]], writes=[idxCk])
    S.dma("sp", idxCv[:], G.inp["idxCv"].t, reads=[G.inp["idxCv"]], writes=[idxCv])
    kT = [S.sbuf(f"kT{h}", [96, NKL], BF16) for h in range(6)]
    gkv = gAb.t.rearrange("a (j x) -> (a j) x", j=4)
    for h in range(6):
        for i_ in range(4):
            S.idma(kT[h][:, i_*(NT//4):(i_+1)*(NT//4)], gkv, idxCk[0:96, h, i_:i_+1], reads=[gAb, idxCk], writes=[kT[h]])
    NKT = NKL // 128
    vaug = S.sbuf("vaug", [128, NKT, 6, 65], BF16)
    S.op("pool", lambda hh: hh.memset(vaug[:], 1.0), writes=[vaug])
    vst = S.sbuf("vst", [128, NKT, 384], BF16)
    for n_ in range(NKT):
        S.idma(vst[:, n_, :], gAv.t, idxCv[:, n_:n_+1], reads=[gAv, idxCv], writes=[vst])
    for n in range(NKT):
        S.op("pool", lambda hh: hh.tensor_copy(out=vaug[:, n, :, 0:64], in_=vst[:, n, :].rearrange("p (h c) -> p h c", c=64)), reads=[vst], writes=[vaug])
    mask = S.sbuf("mask", [128, QT], BF16)
    S.dma("sp", mask[:], G.inp["maskC"][:, :], reads=[G.inp["maskC"]], writes=[mask])
    qb = [[S.sbuf(f"q{i}_{h}", [96, QT], BF16) for h in range(6)] for i in range(2)]
    ps = [S.psum(f"ps{i}", [128, QT]) for i in range(4)]
    po = [S.psum(f"po{i}", [128, QT]) for i in range(2)]
    pT = [S.sbuf(f"pT{i}", [128, QT], BF16) for i in range(4)]
    oo = [S.sbuf(f"oo{i}", [65, QT], F32) for i in range(3)]
    c = 0; oc = 0
    for g in range(NQ):
        qs = qb[g % 2]
        for h in range(6):
            S.dma("sp", qs[h][:], gAb[int(gaddr(CR_B, RAB, g // NTL, h*96)):int(gaddr(CR_B, RAB, g // NTL, h*96)) + 96, (g % NTL)*QT:(g % NTL + 1)*QT], reads=[gAb], writes=[qs[h]])
        for h in range(6):
            acc = po[(g * 6 + h) % 2]
            for i in range(g + 1):
                p = ps[c % 4]; pt = pT[c % 4]; c += 1
                S.op("pe", lambda hh: hh.matmul(p[:], lhsT=kT[h][:, i*128:(i+1)*128], rhs=qs[h][:], start=True, stop=True), reads=[kT[h], qs[h]], writes=[p])
                S.op("act", lambda hh: hh.activation(out=pt[:], in_=p[:], func=AF.Exp, scale=SCALE), reads=[p], writes=[pt])
                if i == g:
                    S.op("dve", lambda hh: hh.tensor_tensor(out=pt[:], in0=pt[:], in1=mask[:], op=ALU.mult), reads=[pt, mask], writes=[pt])
                S.op("pe", lambda hh: hh.matmul(acc[0:65, :], lhsT=vaug[:, i, h, :], rhs=pt[:], start=(i == 0), stop=(i == g)), reads=[vaug, pt], writes=[acc])
            o = oo[oc % 3]; oc += 1
            S.op("dve", lambda hh: hh.tensor_copy(out=o[:], in_=acc[0:65, :]), reads=[acc], writes=[o])
            S.dma("sp", sP[512 + (g // NTL)*390 + h*65:512 + (g // NTL)*390 + (h+1)*65, (g % NTL)*QT:(g % NTL + 1)*QT], o[:], reads=[o], writes=[sP])

    allgather_rows(S, sP, G.gP[l], RP, CR_F)
    S.phase_end()


def phase_D(nc, G, l):
    NT = G.NT; NTL = NT // TT
    moe = (l % 2 == 1)
    G_ = 3 if moe else 4
    F = 3584 if moe else 2816
    NE = 8 if moe else 1
    NF = F // 128
    S = Sched(nc)
    for b_ in G.persist:
        b_.reset()
    gP = G.gP[l]; ycl = G.ycl[l]
    gPv = gP.t.rearrange("a (n c) -> (a n) c", c=TT)
    wo_d = G.inp["w_out%d" % l]; pc_d = G.inp["pcolD%d" % l]; cst_d = G.inp["cstD"]
    wg_d = G.inp["wg%d" % l]; wu_d = G.inp["wu%d" % l]; wd_d = G.inp["wd%d" % l]
    if moe:
        rt_d = G.inp["router"]
    idxDya = S.sbuf("idxDya", [128, 3, NTL], I32); idxDo = S.sbuf("idxDo", [128, 24, NTL], I32)
    S.dma("sp", idxDya[:], G.inp["idxDya"].t, reads=[G.inp["idxDya"]], writes=[idxDya])
    S.dma("sp", idxDo[:], G.inp["idxDo"].t, reads=[G.inp["idxDo"]], writes=[idxDo])
    pc = S.sbuf("pc", [128, 16]); cst = S.sbuf("cst", [128, 2, 128])
    S.dma("sp", pc[:], pc_d[:], reads=[pc_d], writes=[pc])
    S.dma("sp", cst[:], cst_d[:], reads=[cst_d], writes=[cst])
    ONES = cst[:, 0, :]; IDENT = cst[:, 1, :]
    onesb = S.sbuf("onesb", [128, 128], BF16)
    S.op("dve", lambda h: h.tensor_copy(out=onesb[:], in_=ONES), reads=[cst], writes=[onesb])
    epsc = S.sbuf("epsc", [128, 1]); S.op("pool", lambda h: h.memset(epsc[:], EPS), writes=[epsc])
    NPS = 8
    slots = [S.psum(f"slot{i}", [128, TT]) for i in range(NPS)]
    psc = [0]
    def getps():
        psc[0] += 1
        return slots[psc[0] % NPS]
    xT = [S.sbuf(f"xT{k}", [128, NT], F32) for k in range(8)]
    for k in range(8):
        S.dma("sp", xT[k][:], G.xsrc[l][k*128:(k+1)*128, :], reads=[G.xsrc[l]], writes=[xT[k]])
    hT = [S.sbuf(f"hT{k}", [128, NT], BF16) for k in range(8)]
    stg = [S.sbuf(f"stg{i}", [128, 1024], F32) for i in range(2)]
    stc = [0]
    def getstg():
        stc[0] += 1
        return stg[stc[0] % 2]
    wgb = [S.sbuf(f"wgb{i}", [128, 8, G_*128], BF16) for i in range(2)]
    wub = [S.sbuf(f"wub{i}", [128, 8, G_*128], BF16) for i in range(2)]
    wdb = [S.sbuf(f"wdb{i}", [128, G_, D], BF16) for i in range(2)]
    _wob = [wgb[0], wgb[1], wub[0], wub[1]]
    def wo_view(i):
        b = _wob[i // G_]
        return b, b.t[:].rearrange("p k f -> p (k f)")[:, (i % G_) * D:(i % G_ + 1) * D]
    krows = [(i*128, 128) for i in range(3)] + [(384 + i*64, 64) for i in range(6)] + [(768 + i*128, 128) for i in range(2)]
    for i, (r0, n) in enumerate(krows):
        st = getstg()
        S.dma("sp", st[0:n, :], wo_d[r0:r0+n, :], reads=[wo_d], writes=[st])
        wob, wov = wo_view(i)
        S.op("act", lambda h: h.copy(out=wov[0:n, :], in_=st[0:n, :]), reads=[st], writes=[wob])
    mixb = [S.sbuf(f"mixb{i}", [128, TT], BF16) for i in range(11)]
    oh = [S.sbuf(f"oh{i}", [65, TT], F32) for i in range(6)]
    sqb = [S.sbuf(f"sqb{i}", [128, TT], BF16) for i in range(2)]
    rs = S.sbuf("rs", [128, TT], F32)
    ldt = [S.sbuf(f"ldt{i}", [128, TT], F32) for i in range(3)]
    ldc = [0]
    def getld():
        ldc[0] += 1
        return ldt[ldc[0] % 3]
    if moe:
        rtr = S.sbuf("rtr", [128, 8, 8], F32)
        S.dma("sp", rtr[:], rt_d.t.rearrange("(k p) e -> p k e", p=128), reads=[rt_d], writes=[rtr])
        hf = [S.sbuf(f"hf{k}", [128, TT], F32) for k in range(2)]
        gbc2 = [S.sbuf(f"gbc{e}", [128, NT], BF16) for e in range(2)]
        gfall = S.sbuf("gfall", [128, NT // 128, 8], F32)
        lg = S.sbuf("lg", [128, 8], F32); m1 = S.sbuf("m1", [128, 1], F32); m2 = S.sbuf("m2", [128, 1], F32)
        eq1 = S.sbuf("eq1", [128, 8], F32); eq2 = S.sbuf("eq2", [128, 8], F32); msk = S.sbuf("msk", [128, 8], F32)
        g1 = S.sbuf("g1", [128, 1], F32); g2 = S.sbuf("g2", [128, 1], F32); gf = S.sbuf("gf", [128, 8], F32)
        gexp = S.sbuf("gexp", [128, 128], F32)

    for t in range(NTL):
        sl = slice(t*TT, (t+1)*TT)
        for i in range(3):
            st = getld()
            S.idma(st[:], gPv, idxDya[:, i, t:t+1], reads=[gP, idxDya], writes=[st])
            S.op("act", lambda h: h.copy(out=mixb[i][:], in_=st[:]), reads=[st], writes=[mixb[i]])
        for i in range(2):
            st = getld()
            S.dma("sp", st[:], ycl[i*128:(i+1)*128, sl], reads=[ycl], writes=[st])
            S.op("act", lambda h: h.copy(out=mixb[9+i][:], in_=st[:]), reads=[st], writes=[mixb[9+i]])
        pss = getps()
        for hd in range(6):
            o = oh[hd]
            S.idma(o[:], gPv, idxDo[0:65, hd, t:t+1], reads=[gP, idxDo], writes=[o])
            for j in range(1, 4):
                st = getld()
                S.idma(st[0:65, :], gPv, idxDo[0:65, j*6 + hd, t:t+1], reads=[gP, idxDo], writes=[st])
                S.op("pool", lambda h: h.tensor_tensor(out=o[:], in0=o[:], in1=st[0:65, :], op=ALU.add), reads=[o, st], writes=[o])
            S.op("dve", lambda h: h.reciprocal(out=o[64:65, :], in_=o[64:65, :]), reads=[o], writes=[o])
            pb = getps()
            S.op("pe", lambda h: h.matmul(pb[0:64, :], lhsT=ONES[64:65, 0:64], rhs=o[64:65, :], start=True, stop=True), reads=[cst, o], writes=[pb])
            S.op("dve", lambda h: h.tensor_tensor(out=o[0:64, :], in0=o[0:64, :], in1=pb[0:64, :], op=ALU.mult), reads=[o, pb], writes=[o])
            s = sqb[hd % 2]
            S.op("act", lambda h: h.activation(out=s[0:64, :], in_=o[0:64, :], func=AF.Square), reads=[o], writes=[s])
            S.op("pe", lambda h: h.matmul(pss[:], lhsT=onesb[0:64, :], rhs=s[0:64, :], start=(hd == 0), stop=(hd == 5)), reads=[onesb, s], writes=[pss])
        S.op("act", lambda h: h.activation(out=rs[:], in_=pss[:], func=AF.Sqrt, scale=1.0/384, bias=epsc[:]), reads=[pss, epsc], writes=[rs])
        S.op("dve", lambda h: h.reciprocal(out=rs[:], in_=rs[:]), reads=[rs], writes=[rs])
        for hd in range(6):
            S.op("dve", lambda h: h.scalar_tensor_tensor(out=mixb[3+hd][0:64, :], in0=oh[hd][0:64, :], scalar=pc[0:64, 8+hd:9+hd], in1=rs[0:64, :], op0=ALU.mult, op1=ALU.mult), reads=[oh[hd], pc, rs], writes=[mixb[3+hd]])
        for m in range(8):
            p = getps()
            for i, (r0, n) in enumerate(krows):
                wob, wov = wo_view(i)
                S.op("pe", lambda h: h.matmul(p[:], lhsT=wov[0:n, m*128:(m+1)*128], rhs=mixb[i][0:n, :], start=(i == 0), stop=(i == 10)), reads=[wob, mixb[i]], writes=[p])
            S.op("dve", lambda h: h.tensor_tensor(out=xT[m][:, sl], in0=xT[m][:, sl], in1=p[:], op=ALU.add), reads=[xT[m], p], writes=[xT[m]])
        p = getps()
        for k in range(8):
            s = sqb[k % 2]
            S.op("act", lambda h: h.activation(out=s[:], in_=xT[k][:, sl], func=AF.Square), reads=[xT[k]], writes=[s])
            S.op("pe", lambda h: h.matmul(p[:], lhsT=onesb[:], rhs=s[:], start=(k == 0), stop=(k == 7)), reads=[onesb, s], writes=[p])
        S.op("act", lambda h: h.activation(out=rs[:], in_=p[:], func=AF.Sqrt, scale=1.0/D, bias=epsc[:]), reads=[p, epsc], writes=[rs])
        S.op("dve", lambda h: h.reciprocal(out=rs[:], in_=rs[:]), reads=[rs], writes=[rs])
        for k in range(8):
            S.op("dve", lambda h: h.scalar_tensor_tensor(out=hT[k][:, sl], in0=xT[k][:, sl], scalar=pc[:, k:k+1], in1=rs[:], op0=ALU.mult, op1=ALU.mult), reads=[xT[k], pc, rs], writes=[hT[k]])
        if moe:
            pls = [getps() for j in range(TT // 128)]
            for k in range(8):
                hk = hf[k % 2]
                S.op("dve", lambda h: h.scalar_tensor_tensor(out=hk[:], in0=xT[k][:, sl], scalar=pc[:, k:k+1], in1=rs[:], op0=ALU.mult, op1=ALU.mult), reads=[xT[k], pc, rs], writes=[hk])
                for j in range(TT // 128):
                    S.op("pe", lambda h: h.matmul(pls[j][:, 0:8], lhsT=hk[:, j*128:(j+1)*128], rhs=rtr[:, k, :], start=(k == 0), stop=(k == 7)), reads=[hk, rtr], writes=[pls[j]])
            for j in range(TT // 128):
                pl = pls[j]
                S.op("dve", lambda h: h.tensor_copy(out=lg[:], in_=pl[:, 0:8]), reads=[pl], writes=[lg])
                S.op("dve", lambda h: h.reduce_max(out=m1[:], in_=lg[:], axis=AX.X), reads=[lg], writes=[m1])
                S.op("dve", lambda h: h.tensor_scalar(out=eq1[:], in0=lg[:], scalar1=m1[:, 0:1], scalar2=None, op0=ALU.is_equal), reads=[lg, m1], writes=[eq1])
                S.op("dve", lambda h: h.scalar_tensor_tensor(out=msk[:], in0=eq1[:], scalar=-1e30, in1=lg[:], op0=ALU.mult, op1=ALU.add), reads=[eq1, lg], writes=[msk])
                S.op("dve", lambda h: h.reduce_max(out=m2[:], in_=msk[:], axis=AX.X), reads=[msk], writes=[m2])
                S.op("dve", lambda h: h.tensor_scalar(out=eq2[:], in0=msk[:], scalar1=m2[:, 0:1], scalar2=None, op0=ALU.is_equal), reads=[msk, m2], writes=[eq2])
                S.op("dve", lambda h: h.tensor_tensor(out=g1[:], in0=m1[:], in1=m2[:], op=ALU.subtract), reads=[m1, m2], writes=[g1])
                S.op("act", lambda h: h.activation(out=g1[:], in_=g1[:], func=AF.Sigmoid), reads=[g1], writes=[g1])
                S.op("dve", lambda h: h.tensor_scalar(out=g2[:], in0=g1[:], scalar1=-1.0, scalar2=1.0, op0=ALU.mult, op1=ALU.add), reads=[g1], writes=[g2])
                S.op("dve", lambda h: h.tensor_scalar(out=gf[:], in0=eq1[:], scalar1=g1[:, 0:1], scalar2=None, op0=ALU.mult), reads=[eq1, g1], writes=[gf])
                S.op("dve", lambda h: h.scalar_tensor_tensor(out=gf[:], in0=eq2[:], scalar=g2[:, 0:1], in1=gf[:], op0=ALU.mult, op1=ALU.add), reads=[eq2, g2, gf], writes=[gf])
                S.op("dve", lambda h: h.tensor_copy(out=gfall[:, t * (TT // 128) + j, :], in_=gf[:]), reads=[gf], writes=[gfall])

    act = [S.sbuf(f"act{i}", [128, NT], BF16) for i in range(G_)]
    sg = [S.sbuf(f"sg{i}", [128, TT], BF16) for i in range(3)]
    sgc = 0; gi = 0
    groups = []
    f0 = 0
    while f0 < NF:
        n = min(G_, NF - f0); groups.append((f0, n)); f0 += n
    for e in range(NE):
        if moe:
            gbc_e = gbc2[e % 2]
            for jj in range(NT // 128):
                S.op("dve", lambda h: h.tensor_scalar(out=gexp[:], in0=ONES, scalar1=gfall[:, jj, e:e+1], scalar2=None, op0=ALU.mult), reads=[cst, gfall], writes=[gexp])
                pg = getps()
                S.op("pe", lambda h: h.matmul(pg[:, 0:128], lhsT=gexp[:], rhs=IDENT, start=True, stop=True), reads=[gexp, cst], writes=[pg])
                S.op("act", lambda h: h.copy(out=gbc_e[:, jj*128:(jj+1)*128], in_=pg[:, 0:128]), reads=[pg], writes=[gbc_e])
        for (f0, n) in groups:
            bi = gi % 2; gi += 1
            wg_, wu_, wd_ = wgb[bi], wub[bi], wdb[bi]
            for k in range(8):
                for (src, dst) in [(wg_d, wg_), (wu_d, wu_)]:
                    st = getstg()
                    S.dma("sp", st[:, 0:n*128], src.t[e, k*128:(k+1)*128, f0*128:(f0+n)*128], reads=[src], writes=[st])
                    S.op("act", lambda h: h.copy(out=dst[:, k, 0:n*128], in_=st[:, 0:n*128]), reads=[st], writes=[dst])
            for i in range(n):
                st = getstg()
                S.dma("sp", st[:, :], wd_d.t[e, (f0+i)*128:(f0+i+1)*128, :], reads=[wd_d], writes=[st])
                S.op("act", lambda h: h.copy(out=wd_[:, i, :], in_=st[:, :]), reads=[st], writes=[wd_])
            for i in range(n):
                for t in range(NTL):
                    sl = slice(t*TT, (t+1)*TT)
                    pg = getps(); pu = getps()
                    for k in range(8):
                        S.op("pe", lambda h: h.matmul(pg[:], lhsT=wg_[:, k, i*128:(i+1)*128], rhs=hT[k][:, sl], start=(k == 0), stop=(k == 7)), reads=[wg_, hT[k]], writes=[pg])
                    for k in range(8):
                        S.op("pe", lambda h: h.matmul(pu[:], lhsT=wu_[:, k, i*128:(i+1)*128], rhs=hT[k][:, sl], start=(k == 0), stop=(k == 7)), reads=[wu_, hT[k]], writes=[pu])
                    s = sg[sgc % 3]; sgc += 1
                    S.op("act", lambda h: h.activation(out=s[:], in_=pg[:], func=AF.Silu), reads=[pg], writes=[s])
                    if moe:
                        S.op("pool", lambda h: h.tensor_tensor(out=s[:], in0=s[:], in1=gbc_e[:, sl], op=ALU.mult), reads=[s, gbc_e], writes=[s])
                    S.op("dve", lambda h: h.tensor_tensor(out=act[i][:, sl], in0=s[:], in1=pu[:], op=ALU.mult), reads=[s, pu], writes=[act[i]])
            for m in range(8):
                for t in range(NTL):
                    sl = slice(t*TT, (t+1)*TT)
                    p = getps()
                    for i in range(n):
                        S.op("pe", lambda h: h.matmul(p[:], lhsT=wd_[:, i, m*128:(m+1)*128], rhs=act[i][:, sl], start=(i == 0), stop=(i == n-1)), reads=[wd_, act[i]], writes=[p])
                    S.op("dve", lambda h: h.tensor_tensor(out=xT[m][:, sl], in0=xT[m][:, sl], in1=p[:], op=ALU.add), reads=[xT[m], p], writes=[xT[m]])
    for k in range(8):
        S.dma("sp", G.xdst[l][k*128:(k+1)*128, :], xT[k][:], reads=[xT[k]], writes=[G.xdst[l]])

    S.phase_end(last=(l == 1))

def build_fused(NT=2048, stop_after=99):
    SEQ = 4 * NT; NTILE = SEQ // TT; NTL = NT // TT; NKT = (SEQ // 4) // 128
    nc = bass.Bass("TRN2", target_bir_lowering=False)
    G = NS(); G.NT = NT
    G.inp = {}
    def ein(name, shape, dt=F32):
        t = nc.dram_tensor(name, list(shape), dt, kind="ExternalInput")
        b = Buf(None, t.ap(), "dram"); G.inp[name] = b
        return b
    G.xT_d = ein("xT", [D, NT]); ein("pos", [1, NT], I32)
    ein("cstA", [128, 6, 128]); ein("cstB", [128, 8, 128]); ein("cstD", [128, 2, 128]); ein("maskC", [128, QT], BF16)
    ein("idxB", [128, 3, NTILE], I32); ein("idxBh", [128, 3, NTILE], I32); ein("idxCk", [128, 6, 4], I32)
    ein("idxCv", [128, NKT], I32); ein("idxDya", [128, 3, NTL], I32); ein("idxDo", [128, 24, NTL], I32)
    for l in range(2):
        ein("w_in%d" % l, [D, INC]); ein("pcolA%d" % l, [128, 32]); ein("wuq%d" % l, [256, 576]); ein("wukv%d" % l, [128, 768])
        ein("ws%d" % l, [4, 128, 128]); ein("bs%d" % l, [1, 512])
        ein("pcolB%d" % l, [128, 16]); ein("plo%d" % l, [64, 4]); ein("wup%d" % l, [32, 128]); ein("aup%d" % l, [32, 128])
        ein("gup%d" % l, [64, 128]); ein("w0row%d" % l, [1, 128]); ein("w_out%d" % l, [D, D]); ein("pcolD%d" % l, [128, 16])
    ein("vdown", [128, 3, 16]); ein("vup", [16, 128])
    ein("wg0", [1, D, 2816]); ein("wu0", [1, D, 2816]); ein("wd0", [1, 2816, D])
    ein("wg1", [8, D, 3584]); ein("wu1", [8, D, 3584]); ein("wd1", [8, 3584, D]); ein("router", [D, 8])
    xo = nc.dram_tensor("xoT", [D, NT], F32, kind="ExternalOutput")
    G.xo_d = Buf(None, xo.ap(), "dram")
    def idram(name, shape, dt=F32):
        t = nc.dram_tensor(name, list(shape), dt, kind="Internal")
        return Buf(None, t.ap(), "dram")
    G.sAf = [idram("sAf%d" % l, [1280, NT]) for l in range(2)]; G.gAf = [idram("gAf%d" % l, [4 * 1280, NT]) for l in range(2)]
    G.sAb = [idram("sAb%d" % l, [RAB, NT], BF16) for l in range(2)]; G.gAb = [idram("gAb%d" % l, [4 * RAB, NT], BF16) for l in range(2)]
    G.sAv = [idram("sAv%d" % l, [NT, 384], BF16) for l in range(2)]; G.gAv = [idram("gAv%d" % l, [4 * NT, 384], BF16) for l in range(2)]
    G.ycl = [idram("ycl%d" % l, [256, NT]) for l in range(2)]
    G.sP = [idram("sP%d" % l, [RP, NT]) for l in range(2)]; G.gP = [idram("gP%d" % l, [4 * RP, NT]) for l in range(2)]
    es = contextlib.ExitStack()
    G.xsp = idram("xsp", [D, NT])
    G.xsrc = [G.xT_d, G.xsp]; G.xdst = [G.xsp, G.xo_d]
    G.persist = list(G.inp.values()) + [G.xo_d, G.xsp] + G.sAf + G.gAf + G.sAb + G.gAb + G.sAv + G.gAv + G.ycl + G.sP + G.gP
    k_ = 0
    for l in range(2):
        for ph in (phase_A, phase_B, phase_C, phase_D):
            if k_ < stop_after:
                ph(nc, G, l)
            k_ += 1
    es.close()
    return nc

def fused_inputs(d, c, NT=2048):
    SEQ = 4 * NT; NTILE = SEQ // TT; NTL = NT // TT; NKT = (SEQ // 4) // 128; MS = NT // 512
    b, r = c // 4, c % 4
    p = r if r < 3 else 0; j = r; q = r
    m = {}
    x = np.asarray(d["x"])[:, :SEQ]
    m["xT"] = np.ascontiguousarray(x[b, q*NT:(q+1)*NT].T)
    m["pos"] = np.ascontiguousarray(np.asarray(d["positions"])[b:b+1, q*NT:(q+1)*NT]).astype(np.int32)
    m["cstA"] = a2_consts(); m["cstB"] = rwkv_consts(); m["cstD"] = d_consts(); m["maskC"] = np.ascontiguousarray(masks_C()[j])
    pp = np.arange(128)
    iB = np.zeros((128, 3, NTILE), np.int32); iBh = np.zeros((128, 3, NTILE), np.int32)
    for ti in range(NTILE):
        i, n = ti // NTL, ti % NTL
        for kind in range(3):
            row = kind * 384 + p * 128 + pp
            iB[:, kind, ti] = gaddr(CR_F, 1280, i, row) * NTL + n
            if ti > 0:
                if n > 0:
                    iBh[:, kind, ti] = gaddr(CR_F, 1280, i, row) * NT + n * TT - 1
                else:
                    iBh[:, kind, ti] = gaddr(CR_F, 1280, i - 1, row) * NT + NT - 1
    m["idxB"] = iB; m["idxBh"] = iBh
    iCk = np.zeros((128, 6, 4), np.int32)
    for h in range(6):
        for i in range(4):
            iCk[:96, h, i] = gaddr(CR_B, RAB, i, 576 + h * 96 + np.arange(96)) * 4 + j
    m["idxCk"] = iCk
    iCv = np.zeros((128, NKT), np.int32)
    for i in range(4):
        for ms in range(MS):
            iCv[:, i * MS + ms] = gaddr(min(CR_V, NT), NT, i, j * (NT // 4) + ms * 128 + pp)
    m["idxCv"] = iCv
    iDy = np.zeros((128, 3, NTL), np.int32); iDo = np.zeros((128, 24, NTL), np.int32)
    for t in range(NTL):
        for pr in range(3):
            iDy[:, pr, t] = gaddr(CR_F, RP, pr, q * 128 + pp) * NTL + t
        for jj in range(4):
            for h in range(6):
                iDo[:65, jj * 6 + h, t] = gaddr(CR_F, RP, jj, 512 + q * 390 + h * 65 + np.arange(65)) * NTL + t
    m["idxDya"] = iDy; m["idxDo"] = iDo
    for l in range(2):
        pc = np.zeros((128, 32), np.float32)
        pc[:, 0:8] = d["mix_norm_g"][l].reshape(8, 128).T
        pc[:, 8:10] = d["b_q_norm_g"][l].reshape(2, 128).T
        pc[:, 10] = d["b_kv_norm_g"][l]
        pc[0:96, 11] = d["b_q_head_g"][l]; pc[0:96, 12] = d["b_k_head_g"][l]
        pc[:, 13:15] = d["c_ln_g"][l].reshape(2, 128).T; pc[:, 15:17] = d["c_ln_b"][l].reshape(2, 128).T
        pc[:, 17:19] = d["c_out_g"][l].reshape(2, 128).T
        m["w_in%d" % l] = d["w_in"][l]; m["pcolA%d" % l] = pc; m["wuq%d" % l] = d["b_w_uq"][l]; m["wukv%d" % l] = d["b_w_ukv"][l]
        m["ws%d" % l] = d["c_w_s"][l]; m["bs%d" % l] = np.ascontiguousarray(d["c_b_s"][l].reshape(1, 512))
        mu = d["shift_mu"][l]; sl = slice(p * 128, (p + 1) * 128)
        pb = np.zeros((128, 16), np.float32)
        pb[:, 0] = mu[0:384][sl]; pb[:, 1] = mu[384:768][sl]; pb[:, 2] = mu[768:1152][sl]
        pb[:, 3] = d["a_w0"][l][sl]; pb[:, 4] = d["a_a0"][l][sl]; pb[:, 5] = d["a_k_k"][l][sl]; pb[:, 6] = d["a_k_a"][l][sl]
        pb[:, 7] = d["a_r_k"][l].reshape(-1)[sl]; pb[:, 8] = d["a_ln_g"][l][sl]; pb[:, 9] = d["a_ln_b"][l][sl]
        if l == 1:
            pb[:, 10] = d["a_v0"][0][sl]; pb[:, 11] = d["shift_mu"][0][768:1152][sl]
            for cc in range(3):
                pb[:, 12 + cc] = mu[768 + cc * 128:768 + (cc + 1) * 128]
        m["pcolB%d" % l] = pb
        pl = np.zeros((64, 4), np.float32)
        pl[0:32, 0] = mu[1152:1184]; pl[0:32, 1] = mu[1184:1216]; pl[0:64, 2] = mu[1216:1280]
        m["plo%d" % l] = pl
        m["wup%d" % l] = np.ascontiguousarray(d["a_w_up"][l][:, sl]); m["aup%d" % l] = np.ascontiguousarray(d["a_a_up"][l][:, sl])
        m["gup%d" % l] = np.ascontiguousarray(d["a_g_up"][l][:, sl]); m["w0row%d" % l] = np.ascontiguousarray(d["a_w0"][l][sl][None, :])
        pd = np.zeros((128, 16), np.float32)
        pd[:, 0:8] = d["ffn_norm_g"][l].reshape(8, 128).T
        pd[0:64, 8:14] = d["b_out_g"][l].reshape(6, 64).T
        m["w_out%d" % l] = d["w_out"][l]; m["pcolD%d" % l] = pd
    m["vdown"] = np.ascontiguousarray(d["a_v_down"][0].reshape(3, 128, 16).transpose(1, 0, 2))
    m["vup"] = np.ascontiguousarray(d["a_v_up"][0][:, p * 128:(p + 1) * 128])
    m["wg0"] = d["dense_w_gate"]; m["wu0"] = d["dense_w_up"]; m["wd0"] = d["dense_w_down"]
    m["wg1"] = d["moe_w_gate"][0]; m["wu1"] = d["moe_w_up"][0]; m["wd1"] = d["moe_w_down"][0]; m["router"] = d["moe_router"][0]
    return {k: np.ascontiguousarray(v) for k, v in m.items()}


_NC = []
def kernel(**inputs):
    d = {k: np.asarray(v) for k, v in inputs.items()}
    if not _NC:
        _NC.append(build_fused(2048))
    in_maps = [fused_inputs(d, c, 2048) for c in range(8)]
    res = run_bass_kernel_spmd(_NC[0], in_maps, core_ids=list(range(8)))
    outs = [np.asarray(res.results[c]["xoT"]) for c in range(8)]
    out = np.stack([o.T for o in outs]).reshape(2, 8192, 1024)
    return np.ascontiguousarray(out.astype(np.float32))
```

```python
import math, time, sys, contextlib
import numpy as np
import ml_dtypes
from concourse.bass_utils import run_bass_kernel_spmd
import contextlib
import numpy as np
import concourse.bass as bass
import concourse.mybir as mybir

F32 = mybir.dt.float32
BF16 = mybir.dt.bfloat16
I32 = mybir.dt.int32
AF = mybir.ActivationFunctionType
ALU = mybir.AluOpType
AX = mybir.AxisListType

EPOCH = 30000


class Buf:
    _n = 0

    def __init__(self, S, t, kind):
        self.S = S
        self.t = t
        self.kind = kind
        Buf._n += 1
        self.id = Buf._n
        self.lw = {}
        self.lr = {}
        self.dsem = None
        self.dcnt = 0

    def __getitem__(self, idx):
        return self.t[idx]

    def view(self, ap):
        b = Buf(self.S, ap, self.kind)
        b.lw = self.lw; b.lr = self.lr; b.id = self.id
        b.parent = self
        return b

    def reset(self):
        self.lw.clear(); self.lr.clear(); self.dsem = None; self.dcnt = 0


class Eng:
    def __init__(self, S, name, h):
        self.S = S
        self.name = name
        self.h = h
        self.seq = 0
        self.known = {}

    def cur_event(self):
        ep = (self.seq - 1) // EPOCH
        return (self.name, ep), (self.seq - 1) % EPOCH + 1


class Sched:
    _phase = 0

    def __init__(self, nc):
        self.nc = nc
        Sched._phase += 1
        self.pid = Sched._phase
        self.es = contextlib.ExitStack()
        self.E = {
            "pe": Eng(self, "pe", nc.tensor),
            "act": Eng(self, "act", nc.scalar),
            "dve": Eng(self, "dve", nc.vector),
            "pool": Eng(self, "pool", nc.gpsimd),
            "sp": Eng(self, "sp", nc.sync),
        }
        self.semtab = {}
        self.semh = []
        self.final = {}
        self.dram_out = []
        self.ncc = 0

    def sem(self, name):
        h = self.nc.alloc_semaphore("p%d_%s" % (self.pid, name))
        self.semh.append(h)
        return h

    def sbuf(self, name, shape, dt=F32):
        t = self.es.enter_context(self.nc.sbuf_tensor("sb%d_%s" % (self.pid, name), list(shape), dt))
        return Buf(self, t, "sbuf")

    def psum(self, name, shape, dt=F32):
        t = self.es.enter_context(self.nc.psum_tensor("ps%d_%s" % (self.pid, name), list(shape), dt))
        return Buf(self, t, "psum")

    def dram(self, name, shape, dt=F32, kind="Internal"):
        t = self.nc.dram_tensor(name, list(shape), dt, kind=kind)
        b = Buf(self, t.ap(), "dram")
        b.io = kind
        if kind == "ExternalOutput":
            self.dram_out.append(b)
        return b

    def _semfor(self, key):
        if key not in self.semtab:
            self.semtab[key] = self.sem("s_%s_%s" % (str(key[0]), str(key[1])))
        return self.semtab[key]

    def _wait(self, eng, key, val):
        if eng.known.get(key, 0) >= val:
            return
        eng.h.wait_ge(self._semfor(key), val)
        eng.known[key] = val
        if key[0] in self.E:
            for ep in range(key[1]):
                eng.known[(key[0], ep)] = EPOCH

    def _deps(self, eng, reads, writes, skipkey=None):
        for b in reads:
            for k, v in b.lw.items():
                if (k[0] == "pe" and eng.name == "pe"):
                    continue
                self._wait(eng, k, v)
        for b in writes:
            for k, v in b.lw.items():
                if (k[0] == "pe" and eng.name == "pe") or k == skipkey:
                    continue
                self._wait(eng, k, v)
            for k, v in b.lr.items():
                if (k[0] == "pe" and eng.name == "pe"):
                    continue
                self._wait(eng, k, v)

    def _commit(self, key, val, reads, writes):
        if self.final.get(key, 0) < val:
            self.final[key] = val
        for b in reads:
            if b.lr.get(key, 0) < val:
                b.lr[key] = val
        for b in writes:
            if b.kind == "dram":
                b.lw[key] = val
                continue
            b.lw.clear(); b.lw[key] = val
            b.lr.clear()

    def op(self, en, fn, reads=(), writes=()):
        eng = self.E[en]
        reads = [b for b in reads if b is not None]
        writes = [b for b in writes if b is not None]
        self._deps(eng, reads, writes)
        ins = fn(eng.h)
        eng.seq += 1
        key, val = eng.cur_event()
        ins.then_inc(self._semfor(key), 1)
        self._commit(key, val, reads, writes)
        return ins

    def _dma_common(self, qn, reads, writes, issue):
        eng = self.E[qn]
        reads = [b for b in reads if b is not None]
        writes = [b for b in writes if b is not None]
        cand = [b for b in writes if b.kind != "dram"] + [b for b in reads if b.kind != "dram"]
        owner = cand[0] if cand else (writes[0] if writes else reads[0])
        if owner.dsem is None:
            owner.dsem = {}
        key = ("ds" if qn == "pool" else "d", owner.id)
        owner.dsem[key] = owner.dsem.get(key, 0) + 16
        self._deps(eng, reads, [b for b in writes if b.kind != "dram"], skipkey=key)
        val = owner.dsem[key]
        ins = issue(eng.h)
        ins.then_inc(self._semfor(key), 16)
        self._commit(key, val, reads, writes)
        return ins

    def dma(self, qn, out, in_, reads=(), writes=(), **kw):
        return self._dma_common(qn, reads, writes, lambda h: h.dma_start(out=out, in_=in_, **kw))

    def idma(self, out, in_view, idx_ap, reads=(), writes=()):
        return self._dma_common("pool", reads, writes, lambda h: h.indirect_dma_start(
            out=out, out_offset=None, in_=in_view, in_offset=bass.IndirectOffsetOnAxis(ap=idx_ap, axis=0)))

    def allgather(self, in_buf, out_buf, groups):
        eng = self.E["pool"]
        for k, v in in_buf.lw.items():
            self._wait(eng, k, v)
        for k, v in list(out_buf.lw.items()) + list(out_buf.lr.items()):
            if k[0] == "cc":
                continue
            self._wait(eng, k, v)
        self.ncc += 1
        key = ("cc", self.ncc)
        ins = self.nc.gpsimd.collective_compute("AllGather", ALU.bypass, replica_groups=groups, ins=[in_buf.t], outs=[out_buf.t])
        ins.then_inc(self._semfor(key), 1)
        self._commit(key, 1, [in_buf], [])
        out_buf.lw[key] = 1
        return ins

    def drain(self):
        sp = self.E["sp"]
        for k, v in list(self.final.items()):
            self._wait(sp, k, v)

    def phase_end(self, last=False):
        self.drain()
        self.nc.all_engine_barrier()
        if not last:
            self.nc.clear_and_free_semaphores(self.semh)
            self.nc.all_engine_barrier()
        self.es.close()
BF = ml_dtypes.bfloat16
D = 1024; INC = 2208; EPS = 1e-6; TT = 512
CH = [(s, 128) for s in range(0, 1664, 128)] + [(1664, 32)] + [(s, 128) for s in range(1696, 2208, 128)]
TWO_PI = 2.0 * math.pi
T = 64; NCH = TT // T
LWS = -0.6065306597126334
GN_EPS = 64e-5
QT = 512
SCALE = 1.0 / math.sqrt(96.0)
GROUPS = [[0, 1, 2, 3], [4, 5, 6, 7]]
RAB = 1152
RP = 4 * 128 + 4 * 390

class NS:
    pass

CR_F = 128
CR_B = 192
CR_V = 1024

def gaddr(cr, R, i, r):
    m = r // cr
    nr = np.minimum(cr, R - m * cr)
    return 4 * cr * m + i * nr + (r - m * cr)

def allgather_rows(S, send, gath, R, cr, m0=0, m1=None):
    m = m0
    while m * cr < R and (m1 is None or m < m1):
        nr = min(cr, R - m * cr)
        S.allgather(send.view(send.t[m*cr:m*cr+nr, :]), gath.view(gath.t[4*cr*m:4*cr*m + 4*nr, :]), GROUPS)
        m += 1

def a2_consts():
    c = np.zeros((6, 128, 128), np.float32)
    c[0] = 1.0
    c[1] = np.eye(128)
    P = np.zeros((96, 96), np.float32)
    for i in range(16):
        P[64 + i, 80 + i] = -1.0
        P[80 + i, 64 + i] = 1.0
    c[2][:96, :96] = P.T
    s = np.arange(128)[:, None]; t = np.arange(128)[None, :]
    c[3] = (s <= t)
    inv = (10000.0 ** (-np.arange(0, 32, 2, dtype=np.float32) / 32)).astype(np.float32)
    c[4][64:80, 0] = inv; c[4][80:96, 0] = inv
    c[4][0:64, 1] = 1.0
    return np.ascontiguousarray(c.transpose(1, 0, 2))


def rwkv_consts():
    c = np.zeros((8, 128, 128), np.float32)
    bd = np.zeros((128, 128), np.float32); bd[:64, :64] = 1; bd[64:, 64:] = 1
    s = np.arange(128)[:, None]; t = np.arange(128)[None, :]
    c[0] = bd
    c[1] = bd * (s <= t)
    c[2] = bd * (s < t)
    c[3] = bd * (s > t)
    c[4] = bd * (s <= t)
    c[5] = -c[4]
    c[6] = np.eye(128)
    c[7] = bd / 64.0
    return np.ascontiguousarray(c.transpose(1, 0, 2))


def masks_C():
    m = np.zeros((4, 128, QT), np.float32)
    for d in range(4):
        kk = d * 128 + np.arange(128)[:, None]; qq = np.arange(QT)[None, :]
        m[d] = (kk // 64 <= qq // 64)
    return m.astype(BF)


def d_consts():
    c = np.zeros((2, 128, 128), np.float32)
    c[0] = 1.0; c[1] = np.eye(128)
    return np.ascontiguousarray(c.transpose(1, 0, 2))


def phase_A(nc, G, l):
    NT = G.NT
    S = Sched(nc)
    for b_ in G.persist:
        b_.reset()
    xT_d = G.xT_d; w_d = G.inp["w_in%d" % l]; pc_d = G.inp["pcolA%d" % l]; pos_d = G.inp["pos"]
    wuq_d = G.inp["wuq%d" % l]; wukv_d = G.inp["wukv%d" % l]; ws_d = G.inp["ws%d" % l]; bs_d = G.inp["bs%d" % l]; cst_d = G.inp["cstA"]
    sAf = G.sAf[l]; sAb = G.sAb[l]; sAv = G.sAv[l]; ycl = G.ycl[l]
    pc = S.sbuf("pc", [128, 32]); cst = S.sbuf("cst", [128, 6, 128])
    S.dma("sp", pc[:], pc_d[:], reads=[pc_d], writes=[pc])
    S.dma("sp", cst[:], cst_d[:], reads=[cst_d], writes=[cst])
    ONES = cst[:, 0, :]; IDENT = cst[:, 1, :]; PT = cst[0:96, 2, 0:96]; MASK = cst[:, 3, :]
    INVF = cst[0:96, 4, 0:1]; NOPE = cst[0:96, 4, 1:2]
    onesb = S.sbuf("onesb", [128, 128], BF16); identb = S.sbuf("identb", [128, 128], BF16); ptb = S.sbuf("ptb", [96, 96], BF16)
    onesrow = S.sbuf("onesrow", [1, 128], BF16)
    S.op("dve", lambda h: h.tensor_copy(out=onesb[:], in_=ONES), reads=[cst], writes=[onesb])
    S.op("dve", lambda h: h.tensor_copy(out=identb[:], in_=IDENT), reads=[cst], writes=[identb])
    S.op("dve", lambda h: h.tensor_copy(out=ptb[:], in_=PT), reads=[cst], writes=[ptb])
    S.op("pool", lambda h: h.memset(onesrow[:], 1.0), writes=[onesrow])
    epsc = S.sbuf("epsc", [128, 1]); S.op("pool", lambda h: h.memset(epsc[:], EPS), writes=[epsc])
    pic = S.sbuf("pic", [128, 1]); S.op("pool", lambda h: h.memset(pic[:], -math.pi), writes=[pic])

    NPS = 7
    slots = [S.psum(f"slot{i}", [128, TT]) for i in range(NPS)]
    ptr = S.psum("ptr", [128, 1024], BF16)
    psc = [0]
    def getps():
        psc[0] += 1
        return slots[psc[0] % NPS]
    tmpc = [0]
    tmps = [S.sbuf(f"tmp{i}", [128, TT]) for i in range(4)]
    def gettmp():
        tmpc[0] += 1
        return tmps[tmpc[0] % 4]

    xT = [S.sbuf(f"xT{k}", [128, NT], F32) for k in range(8)]
    wb = [S.sbuf(f"wb{k}", [128, INC], BF16) for k in range(8)]
    for k in range(8):
        S.dma("sp", xT[k][:], G.xsrc[l][k*128:(k+1)*128, :], reads=[G.xsrc[l]], writes=[xT[k]])
    for k in range(8):
        S.dma("pool", wb[k][:], w_d[k*128:(k+1)*128, :], reads=[w_d], writes=[wb[k]])
    wuq = [S.sbuf(f"wuq{k}", [128, 576], BF16) for k in range(2)]
    for k in range(2):
        S.dma("pool", wuq[k][:], wuq_d[k*128:(k+1)*128, :], reads=[wuq_d], writes=[wuq[k]])
    wukv = S.sbuf("wukv", [128, 768], BF16)
    S.dma("pool", wukv[:], wukv_d[:, :], reads=[wukv_d], writes=[wukv])
    wukv_v = S.sbuf("wukv_v", [128, 6, 64], BF16)
    S.op("pool", lambda h: h.tensor_copy(out=wukv_v[:], in_=wukv[:].rearrange("p (h c) -> p h c", c=128)[:, :, 64:128]), reads=[wukv], writes=[wukv_v])
    wsT = [S.sbuf(f"wsT{g}", [128, 128], BF16) for g in range(4)]
    wsl = S.sbuf("wsl", [128, 4, 128], F32)
    S.dma("sp", wsl[:], ws_d.t.rearrange("g t s -> t g s"), reads=[ws_d], writes=[wsl])
    for g in range(4):
        p = getps()
        S.op("pe", lambda h: h.transpose(p[:, 0:128], wsl[:, g, :], IDENT), reads=[wsl, cst], writes=[p])
        S.op("dve", lambda h: h.tensor_tensor(out=wsT[g][:], in0=p[:, 0:128], in1=MASK, op=ALU.mult), reads=[p, cst], writes=[wsT[g]])
    bsr = S.sbuf("bsr", [1, 512], BF16); bsf = S.sbuf("bsf", [1, 512], F32)
    S.dma("sp", bsf[:], bs_d[:, :], reads=[bs_d], writes=[bsf])
    S.op("dve", lambda h: h.tensor_copy(out=bsr[:], in_=bsf[:]), reads=[bsf], writes=[bsr])

    posi = S.sbuf("posi", [96, TT], I32); posf = S.sbuf("posf", [96, TT], F32)
    cosf = S.sbuf("cosf", [96, TT], F32); sinf = S.sbuf("sinf", [96, TT], F32)
    def rope_tables(sl):
        S.dma("sp", posi[:], pos_d.t[0:1, sl].partition_broadcast(96), reads=[pos_d], writes=[posi])
        S.op("dve", lambda h: h.tensor_copy(out=posf[:], in_=posi[:]), reads=[posi], writes=[posf])
        S.op("dve", lambda h: h.tensor_scalar(out=posf[:], in0=posf[:], scalar1=INVF, scalar2=None, op0=ALU.mult), reads=[posf, cst], writes=[posf])
        for dst, shift in [(sinf, 0.0), (cosf, 0.5 * math.pi)]:
            S.op("dve", lambda h: h.tensor_scalar(out=dst[:], in0=posf[:], scalar1=shift, scalar2=None, op0=ALU.add), reads=[posf], writes=[dst])
            S.op("dve", lambda h: h.tensor_scalar(out=rrf[:], in0=dst[:], scalar1=1.0 / TWO_PI, scalar2=None, op0=ALU.mult), reads=[dst], writes=[rrf])
            S.op("dve", lambda h: h.tensor_copy(out=rri[:], in_=rrf[:]), reads=[rrf], writes=[rri])
            S.op("dve", lambda h: h.tensor_copy(out=rrf[:], in_=rri[:]), reads=[rri], writes=[rrf])
            S.op("dve", lambda h: h.scalar_tensor_tensor(out=dst[:], in0=rrf[:], scalar=-6.28125, in1=dst[:], op0=ALU.mult, op1=ALU.add), reads=[rrf, dst], writes=[dst])
            S.op("dve", lambda h: h.scalar_tensor_tensor(out=dst[:], in0=rrf[:], scalar=-(TWO_PI - 6.28125), in1=dst[:], op0=ALU.mult, op1=ALU.add), reads=[rrf, dst], writes=[dst])
            S.op("dve", lambda h: h.tensor_scalar(out=rrf[:], in0=dst[:], scalar1=math.pi, scalar2=None, op0=ALU.is_gt), reads=[dst], writes=[rrf])
            S.op("dve", lambda h: h.scalar_tensor_tensor(out=dst[:], in0=rrf[:], scalar=-TWO_PI, in1=dst[:], op0=ALU.mult, op1=ALU.add), reads=[rrf, dst], writes=[dst])
            S.op("dve", lambda h: h.tensor_scalar(out=rrf[:], in0=dst[:], scalar1=-math.pi, scalar2=None, op0=ALU.is_lt), reads=[dst], writes=[rrf])
            S.op("dve", lambda h: h.scalar_tensor_tensor(out=dst[:], in0=rrf[:], scalar=TWO_PI, in1=dst[:], op0=ALU.mult, op1=ALU.add), reads=[rrf, dst], writes=[dst])
            S.op("act", lambda h: h.activation(out=dst[:], in_=dst[:], func=AF.Sin), reads=[dst], writes=[dst])
    rrf = S.sbuf("rrf", [96, TT], F32); rri = S.sbuf("rri", [96, TT], I32)

    sq = [S.sbuf(f"sq{i}", [128, TT], BF16) for i in range(2)]
    hT = [S.sbuf(f"hT{k}", [128, TT], BF16) for k in range(8)]
    rs = S.sbuf("rs", [128, TT], F32)
    zo = [S.sbuf(f"zo{i}", [128, TT], F32) for i in range(3)]
    zB = [S.sbuf(f"zB{i}", [128, TT], F32) for i in range(4)]
    zC = [S.sbuf(f"zC{i}", [128, TT], F32) for i in range(4)]
    cqn = [S.sbuf(f"cqn{i}", [128, TT], BF16) for i in range(2)]
    ckvn = S.sbuf("ckvn", [128, TT], BF16)
    kfull = S.sbuf("kfull", [96, TT], F32)
    sq96 = S.sbuf("sq96", [96, TT], BF16)
    rs96 = S.sbuf("rs96", [96, TT], F32)
    qn = S.sbuf("qn", [96, TT], BF16)
    t1 = S.sbuf("t1", [96, TT], F32); t2 = S.sbuf("t2", [96, TT], F32)
    qo = [S.sbuf(f"qo{i}", [96, TT], BF16) for i in range(3)]
    vo = [S.sbuf(f"vo{i}", [128, 384], BF16) for i in range(2)]
    vnb = [S.sbuf(f"vnb{i}", [128, TT], BF16) for i in range(2)]
    vtok = [S.sbuf(f"vtok{i}", [128, 128], BF16) for i in range(2)]
    yg = [S.sbuf(f"yg{i}", [128, TT], F32) for i in range(2)]
    yco = [S.sbuf(f"yco{i}", [128, TT], F32) for i in range(2)]
    cnt = [0]; qc = [0]

    def headnorm_rope(src_ps_or_sb, srcbuf, gcol, out_d, h, sl, t):
        S.op("act", lambda hh: hh.activation(out=sq96[:], in_=src_ps_or_sb, func=AF.Square), reads=[srcbuf], writes=[sq96])
        p = getps()
        S.op("pe", lambda hh: hh.matmul(p[0:96, :], lhsT=onesb[0:96, 0:96], rhs=sq96[:], start=True, stop=True), reads=[onesb, sq96], writes=[p])
        S.op("act", lambda hh: hh.activation(out=rs96[:], in_=p[0:96, :], func=AF.Sqrt, scale=1.0/96, bias=epsc[0:96, :]), reads=[p, epsc], writes=[rs96])
        S.op("dve", lambda hh: hh.reciprocal(out=rs96[:], in_=rs96[:]), reads=[rs96], writes=[rs96])
        S.op("dve", lambda hh: hh.scalar_tensor_tensor(out=qn[:], in0=src_ps_or_sb, scalar=gcol, in1=rs96[:], op0=ALU.mult, op1=ALU.mult), reads=[srcbuf, pc, rs96], writes=[qn])
        p2 = getps()
        S.op("pe", lambda hh: hh.matmul(p2[0:96, :], lhsT=ptb[:], rhs=qn[:], start=True, stop=True), reads=[ptb, qn], writes=[p2])
        S.op("pool", lambda hh: hh.tensor_tensor(out=t1[:], in0=qn[:], in1=cosf[:], op=ALU.mult), reads=[qn, cosf], writes=[t1])
        S.op("dve", lambda hh: hh.tensor_tensor(out=t2[:], in0=p2[0:96, :], in1=sinf[:], op=ALU.mult), reads=[p2, sinf], writes=[t2])
        o = qo[qc[0] % 3]; qc[0] += 1
        S.op("pool", lambda hh: hh.tensor_tensor(out=o[:], in0=t1[:], in1=t2[:], op=ALU.add), reads=[t1, t2], writes=[o])
        if out_d == "q":
            S.dma("sp", sAb[h*96:(h+1)*96, sl], o[:], reads=[o], writes=[sAb])
        else:
            dst = sAb.t[576 + h*96:576 + (h+1)*96, :].rearrange("d (j x) -> d j x", j=4)[:, :, t*128:(t+1)*128]
            S.dma("sp", dst, o[:].rearrange("d (j c) -> d j c", c=128), reads=[o], writes=[sAb])

    for t in range(NT // TT):
        sl = slice(t*TT, (t+1)*TT)
        rope_tables(sl)
        for k in range(8):
            s = sq[k % 2]
            S.op("act", lambda h: h.activation(out=s[:], in_=xT[k][:, sl], func=AF.Square), reads=[xT[k]], writes=[s])
            ps_ss = getps() if k == 0 else ps_ss
            S.op("pe", lambda h: h.matmul(ps_ss[:], lhsT=onesb[:], rhs=s[:], start=(k == 0), stop=(k == 7)), reads=[onesb, s], writes=[ps_ss])
        S.op("act", lambda h: h.activation(out=rs[:], in_=ps_ss[:], func=AF.Sqrt, scale=1.0/D, bias=epsc[:]), reads=[ps_ss, epsc], writes=[rs])
        S.op("dve", lambda h: h.reciprocal(out=rs[:], in_=rs[:]), reads=[rs], writes=[rs])
        for k in range(8):
            S.op("dve", lambda h: h.scalar_tensor_tensor(out=hT[k][:], in0=xT[k][:, sl], scalar=pc[:, k:k+1], in1=rs[:], op0=ALU.mult, op1=ALU.mult), reads=[xT[k], pc, rs], writes=[hT[k]])
        for m, (c0, mw) in enumerate(CH):
            pz = getps()
            for k in range(8):
                S.op("pe", lambda h: h.matmul(pz[:mw, :], lhsT=wb[k][:, c0:c0+mw], rhs=hT[k][:], start=(k == 0), stop=(k == 7)), reads=[wb[k], hT[k]], writes=[pz])
            if m < 10:
                o = zo[cnt[0] % 3]; cnt[0] += 1
                if m % 2 == 0:
                    S.op("act", lambda h: h.copy(out=o[:mw, :], in_=pz[:mw, :]), reads=[pz], writes=[o])
                else:
                    S.op("dve", lambda h: h.tensor_copy(out=o[:mw, :], in_=pz[:mw, :]), reads=[pz], writes=[o])
                S.dma("sp", sAf[c0:c0+mw, sl], o[:mw, :], reads=[o], writes=[sAf])
            elif m < 14:
                o = zB[m - 10]
                S.op("act", lambda h: h.copy(out=o[:mw, :], in_=pz[:mw, :]), reads=[pz], writes=[o])
            else:
                o = zC[m - 14]
                S.op("act", lambda h: h.activation(out=o[:], in_=pz[:], func=AF.Gelu), reads=[pz], writes=[o])
        ps1 = getps()
        for k in range(2):
            s = sq[k % 2]
            S.op("act", lambda h: h.activation(out=s[:], in_=zB[k][:], func=AF.Square), reads=[zB[k]], writes=[s])
            S.op("pe", lambda h: h.matmul(ps1[:], lhsT=onesb[:], rhs=s[:], start=(k == 0), stop=(k == 1)), reads=[onesb, s], writes=[ps1])
        S.op("act", lambda h: h.activation(out=rs[:], in_=ps1[:], func=AF.Sqrt, scale=1.0/256, bias=epsc[:]), reads=[ps1, epsc], writes=[rs])
        S.op("dve", lambda h: h.reciprocal(out=rs[:], in_=rs[:]), reads=[rs], writes=[rs])
        for k in range(2):
            S.op("dve", lambda h: h.scalar_tensor_tensor(out=cqn[k][:], in0=zB[k][:], scalar=pc[:, 8+k:9+k], in1=rs[:], op0=ALU.mult, op1=ALU.mult), reads=[zB[k], pc, rs], writes=[cqn[k]])
        for hd in range(6):
            pq = getps()
            for k in range(2):
                S.op("pe", lambda h: h.matmul(pq[0:96, :], lhsT=wuq[k][:, hd*96:(hd+1)*96], rhs=cqn[k][:], start=(k == 0), stop=(k == 1)), reads=[wuq[k], cqn[k]], writes=[pq])
            headnorm_rope(pq[0:96, :], pq, pc[0:96, 11:12], "q", hd, sl, t)
        s = sq[0]
        S.op("act", lambda h: h.activation(out=s[:], in_=zB[2][:], func=AF.Square), reads=[zB[2]], writes=[s])
        ps2 = getps()
        S.op("pe", lambda h: h.matmul(ps2[:], lhsT=onesb[:], rhs=s[:], start=True, stop=True), reads=[onesb, s], writes=[ps2])
        S.op("act", lambda h: h.activation(out=rs[:], in_=ps2[:], func=AF.Sqrt, scale=1.0/128, bias=epsc[:]), reads=[ps2, epsc], writes=[rs])
        S.op("dve", lambda h: h.reciprocal(out=rs[:], in_=rs[:]), reads=[rs], writes=[rs])
        S.op("dve", lambda h: h.scalar_tensor_tensor(out=ckvn[:], in0=zB[2][:], scalar=pc[:, 10:11], in1=rs[:], op0=ALU.mult, op1=ALU.mult), reads=[zB[2], pc, rs], writes=[ckvn])
        S.op("pool", lambda h: h.tensor_copy(out=kfull[64:96, :], in_=zB[3][0:32, :]), reads=[zB[3]], writes=[kfull])
        for hd in range(6):
            pk = getps()
            S.op("pe", lambda h: h.matmul(pk[0:64, :], lhsT=wukv[:, hd*128:hd*128+64], rhs=ckvn[:], start=True, stop=True), reads=[wukv, ckvn], writes=[pk])
            S.op("act", lambda h: h.copy(out=kfull[0:64, :], in_=pk[0:64, :]), reads=[pk], writes=[kfull])
            headnorm_rope(kfull[:], kfull, pc[0:96, 12:13], "k", hd, sl, t)
        for j in range(TT // 128):
            pv = getps()
            S.op("pe", lambda h: h.matmul(pv[:, 0:384], lhsT=ckvn[:, j*128:(j+1)*128], rhs=wukv_v[:].rearrange("p h c -> p (h c)"), start=True, stop=True), reads=[ckvn, wukv_v], writes=[pv])
            o = vo[j % 2]
            S.op("act", lambda h: h.copy(out=o[:], in_=pv[:, 0:384]), reads=[pv], writes=[o])
            S.dma("sp", sAv[j*(NT//4) + t*128: j*(NT//4) + (t+1)*128, :], o[:], reads=[o], writes=[sAv])
        pm = getps()
        for k in range(2):
            S.op("pe", lambda h: h.matmul(pm[:], lhsT=ONES, rhs=zC[2+k][:], start=(k == 0), stop=(k == 1)), reads=[cst, zC[2+k]], writes=[pm])
        vc = [gettmp(), gettmp()]
        for k in range(2):
            S.op("dve", lambda h: h.scalar_tensor_tensor(out=vc[k][:], in0=pm[:], scalar=-1.0/256, in1=zC[2+k][:], op0=ALU.mult, op1=ALU.add), reads=[pm, zC[2+k]], writes=[vc[k]])
        pvv = getps()
        for k in range(2):
            s2 = gettmp()
            S.op("pool", lambda h: h.tensor_tensor(out=s2[:], in0=vc[k][:], in1=vc[k][:], op=ALU.mult), reads=[vc[k]], writes=[s2])
            S.op("pe", lambda h: h.matmul(pvv[:], lhsT=ONES, rhs=s2[:], start=(k == 0), stop=(k == 1)), reads=[cst, s2], writes=[pvv])
        S.op("act", lambda h: h.activation(out=rs[:], in_=pvv[:], func=AF.Sqrt, scale=1.0/256, bias=epsc[:]), reads=[pvv, epsc], writes=[rs])
        S.op("dve", lambda h: h.reciprocal(out=rs[:], in_=rs[:]), reads=[rs], writes=[rs])
        for k in range(2):
            S.op("dve", lambda h: h.tensor_tensor(out=vc[k][:], in0=vc[k][:], in1=rs[:], op=ALU.mult), reads=[vc[k], rs], writes=[vc[k]])
            S.op("dve", lambda h: h.tensor_scalar(out=vnb[k][:], in0=vc[k][:], scalar1=pc[:, 13+k:14+k], scalar2=pc[:, 15+k:16+k], op0=ALU.mult, op1=ALU.add), reads=[vc[k], pc], writes=[vnb[k]])
        for j in range(TT // 128):
            bsl = slice(j*128, (j+1)*128)
            for k in range(2):
                S.op("pe", lambda h: h.transpose(ptr[:, k*128:(k+1)*128], vnb[k][:, bsl], identb[:]), reads=[vnb[k], identb], writes=[ptr])
                S.op("act", lambda h: h.copy(out=vtok[k][:], in_=ptr[:, k*128:(k+1)*128]), reads=[ptr], writes=[vtok[k]])
            for g in range(4):
                k = g // 2; hf = slice((g % 2)*64, (g % 2)*64 + 64)
                pg = getps()
                S.op("pe", lambda h: h.matmul(pg[:, 0:128], lhsT=vtok[k][:], rhs=wsT[g][:], start=True, stop=False), reads=[vtok[k], wsT[g]], writes=[pg])
                S.op("pe", lambda h: h.matmul(pg[:, 0:128], lhsT=onesrow[:], rhs=bsr[:, g*128:(g+1)*128], start=False, stop=True), reads=[onesrow, bsr], writes=[pg])
                S.op("dve", lambda h: h.tensor_tensor(out=yg[k][hf, bsl], in0=pg[hf, 0:128], in1=zC[k][hf, bsl], op=ALU.mult), reads=[pg, zC[k]], writes=[yg[k]])
        py = getps()
        for k in range(2):
            s2 = gettmp()
            S.op("pool", lambda h: h.tensor_tensor(out=s2[:], in0=yg[k][:], in1=yg[k][:], op=ALU.mult), reads=[yg[k]], writes=[s2])
            S.op("pe", lambda h: h.matmul(py[:], lhsT=ONES, rhs=s2[:], start=(k == 0), stop=(k == 1)), reads=[cst, s2], writes=[py])
        S.op("act", lambda h: h.activation(out=rs[:], in_=py[:], func=AF.Sqrt, scale=1.0/256, bias=epsc[:]), reads=[py, epsc], writes=[rs])
        S.op("dve", lambda h: h.reciprocal(out=rs[:], in_=rs[:]), reads=[rs], writes=[rs])
        for k in range(2):
            o = yco[k]
            S.op("dve", lambda h: h.scalar_tensor_tensor(out=o[:], in0=yg[k][:], scalar=pc[:, 17+k:18+k], in1=rs[:], op0=ALU.mult, op1=ALU.mult), reads=[yg[k], pc, rs], writes=[o])
            S.dma("sp", ycl[k*128:(k+1)*128, sl], o[:], reads=[o], writes=[ycl])

    allgather_rows(S, sAb, G.gAb[l], RAB, CR_B)
    allgather_rows(S, sAv, G.gAv[l], NT, min(CR_V, NT))
    S.phase_end()


def phase_B(nc, G, l):
    NT = G.NT; SEQ = 4 * NT; NTILE = SEQ // TT; NTL = NT // TT
    S = Sched(nc)
    for b_ in G.persist:
        b_.reset()
    gAf = G.gAf[l]; sP = G.sP[l]
    allgather_rows(S, sP, G.gP[l], RP, CR_F, m0=4)
    pcol_d = G.inp["pcolB%d" % l]; plo_d = G.inp["plo%d" % l]; wup_d = G.inp["wup%d" % l]; aup_d = G.inp["aup%d" % l]
    gup_d = G.inp["gup%d" % l]; w0row_d = G.inp["w0row%d" % l]; cst_d = G.inp["cstB"]
    if l == 1:
        vdown_d = G.inp["vdown"]; vup_d = G.inp["vup"]
    idxB = S.sbuf("idxB", [128, 3, NTILE], I32); idxBh = S.sbuf("idxBh", [128, 3, NTILE], I32)
    S.dma("sp", idxB[:], G.inp["idxB"].t, reads=[G.inp["idxB"]], writes=[idxB])
    S.dma("sp", idxBh[:], G.inp["idxBh"].t, reads=[G.inp["idxBh"]], writes=[idxBh])
    pcol = S.sbuf("pcol", [128, 16]); plo = S.sbuf("plo", [64, 4])
    wup = S.sbuf("wup", [32, 128]); aup = S.sbuf("aup", [32, 128]); gup = S.sbuf("gup", [64, 128])
    w0row = S.sbuf("w0row", [1, 128]); cst = S.sbuf("cst", [128, 8, 128])
    onesrow = S.sbuf("onesrow", [1, 128])
    omk = S.sbuf("omk", [128, 1])
    for sb, dr in [(pcol, pcol_d), (plo, plo_d), (wup, wup_d), (aup, aup_d), (gup, gup_d), (w0row, w0row_d)]:
        S.dma("sp", sb[:], dr[:], reads=[dr], writes=[sb])
    S.dma("sp", cst[:], cst_d[:], reads=[cst_d], writes=[cst])
    if l == 1:
        vdown = S.sbuf("vdown", [128, 3, 16]); vup = S.sbuf("vup", [16, 128])
        S.dma("sp", vdown[:], vdown_d[:], reads=[vdown_d], writes=[vdown])
        S.dma("sp", vup[:], vup_d[:], reads=[vup_d], writes=[vup])
    S.op("pool", lambda h: h.memset(onesrow[:], 1.0), writes=[onesrow])
    S.op("dve", lambda h: h.tensor_scalar(out=omk[:], in0=pcol[:, 6:7], scalar1=-1.0, scalar2=1.0, op0=ALU.mult, op1=ALU.add), reads=[pcol], writes=[omk])
    ONESB = cst[:, 0, :]; TRI = cst[:, 1, :]; TRIS = cst[:, 2, :]; MSU = cst[:, 2, :]; MSL = cst[:, 3, :]
    MUI = cst[:, 4, :]; MNUI = cst[:, 5, :]; IDENT = cst[:, 6, :]; MEANB = cst[:, 7, :]
    C_MUR, C_MUK, C_MUV, C_W0, C_A0, C_KK, C_KA, C_RK, C_LNG, C_LNB, C_V0, C_MU0V = range(12)

    def col(i, n=128):
        return pcol[0:n, i:i+1]

    NB = 2
    raw = {}
    names = [("zr", 128), ("zk", 128), ("zv", 128), ("zw", 32), ("za", 32), ("zg", 64)]
    if l == 1:
        names += [("zva0", 128), ("zva1", 128), ("zva2", 128), ("zv0", 128)]
    for nm, rows in names:
        raw[nm] = [S.sbuf(f"raw_{nm}{i}", [rows, TT + 1]) for i in range(1)] * NB
        S.op("pool", lambda h: h.memset(raw[nm][0][:, 0:1], 0.0), writes=[raw[nm][0]])
    tmp = [S.sbuf(f"tmp{i}", [128, TT]) for i in range(3)]
    tmpc = [0]
    def gettmp():
        tmpc[0] += 1
        return tmp[tmpc[0] % 3]
    sh = {nm: S.sbuf(f"sh_{nm}", [rows, TT]) for nm, rows in names}
    th = S.sbuf("th", [32, TT])
    sgd = S.sbuf("sgd", [64, TT])
    a_t = S.sbuf("a_t", [128, TT]); g_t = [S.sbuf(f"g_t{i}", [128, TT]) for i in range(NB)]
    kk = S.sbuf("kk", [128, TT]); kap = S.sbuf("kap", [128, TT]); kmod = S.sbuf("kmod", [128, TT])
    b_t = S.sbuf("b_t", [128, TT]); v_t = [S.sbuf(f"v_t{i}", [128, TT]) for i in range(NB)]
    bv = [S.sbuf(f"bv{i}", [128, TT]) for i in range(NB)]
    sq = S.sbuf("sq", [128, TT]); nrm = S.sbuf("nrm", [128, TT])
    sgtok = [S.sbuf(f"sgtok{i}", [128, 128]) for i in range(2)]
    eL = S.sbuf("eL", [128, TT]); eLx = S.sbuf("eLx", [128, TT]); enL = S.sbuf("enL", [128, TT])
    gam = [S.sbuf(f"gam{i}", [128, NCH]) for i in range(NB)]
    vd_sb = S.sbuf("vd_sb", [16, TT]); sv = S.sbuf("sv", [128, TT])
    bd = {nm: [S.sbuf(f"bd_{nm}{i}", [128, NCH, 128]) for i in range(NB)] for nm in ["RT", "KpT", "KT", "BT", "VT"]}
    for nm in bd:
        for i in range(NB):
            S.op("pool", lambda h: h.memset(bd[nm][i][:], 0.0), writes=[bd[nm][i]])
    yT = [S.sbuf(f"yT{i}", [128, TT]) for i in range(NB)]
    yo = [S.sbuf(f"yo{i}", [128, TT]) for i in range(1)] * NB
    pL = S.psum("pL", [128, TT]); pLx = S.psum("pLx", [128, TT])
    NPS = 6
    slots = [S.psum(f"slot{i}", [128, TT]) for i in range(NPS)]
    psc = [0]
    def getps():
        psc[0] += 1
        s_ = slots[psc[0] % NPS]
        return s_.view(s_.t[:, 0:128])
    def getpbig():
        psc[0] += 1
        return slots[psc[0] % NPS]
    def pool_of(name, n, shape=(128, 128)):
        bufs = [S.sbuf(f"{name}{i}", list(shape)) for i in range(n)]
        c = [0]
        def get():
            c[0] += 1
            return bufs[c[0] % n]
        return get
    NPIPE = 6
    g_N = pool_of("cN", NPIPE); g_Q = pool_of("cQ", NPIPE); g_Aak = pool_of("cAak", NPIPE)
    g_nArb = pool_of("cnArb", NPIPE); g_Ark = pool_of("cArk", NPIPE)
    g_nB = pool_of("cnB", NPIPE); g_Kb = pool_of("cKb", NPIPE); g_Vb = pool_of("cVb", NPIPE)
    g_X = pool_of("cX", 6); g_XT = pool_of("cXT", 6); g_WT = pool_of("cWT", 6); g_WTf = pool_of("cWTf", NPIPE + 1)
    g_rhs = pool_of("crhs", 2); g_U = pool_of("cU", 2)
    Mst = [S.sbuf(f"Mst{i}", [128, 128]) for i in range(2)]
    Mg = S.sbuf("Mg", [128, 128])
    S.op("pool", lambda h: h.memset(Mst[0][:], 0.0), writes=[Mst[0]])
    evc = [0]
    def evac_copy(dst, src, scale=None):
        evc[0] += 1
        if evc[0] % 2 == 0:
            if scale is None:
                S.op("act", lambda h: h.copy(out=dst[:], in_=src[:]), reads=[src], writes=[dst])
            else:
                S.op("act", lambda h: h.mul(out=dst[:], in_=src[:], mul=scale), reads=[src], writes=[dst])
        else:
            if scale is None:
                S.op("dve", lambda h: h.tensor_copy(out=dst[:], in_=src[:]), reads=[src], writes=[dst])
            else:
                S.op("dve", lambda h: h.tensor_scalar(out=dst[:], in0=src[:], scalar1=scale, scalar2=None, op0=ALU.mult), reads=[src], writes=[dst])

    def mm(ps, lhsT, rhs, rl, rr, start=True, stop=True, psl=None):
        o = ps[:] if psl is None else psl
        S.op("pe", lambda h: h.matmul(o, lhsT=lhsT, rhs=rhs, start=start, stop=stop), reads=[rl, rr], writes=[ps])

    def pre_steps(ti):
        bi = ti % NB
        t0 = ti * TT
        steps = []
        def loads():
            i_ = ti // NTL; n_ = ti % NTL
            gv = gAf.t.rearrange("a (n c) -> (a n) c", c=TT)
            gh = gAf.t.rearrange("a (c o) -> (a c) o", o=1)
            kinds = [("zr", 0, gAf, gv, gh), ("zk", 1, gAf, gv, gh), ("zv", 2, gAf, gv, gh)]
            if l == 1:
                gv0 = G.gAf[0].t.rearrange("a (n c) -> (a n) c", c=TT)
                gh0 = G.gAf[0].t.rearrange("a (c o) -> (a c) o", o=1)
                kinds.append(("zv0", 2, G.gAf[0], gv0, gh0))
            for nm, kd_, gb, v_, h_ in kinds:
                dst = raw[nm][bi]
                S.idma(dst[:, 1:TT+1], v_, idxB[:, kd_, ti:ti+1], reads=[gb, idxB], writes=[dst])
                if ti > 0:
                    S.idma(dst[:, 0:1], h_, idxBh[:, kd_, ti:ti+1], reads=[gb, idxBh], writes=[dst])
            stat = [("zw", 1152, 32), ("za", 1184, 32), ("zg", 1216, 64)]
            if l == 1:
                stat += [("zva0", 768, 128), ("zva1", 896, 128), ("zva2", 1024, 128)]
            for nm, r0, nr in stat:
                dst = raw[nm][bi]
                base = int(gaddr(CR_F, 1280, i_, r0))
                if n_ > 0:
                    S.dma("sp", dst[:, 0:TT+1], gAf[base:base+nr, n_*TT-1:(n_+1)*TT], reads=[gAf], writes=[dst])
                else:
                    S.dma("sp", dst[:, 1:TT+1], gAf[base:base+nr, 0:TT], reads=[gAf], writes=[dst])
                    if ti > 0:
                        pb = int(gaddr(CR_F, 1280, i_ - 1, r0))
                        S.dma("sp", dst[:, 0:1], gAf[pb:pb+nr, NT-1:NT], reads=[gAf], writes=[dst], allow_slow_non_contiguous=True)
        steps.append(loads)
        def shift(nm, mucol):
            X = raw[nm][bi]; o = sh[nm]; rows = X.t.shape[0]
            tp = gettmp()
            S.op("pool", lambda h: h.tensor_tensor(out=tp[0:rows, :], in0=X[:, 0:TT], in1=X[:, 1:TT+1], op=ALU.subtract), reads=[X], writes=[tp])
            S.op("dve", lambda h: h.scalar_tensor_tensor(out=o[:], in0=tp[0:rows, :], scalar=mucol, in1=X[:, 1:TT+1], op0=ALU.mult, op1=ALU.add), reads=[tp, X, pcol, plo], writes=[o])
        def shifts():
            shift("zr", col(C_MUR)); shift("zk", col(C_MUK)); shift("zv", col(C_MUV))
            shift("zw", plo[0:32, 0:1]); shift("za", plo[0:32, 1:2]); shift("zg", plo[0:64, 2:3])
            if l == 1:
                shift("zva0", col(12)); shift("zva1", col(13)); shift("zva2", col(14)); shift("zv0", col(C_MU0V))
        steps.append(shifts)
        def loras():
            S.op("act", lambda h: h.activation(out=th[:], in_=sh["zw"][:], func=AF.Tanh), reads=[sh["zw"]], writes=[th])
            for j in range(TT // 128):
                pt = getps()
                mm(pt, th[:, j*128:(j+1)*128], wup[:], th, wup, start=True, stop=False)
                mm(pt, onesrow[:], w0row[:], onesrow, w0row, start=False, stop=True)
                st = sgtok[j % 2]
                S.op("act", lambda h: h.activation(out=st[:], in_=pt[:], func=AF.Sigmoid), reads=[pt], writes=[st])
                mm(pL, st[:], TRI, st, cst, psl=pL[:, j*128:(j+1)*128])
                mm(pLx, st[:], TRIS, st, cst, psl=pLx[:, j*128:(j+1)*128])
            S.op("act", lambda h: h.activation(out=eL[:], in_=pL[:], func=AF.Exp, scale=LWS), reads=[pL], writes=[eL])
            S.op("act", lambda h: h.activation(out=enL[:], in_=pL[:], func=AF.Exp, scale=-LWS), reads=[pL], writes=[enL])
            S.op("act", lambda h: h.activation(out=eLx[:], in_=pLx[:], func=AF.Exp, scale=LWS), reads=[pLx], writes=[eLx])
            gm = gam[bi]
            S.op("dve", lambda h: h.tensor_copy(out=gm[:], in_=eL[:, T-1::T]), reads=[eL], writes=[gm])
            p = getpbig()
            mm(p, aup[:], sh["za"][:], aup, sh["za"])
            S.op("act", lambda h: h.activation(out=a_t[:], in_=p[:], func=AF.Sigmoid, bias=col(C_A0)), reads=[p, pcol], writes=[a_t])
            S.op("act", lambda h: h.activation(out=sgd[:], in_=sh["zg"][:], func=AF.Sigmoid), reads=[sh["zg"]], writes=[sgd])
            p2 = getpbig()
            mm(p2, gup[:], sgd[:], gup, sgd)
            S.op("act", lambda h: h.copy(out=g_t[bi][:], in_=p2[:]), reads=[p2], writes=[g_t[bi]])
        steps.append(loras)
        def vres():
            vt = v_t[bi]
            if l == 0:
                S.op("pool", lambda h: h.tensor_copy(out=vt[:], in_=sh["zv"][:]), reads=[sh["zv"]], writes=[vt])
                return
            p = getps()
            for c in range(3):
                mm(p, vdown[:, c, :], sh[f"zva{c}"][:], vdown, sh[f"zva{c}"], start=(c == 0), stop=(c == 2), psl=None) if False else \
                    S.op("pe", lambda h: h.matmul(pbig_v[0:16, :], lhsT=vdown[:, c, :], rhs=sh[f"zva{c}"][:], start=(c == 0), stop=(c == 2)), reads=[vdown, sh[f"zva{c}"]], writes=[pbig_vb])
            S.op("act", lambda h: h.copy(out=vd_sb[:], in_=pbig_v[0:16, :]), reads=[pbig_vb], writes=[vd_sb])
            p3 = getpbig()
            mm(p3, vup[:], vd_sb[:], vup, vd_sb)
            S.op("act", lambda h: h.activation(out=sv[:], in_=p3[:], func=AF.Sigmoid, bias=col(C_V0)), reads=[p3, pcol], writes=[sv])
            tp = gettmp()
            S.op("pool", lambda h: h.tensor_tensor(out=tp[:], in0=sh["zv0"][:], in1=sh["zv"][:], op=ALU.subtract), reads=[sh["zv0"], sh["zv"]], writes=[tp])
            S.op("dve", lambda h: h.tensor_tensor(out=tp[:], in0=tp[:], in1=sv[:], op=ALU.mult), reads=[tp, sv], writes=[tp])
            S.op("pool", lambda h: h.tensor_tensor(out=vt[:], in0=tp[:], in1=sh["zv"][:], op=ALU.add), reads=[tp, sh["zv"]], writes=[vt])
        if l == 1:
            pbig_vb = getpbig(); pbig_v = pbig_vb.t
        steps.append(vres)
        def kstuff():
            S.op("dve", lambda h: h.tensor_scalar(out=kk[:], in0=sh["zk"][:], scalar1=col(C_KK), scalar2=None, op0=ALU.mult), reads=[sh["zk"], pcol], writes=[kk])
            S.op("pool", lambda h: h.tensor_tensor(out=sq[:], in0=kk[:], in1=kk[:], op=ALU.mult), reads=[kk], writes=[sq])
            p = getpbig()
            mm(p, ONESB, sq[:], cst, sq)
            S.op("act", lambda h: h.activation(out=nrm[:], in_=p[:], func=AF.Sqrt), reads=[p], writes=[nrm])
            S.op("dve", lambda h: h.tensor_scalar(out=nrm[:], in0=nrm[:], scalar1=1e-12, scalar2=None, op0=ALU.max), reads=[nrm], writes=[nrm])
            S.op("dve", lambda h: h.reciprocal(out=nrm[:], in_=nrm[:]), reads=[nrm], writes=[nrm])
            S.op("dve", lambda h: h.tensor_tensor(out=kap[:], in0=kk[:], in1=nrm[:], op=ALU.mult), reads=[kk, nrm], writes=[kap])
            tp = gettmp()
            S.op("dve", lambda h: h.tensor_scalar(out=tp[:], in0=a_t[:], scalar1=col(C_KA), scalar2=omk[:, 0:1], op0=ALU.mult, op1=ALU.add), reads=[a_t, pcol, omk], writes=[tp])
            S.op("pool", lambda h: h.tensor_tensor(out=kmod[:], in0=sh["zk"][:], in1=tp[:], op=ALU.mult), reads=[sh["zk"], tp], writes=[kmod])
            S.op("pool", lambda h: h.tensor_tensor(out=b_t[:], in0=kap[:], in1=a_t[:], op=ALU.mult), reads=[kap, a_t], writes=[b_t])
            tp2 = gettmp()
            S.op("dve", lambda h: h.scalar_tensor_tensor(out=tp2[:], in0=sh["zr"][:], scalar=col(C_RK), in1=kmod[:], op0=ALU.mult, op1=ALU.mult), reads=[sh["zr"], pcol, kmod], writes=[tp2])
            p2 = getpbig()
            mm(p2, ONESB, tp2[:], cst, tp2)
            S.op("dve", lambda h: h.tensor_tensor(out=bv[bi][:], in0=p2[:], in1=v_t[bi][:], op=ALU.mult), reads=[p2, v_t[bi]], writes=[bv[bi]])
        steps.append(kstuff)
        def expand():
            for nm, src, ee in [("RT", sh["zr"], eL), ("KpT", kap, eLx), ("KT", kmod, enL), ("BT", b_t, enL), ("VT", v_t[bi], None)]:
                dst = bd[nm][bi]
                for hh in range(2):
                    ps_ = slice(hh*64, (hh+1)*64)
                    o = dst[ps_, :, hh*64:(hh+1)*64]
                    i0 = src[ps_, :].rearrange("p (c t) -> p c t", t=T)
                    eng = "dve" if hh == 0 else "pool"
                    if ee is None:
                        S.op(eng, lambda h: h.tensor_copy(out=o, in_=i0), reads=[src], writes=[dst])
                    else:
                        i1 = ee[ps_, :].rearrange("p (c t) -> p c t", t=T)
                        S.op(eng, lambda h: h.tensor_tensor(out=o, in0=i0, in1=i1, op=ALU.mult), reads=[src, ee], writes=[dst])
        steps.append(expand)
        return steps

    def chunk_par(ti, c):
        bi = ti % NB
        RT = bd["RT"][bi]; KpT = bd["KpT"][bi]; KT = bd["KT"][bi]; BT = bd["BT"][bi]; VT = bd["VT"][bi]
        rt = RT[:, c, :]; kpt = KpT[:, c, :]; kt = KT[:, c, :]; bt = BT[:, c, :]; vt = VT[:, c, :]
        st = {}
        steps = []
        def amats():
            N = g_N(); Q = g_Q(); Aak = g_Aak(); nArb = g_nArb(); Ark = g_Ark()
            for dst, l, ll, r, rr, mask in [(N, bt, BT, kpt, KpT, MSU), (Q, kpt, KpT, bt, BT, MSL), (Aak, kt, KT, kpt, KpT, MSU),
                                            (nArb, bt, BT, rt, RT, MNUI), (Ark, kt, KT, rt, RT, MUI)]:
                p = getps()
                mm(p, l, r, ll, rr)
                S.op("dve", lambda h: h.tensor_tensor(out=dst[:], in0=p[:], in1=mask, op=ALU.mult), reads=[p, cst], writes=[dst])
            st.update(N=N, Q=Q, Aak=Aak, nArb=nArb, Ark=Ark)
        steps.append(amats)
        def transposes():
            nB = g_nB(); Kb = g_Kb(); Vb = g_Vb()
            for dst, src, sb, scale in [(nB, bt, BT, -1.0), (Kb, kt, KT, None), (Vb, vt, VT, None)]:
                p = getps()
                S.op("pe", lambda h: h.transpose(p[:], src, IDENT), reads=[sb, cst], writes=[p])
                evac_copy(dst, p, scale)
            st.update(nB=nB, Kb=Kb, Vb=Vb)
        steps.append(transposes)
        def inv0():
            WT = g_WT()
            S.op("pool", lambda h: h.tensor_tensor(out=WT[:], in0=IDENT, in1=st["N"][:], op=ALU.subtract), reads=[cst, st["N"]], writes=[WT])
            st.update(WT=WT, X=st["N"], XT=st["Q"])
        steps.append(inv0)
        def invj(j):
            def f():
                X = st["X"]; XT = st["XT"]; WT = st["WT"]
                last = (j == 4)
                XTn = g_XT()
                p2 = getps(); mm(p2, X[:], XT[:], X, XT)
                if not last:
                    Xn = g_X()
                    p1 = getps(); mm(p1, XT[:], X[:], XT, X)
                    S.op("act", lambda h: h.copy(out=Xn[:], in_=p1[:]), reads=[p1], writes=[Xn])
                S.op("dve", lambda h: h.tensor_copy(out=XTn[:], in_=p2[:]), reads=[p2], writes=[XTn])
                p3 = getps(); mm(p3, XTn[:], WT[:], XTn, WT)
                WTn = g_WTf() if last else g_WT()
                S.op("dve", lambda h: h.tensor_tensor(out=WTn[:], in0=p3[:], in1=WT[:], op=ALU.add), reads=[p3, WT], writes=[WTn])
                st["XT"] = XTn; st["WT"] = WTn
                if not last:
                    st["X"] = Xn
            return f
        for j in range(5):
            steps.append(invj(j))
        return steps, st

    mcur = [0]
    def chunk_seq(ti, c, st):
        bi = ti % NB
        RT = bd["RT"][bi]; KpT = bd["KpT"][bi]
        rt = RT[:, c, :]; kpt = KpT[:, c, :]
        steps = []
        def s1():
            M0 = Mst[mcur[0] % 2]
            p = getps()
            mm(p, kpt, M0[:], KpT, M0, start=True, stop=False)
            mm(p, st["Aak"][:], st["Vb"][:], st["Aak"], st["Vb"], start=False, stop=True)
            rhs = g_rhs()
            S.op("act", lambda h: h.copy(out=rhs[:], in_=p[:]), reads=[p], writes=[rhs])
            st["rhs"] = rhs
            S.op("pool", lambda h: h.tensor_scalar(out=Mg[:], in0=M0[:], scalar1=gam[bi][:, c:c+1], scalar2=None, op0=ALU.mult), reads=[M0, gam[bi]], writes=[Mg])
        def s2():
            p = getps()
            mm(p, st["WT"][:], st["rhs"][:], st["WT"], st["rhs"])
            U = g_U()
            S.op("dve", lambda h: h.tensor_copy(out=U[:], in_=p[:]), reads=[p], writes=[U])
            st["U"] = U
        def s3():
            M0 = Mst[mcur[0] % 2]; M1 = Mst[(mcur[0] + 1) % 2]
            U = st["U"]
            pm = getps()
            mm(pm, st["Kb"][:], st["Vb"][:], st["Kb"], st["Vb"], start=True, stop=False)
            mm(pm, st["nB"][:], U[:], st["nB"], U, start=False, stop=True)
            S.op("dve", lambda h: h.scalar_tensor_tensor(out=M1[:], in0=pm[:], scalar=gam[bi][:, c:c+1], in1=Mg[:], op0=ALU.mult, op1=ALU.add), reads=[pm, gam[bi], Mg], writes=[M1])
            py = getps()
            mm(py, M0[:], rt, M0, RT, start=True, stop=False)
            mm(py, U[:], st["nArb"][:], U, st["nArb"], start=False, stop=False)
            mm(py, st["Vb"][:], st["Ark"][:], st["Vb"], st["Ark"], start=False, stop=True)
            y = yT[bi]
            S.op("act", lambda h: h.copy(out=y[0:64, c*T:(c+1)*T], in_=py[0:64, 0:64]), reads=[py], writes=[y])
            S.op("act", lambda h: h.copy(out=y[64:128, c*T:(c+1)*T], in_=py[64:128, 64:128]), reads=[py], writes=[y])
            mcur[0] += 1
        return [s1, s2, s3]

    def post_steps(ti):
        bi = ti % NB
        t0 = ti * TT
        def f():
            y = yT[bi]
            p = getpbig()
            mm(p, MEANB, y[:], cst, y)
            yc = gettmp()
            S.op("dve", lambda h: h.tensor_tensor(out=yc[:], in0=y[:], in1=p[:], op=ALU.subtract), reads=[y, p], writes=[yc])
            s2 = gettmp()
            S.op("pool", lambda h: h.tensor_tensor(out=s2[:], in0=yc[:], in1=yc[:], op=ALU.mult), reads=[yc], writes=[s2])
            p2 = getpbig()
            mm(p2, MEANB, s2[:], cst, s2)
            S.op("act", lambda h: h.activation(out=s2[:], in_=p2[:], func=AF.Sqrt, bias=epsc[:, 0:1]), reads=[p2, epsc], writes=[s2])
            S.op("dve", lambda h: h.reciprocal(out=s2[:], in_=s2[:]), reads=[s2], writes=[s2])
            S.op("dve", lambda h: h.tensor_tensor(out=yc[:], in0=yc[:], in1=s2[:], op=ALU.mult), reads=[yc, s2], writes=[yc])
            S.op("dve", lambda h: h.tensor_scalar(out=yc[:], in0=yc[:], scalar1=col(C_LNG), scalar2=col(C_LNB), op0=ALU.mult, op1=ALU.add), reads=[yc, pcol], writes=[yc])
            S.op("pool", lambda h: h.tensor_tensor(out=yc[:], in0=yc[:], in1=bv[bi][:], op=ALU.add), reads=[yc, bv[bi]], writes=[yc])
            o = yo[bi]
            S.op("dve", lambda h: h.tensor_tensor(out=o[:], in0=yc[:], in1=g_t[bi][:], op=ALU.mult), reads=[yc, g_t[bi]], writes=[o])
            S.dma("sp", sP[(ti // NTL)*128:(ti // NTL + 1)*128, (ti % NTL)*TT:(ti % NTL + 1)*TT], o[:], reads=[o], writes=[sP])
        return [f]
    epsc = S.sbuf("epsc", [128, 1])
    S.op("pool", lambda h: h.memset(epsc[:], GN_EPS), writes=[epsc])

    for s in pre_steps(0):
        s()
    LAG = 2
    pend = []
    nextpre = []
    for ti in range(NTILE):
        if ti + 1 < NTILE:
            nextpre = pre_steps(ti + 1)
        else:
            nextpre = []
        for c in range(0, NCH, 2):
            pa_, sta_ = chunk_par(ti, c)
            pb__, stb_ = chunk_par(ti, c + 1)
            psteps = [x_ for pr_ in zip(pa_, pb__) for x_ in pr_]
            seqs = []
            if len(pend) >= LAG:
                for _ in range(2):
                    pti, pc, pst = pend.pop(0)
                    seqs = seqs + chunk_seq(pti, pc, pst)
                    if pc == NCH - 1:
                        seqs = seqs + post_steps(pti)
            qi = 0
            for i in range(len(psteps)):
                psteps[i]()
                while qi < len(seqs) and (qi + 1) * len(psteps) <= (i + 1) * max(1, len(seqs)):
                    seqs[qi](); qi += 1
            while qi < len(seqs):
                seqs[qi](); qi += 1
            pend.append((ti, c, sta_)); pend.append((ti, c + 1, stb_))
            for cc_ in (c, c + 1):
                if nextpre and cc_ < len(nextpre):
                    nextpre[cc_]()
        for s in nextpre[NCH:]:
            s()
    while pend:
        pti, pc, pst = pend.pop(0)
        for s in chunk_seq(pti, pc, pst):
            s()
        if pc == NCH - 1:
            for s in post_steps(pti):
                s()

    allgather_rows(S, sP, G.gP[l], RP, CR_F, m0=0, m1=4)
    S.phase_end()


def phase_C(nc, G, l):
    NT = G.NT; SEQ = 4 * NT; NTL = NT // TT
    NQ = SEQ // QT; NKL = SEQ // 4
    S = Sched(nc)
    for b_ in G.persist:
        b_.reset()
    gAb = G.gAb[l]; gAv = G.gAv[l]; sP = G.sP[l]
    allgather_rows(S, G.sAf[l], G.gAf[l], 1280, CR_F)
    idxCk = S.sbuf("idxCk", [128, 6, 4], I32); idxCv = S.sbuf("idxCv", [128, NKL // 128], I32)
    S.dma("sp", idxCk[:], G.inp["idxCk"].t, reads=[G.inp["idxCk"]], writes=[idxCk])
    S.dma("sp", idxCv[:], G.inp["idxCv"].t, reads=[G.inp["idxCv"]], writes=[idxCv])
    kT = [S.sbuf(f"kT{h}", [96, NKL], BF16) for h in range(6)]
    gkv = gAb.t.rearrange("a (j x) -> (a j) x", j=4)
    for h in range(6):
        for i_ in range(4):
            S.idma(kT[h][:, i_*(NT//4):(i_+1)*(NT//4)], gkv, idxCk[0:96, h, i_:i_+1], reads=[gAb, idxCk], writes=[kT[h]])
    NKT = NKL // 128
    vaug = S.sbuf("vaug", [128, NKT, 6, 65], BF16)
    S.op("pool", lambda hh: hh.memset(vaug[:], 1.0), writes=[vaug])
    vst = S.sbuf("vst", [128, NKT, 384], BF16)
    for n_ in range(NKT):
        S.idma(vst[:, n_, :], gAv.t, idxCv[:, n_:n_+1], reads=[gAv, idxCv], writes=[vst])
    for n in range(NKT):
        S.op("pool", lambda hh: hh.tensor_copy(out=vaug[:, n, :, 0:64], in_=vst[:, n, :].rearrange("p (h c) -> p h c", c=64)), reads=[vst], writes=[vaug])
    mask = S.sbuf("mask", [128, QT], BF16)
    S.dma("sp", mask[:], G.inp["maskC"][:, :], reads=[G.inp["maskC"]], writes=[mask])
    qb = [[S.sbuf(f"q{i}_{h}", [96, QT], BF16) for h in range(6)] for i in range(2)]
    ps = [S.psum(f"ps{i}", [128, QT]) for i in range(4)]
    po = [S.psum(f"po{i}", [128, QT]) for i in range(2)]
    pT = [S.sbuf(f"pT{i}", [128, QT], BF16) for i in range(4)]
    oo = [S.sbuf(f"oo{i}", [65, QT], F32) for i in range(3)]
    steps_ = [(g, h, i) for g in range(NQ) for h in range(6) for i in range(g + 1)]
    oc = [0]
    def emit_qk(s_):
        g, h, i = steps_[s_]
        qs = qb[g % 2]
        if h == 0 and i == 0:
            for hh_ in range(6):
                S.dma("sp", qs[hh_][:], gAb[int(gaddr(CR_B, RAB, g // NTL, hh_*96)):int(gaddr(CR_B, RAB, g // NTL, hh_*96)) + 96, (g % NTL)*QT:(g % NTL + 1)*QT], reads=[gAb], writes=[qs[hh_]])
        p = ps[s_ % 4]
        S.op("pe", lambda hh: hh.matmul(p[:], lhsT=kT[h][:, i*128:(i+1)*128], rhs=qs[h][:], start=True, stop=True), reads=[kT[h], qs[h]], writes=[p])
    def emit_rest(s_):
        g, h, i = steps_[s_]
        p = ps[s_ % 4]; pt = pT[s_ % 4]
        acc = po[(g * 6 + h) % 2]
        S.op("act", lambda hh: hh.activation(out=pt[:], in_=p[:], func=AF.Exp, scale=SCALE), reads=[p], writes=[pt])
        if i == g:
            S.op("dve", lambda hh: hh.tensor_tensor(out=pt[:], in0=pt[:], in1=mask[:], op=ALU.mult), reads=[pt, mask], writes=[pt])
        S.op("pe", lambda hh: hh.matmul(acc[0:65, :], lhsT=vaug[:, i, h, :], rhs=pt[:], start=(i == 0), stop=(i == g)), reads=[vaug, pt], writes=[acc])
        if i == g:
            o = oo[oc[0] % 3]; oc[0] += 1
            S.op("dve", lambda hh: hh.tensor_copy(out=o[:], in_=acc[0:65, :]), reads=[acc], writes=[o])
            S.dma("sp", sP[512 + (g // NTL)*390 + h*65:512 + (g // NTL)*390 + (h+1)*65, (g % NTL)*QT:(g % NTL + 1)*QT], o[:], reads=[o], writes=[sP])
    AHEAD = 2
    for s_ in range(len(steps_) + AHEAD):
        if s_ < len(steps_):
            emit_qk(s_)
        if s_ - AHEAD >= 0:
            emit_rest(s_ - AHEAD)
    S.phase_end()


def phase_D(nc, G, l):
    NT = G.NT; NTL = NT // TT
    moe = (l % 2 == 1)
    G_ = 3 if moe else 4
    F = 3584 if moe else 2816
    NE = 8 if moe else 1
    NF = F // 128
    S = Sched(nc)
    for b_ in G.persist:
        b_.reset()
    gP = G.gP[l]; ycl = G.ycl[l]
    gPv = gP.t.rearrange("a (n c) -> (a n) c", c=TT)
    wo_d = G.inp["w_out%d" % l]; pc_d = G.inp["pcolD%d" % l]; cst_d = G.inp["cstD"]
    wg_d = G.inp["wg%d" % l]; wu_d = G.inp["wu%d" % l]; wd_d = G.inp["wd%d" % l]
    if moe:
        rt_d = G.inp["router"]
    idxDya = S.sbuf("idxDya", [128, 3, NTL], I32); idxDo = S.sbuf("idxDo", [128, 24, NTL], I32)
    S.dma("sp", idxDya[:], G.inp["idxDya"].t, reads=[G.inp["idxDya"]], writes=[idxDya])
    S.dma("sp", idxDo[:], G.inp["idxDo"].t, reads=[G.inp["idxDo"]], writes=[idxDo])
    pc = S.sbuf("pc", [128, 16]); cst = S.sbuf("cst", [128, 2, 128])
    S.dma("sp", pc[:], pc_d[:], reads=[pc_d], writes=[pc])
    S.dma("sp", cst[:], cst_d[:], reads=[cst_d], writes=[cst])
    ONES = cst[:, 0, :]; IDENT = cst[:, 1, :]
    onesb = S.sbuf("onesb", [128, 128], BF16)
    S.op("dve", lambda h: h.tensor_copy(out=onesb[:], in_=ONES), reads=[cst], writes=[onesb])
    epsc = S.sbuf("epsc", [128, 1]); S.op("pool", lambda h: h.memset(epsc[:], EPS), writes=[epsc])
    NPS = 8
    slots = [S.psum(f"slot{i}", [128, TT]) for i in range(NPS)]
    psc = [0]
    def getps():
        psc[0] += 1
        return slots[psc[0] % NPS]
    xT = [S.sbuf(f"xT{k}", [128, NT], F32) for k in range(8)]
    for k in range(8):
        S.dma("sp", xT[k][:], G.xsrc[l][k*128:(k+1)*128, :], reads=[G.xsrc[l]], writes=[xT[k]])
    hT = [S.sbuf(f"hT{k}", [128, NT], BF16) for k in range(8)]
    stg = []
    stc = [0]
    def getstg():
        stc[0] += 1
        return stg[stc[0] % 2]
    wgb = [S.sbuf(f"wgb{i}", [128, 8, G_*128], BF16) for i in range(2)]
    wub = [S.sbuf(f"wub{i}", [128, 8, G_*128], BF16) for i in range(2)]
    wdb = [S.sbuf(f"wdb{i}", [128, G_, D], BF16) for i in range(2)]
    _wob = [wgb[0], wgb[1], wub[0], wub[1]]
    def wo_view(i):
        b = _wob[i // G_]
        return b, b.t[:].rearrange("p k f -> p (k f)")[:, (i % G_) * D:(i % G_ + 1) * D]
    krows = [(i*128, 128) for i in range(3)] + [(384 + i*64, 64) for i in range(6)] + [(768 + i*128, 128) for i in range(2)]
    for i, (r0, n) in enumerate(krows):
        wob, wov = wo_view(i)
        S.dma("pool", wov[0:n, :], wo_d[r0:r0+n, :], reads=[wo_d], writes=[wob])
    mixb = [S.sbuf(f"mixb{i}", [128, TT], BF16) for i in range(11)]
    oh = [S.sbuf(f"oh{i}", [65, TT], F32) for i in range(6)]
    sqb = [S.sbuf(f"sqb{i}", [128, TT], BF16) for i in range(2)]
    rs = S.sbuf("rs", [128, TT], F32)
    ldt = [S.sbuf(f"ldt{i}", [128, TT], F32) for i in range(3)]
    ldc = [0]
    def getld():
        ldc[0] += 1
        return ldt[ldc[0] % 3]
    if moe:
        rtr = S.sbuf("rtr", [128, 8, 8], F32)
        S.dma("sp", rtr[:], rt_d.t.rearrange("(k p) e -> p k e", p=128), reads=[rt_d], writes=[rtr])
        hf = [S.sbuf(f"hf{k}", [128, TT], F32) for k in range(2)]
        gbc2 = [S.sbuf(f"gbc{e}", [128, NT], BF16) for e in range(2)]
        gfall = S.sbuf("gfall", [128, NT // 128, 8], F32)
        lg = S.sbuf("lg", [128, 8], F32); m1 = S.sbuf("m1", [128, 1], F32); m2 = S.sbuf("m2", [128, 1], F32)
        eq1 = S.sbuf("eq1", [128, 8], F32); eq2 = S.sbuf("eq2", [128, 8], F32); msk = S.sbuf("msk", [128, 8], F32)
        g1 = S.sbuf("g1", [128, 1], F32); g2 = S.sbuf("g2", [128, 1], F32); gf = S.sbuf("gf", [128, 8], F32)
        gexp = S.sbuf("gexp", [128, 128], F32)

    for t in range(NTL):
        sl = slice(t*TT, (t+1)*TT)
        for i in range(3):
            st = getld()
            S.idma(st[:], gPv, idxDya[:, i, t:t+1], reads=[gP, idxDya], writes=[st])
            S.op("act", lambda h: h.copy(out=mixb[i][:], in_=st[:]), reads=[st], writes=[mixb[i]])
        for i in range(2):
            st = getld()
            S.dma("sp", st[:], ycl[i*128:(i+1)*128, sl], reads=[ycl], writes=[st])
            S.op("act", lambda h: h.copy(out=mixb[9+i][:], in_=st[:]), reads=[st], writes=[mixb[9+i]])
        pss = getps()
        for hd in range(6):
            o = oh[hd]
            S.idma(o[:], gPv, idxDo[0:65, hd, t:t+1], reads=[gP, idxDo], writes=[o])
            for j in range(1, 4):
                st = getld()
                S.idma(st[0:65, :], gPv, idxDo[0:65, j*6 + hd, t:t+1], reads=[gP, idxDo], writes=[st])
                S.op("pool", lambda h: h.tensor_tensor(out=o[:], in0=o[:], in1=st[0:65, :], op=ALU.add), reads=[o, st], writes=[o])
            S.op("dve", lambda h: h.reciprocal(out=o[64:65, :], in_=o[64:65, :]), reads=[o], writes=[o])
            pb = getps()
            S.op("pe", lambda h: h.matmul(pb[0:64, :], lhsT=ONES[64:65, 0:64], rhs=o[64:65, :], start=True, stop=True), reads=[cst, o], writes=[pb])
            S.op("dve", lambda h: h.tensor_tensor(out=o[0:64, :], in0=o[0:64, :], in1=pb[0:64, :], op=ALU.mult), reads=[o, pb], writes=[o])
            s = sqb[hd % 2]
            S.op("act", lambda h: h.activation(out=s[0:64, :], in_=o[0:64, :], func=AF.Square), reads=[o], writes=[s])
            S.op("pe", lambda h: h.matmul(pss[:], lhsT=onesb[0:64, :], rhs=s[0:64, :], start=(hd == 0), stop=(hd == 5)), reads=[onesb, s], writes=[pss])
        S.op("act", lambda h: h.activation(out=rs[:], in_=pss[:], func=AF.Sqrt, scale=1.0/384, bias=epsc[:]), reads=[pss, epsc], writes=[rs])
        S.op("dve", lambda h: h.reciprocal(out=rs[:], in_=rs[:]), reads=[rs], writes=[rs])
        for hd in range(6):
            S.op("dve", lambda h: h.scalar_tensor_tensor(out=mixb[3+hd][0:64, :], in0=oh[hd][0:64, :], scalar=pc[0:64, 8+hd:9+hd], in1=rs[0:64, :], op0=ALU.mult, op1=ALU.mult), reads=[oh[hd], pc, rs], writes=[mixb[3+hd]])
        for m in range(8):
            p = getps()
            for i, (r0, n) in enumerate(krows):
                wob, wov = wo_view(i)
                S.op("pe", lambda h: h.matmul(p[:], lhsT=wov[0:n, m*128:(m+1)*128], rhs=mixb[i][0:n, :], start=(i == 0), stop=(i == 10)), reads=[wob, mixb[i]], writes=[p])
            S.op("dve", lambda h: h.tensor_tensor(out=xT[m][:, sl], in0=xT[m][:, sl], in1=p[:], op=ALU.add), reads=[xT[m], p], writes=[xT[m]])
        p = getps()
        for k in range(8):
            s = sqb[k % 2]
            S.op("act", lambda h: h.activation(out=s[:], in_=xT[k][:, sl], func=AF.Square), reads=[xT[k]], writes=[s])
            S.op("pe", lambda h: h.matmul(p[:], lhsT=onesb[:], rhs=s[:], start=(k == 0), stop=(k == 7)), reads=[onesb, s], writes=[p])
        S.op("act", lambda h: h.activation(out=rs[:], in_=p[:], func=AF.Sqrt, scale=1.0/D, bias=epsc[:]), reads=[p, epsc], writes=[rs])
        S.op("dve", lambda h: h.reciprocal(out=rs[:], in_=rs[:]), reads=[rs], writes=[rs])
        for k in range(8):
            S.op("dve", lambda h: h.scalar_tensor_tensor(out=hT[k][:, sl], in0=xT[k][:, sl], scalar=pc[:, k:k+1], in1=rs[:], op0=ALU.mult, op1=ALU.mult), reads=[xT[k], pc, rs], writes=[hT[k]])
        if moe:
            pls = [getps() for j in range(TT // 128)]
            for k in range(8):
                hk = hf[k % 2]
                S.op("dve", lambda h: h.scalar_tensor_tensor(out=hk[:], in0=xT[k][:, sl], scalar=pc[:, k:k+1], in1=rs[:], op0=ALU.mult, op1=ALU.mult), reads=[xT[k], pc, rs], writes=[hk])
                for j in range(TT // 128):
                    S.op("pe", lambda h: h.matmul(pls[j][:, 0:8], lhsT=hk[:, j*128:(j+1)*128], rhs=rtr[:, k, :], start=(k == 0), stop=(k == 7)), reads=[hk, rtr], writes=[pls[j]])
            for j in range(TT // 128):
                pl = pls[j]
                S.op("dve", lambda h: h.tensor_copy(out=lg[:], in_=pl[:, 0:8]), reads=[pl], writes=[lg])
                S.op("dve", lambda h: h.reduce_max(out=m1[:], in_=lg[:], axis=AX.X), reads=[lg], writes=[m1])
                S.op("dve", lambda h: h.tensor_scalar(out=eq1[:], in0=lg[:], scalar1=m1[:, 0:1], scalar2=None, op0=ALU.is_equal), reads=[lg, m1], writes=[eq1])
                S.op("dve", lambda h: h.scalar_tensor_tensor(out=msk[:], in0=eq1[:], scalar=-1e30, in1=lg[:], op0=ALU.mult, op1=ALU.add), reads=[eq1, lg], writes=[msk])
                S.op("dve", lambda h: h.reduce_max(out=m2[:], in_=msk[:], axis=AX.X), reads=[msk], writes=[m2])
                S.op("dve", lambda h: h.tensor_scalar(out=eq2[:], in0=msk[:], scalar1=m2[:, 0:1], scalar2=None, op0=ALU.is_equal), reads=[msk, m2], writes=[eq2])
                S.op("dve", lambda h: h.tensor_tensor(out=g1[:], in0=m1[:], in1=m2[:], op=ALU.subtract), reads=[m1, m2], writes=[g1])
                S.op("act", lambda h: h.activation(out=g1[:], in_=g1[:], func=AF.Sigmoid), reads=[g1], writes=[g1])
                S.op("dve", lambda h: h.tensor_scalar(out=g2[:], in0=g1[:], scalar1=-1.0, scalar2=1.0, op0=ALU.mult, op1=ALU.add), reads=[g1], writes=[g2])
                S.op("dve", lambda h: h.tensor_scalar(out=gf[:], in0=eq1[:], scalar1=g1[:, 0:1], scalar2=None, op0=ALU.mult), reads=[eq1, g1], writes=[gf])
                S.op("dve", lambda h: h.scalar_tensor_tensor(out=gf[:], in0=eq2[:], scalar=g2[:, 0:1], in1=gf[:], op0=ALU.mult, op1=ALU.add), reads=[eq2, g2, gf], writes=[gf])
                S.op("dve", lambda h: h.tensor_copy(out=gfall[:, t * (TT // 128) + j, :], in_=gf[:]), reads=[gf], writes=[gfall])

    act = [S.sbuf(f"act{i}", [128, NT], BF16) for i in range(G_)]
    sg = [S.sbuf(f"sg{i}", [128, TT], BF16) for i in range(3)]
    sgc = 0; gi = 0
    groups = []
    f0 = 0
    while f0 < NF:
        n = min(G_, NF - f0); groups.append((f0, n)); f0 += n
    for e in range(NE):
        if moe:
            gbc_e = gbc2[e % 2]
            for jj in range(NT // 128):
                S.op("dve", lambda h: h.tensor_scalar(out=gexp[:], in0=ONES, scalar1=gfall[:, jj, e:e+1], scalar2=None, op0=ALU.mult), reads=[cst, gfall], writes=[gexp])
                pg = getps()
                S.op("pe", lambda h: h.matmul(pg[:, 0:128], lhsT=gexp[:], rhs=IDENT, start=True, stop=True), reads=[gexp, cst], writes=[pg])
                S.op("act", lambda h: h.copy(out=gbc_e[:, jj*128:(jj+1)*128], in_=pg[:, 0:128]), reads=[pg], writes=[gbc_e])
        for (f0, n) in groups:
            bi = gi % 2; gi += 1
            wg_, wu_, wd_ = wgb[bi], wub[bi], wdb[bi]
            for (src, dst) in [(wg_d, wg_), (wu_d, wu_)]:
                for kh in range(2):
                    S.dma("pool", dst[:, kh*4:(kh+1)*4, 0:n*128], src.t[e, kh*512:(kh+1)*512, f0*128:(f0+n)*128].rearrange("(k p) f -> p k f", p=128), reads=[src], writes=[dst])
            S.dma("pool", wd_[:, 0:n, :], wd_d.t[e, f0*128:(f0+n)*128, :].rearrange("(i p) d -> p i d", p=128), reads=[wd_d], writes=[wd_])
            for i in range(n):
                for t in range(NTL):
                    sl = slice(t*TT, (t+1)*TT)
                    pg = getps(); pu = getps()
                    for k in range(8):
                        S.op("pe", lambda h: h.matmul(pg[:], lhsT=wg_[:, k, i*128:(i+1)*128], rhs=hT[k][:, sl], start=(k == 0), stop=(k == 7)), reads=[wg_, hT[k]], writes=[pg])
                    for k in range(8):
                        S.op("pe", lambda h: h.matmul(pu[:], lhsT=wu_[:, k, i*128:(i+1)*128], rhs=hT[k][:, sl], start=(k == 0), stop=(k == 7)), reads=[wu_, hT[k]], writes=[pu])
                    s = sg[sgc % 3]; sgc += 1
                    S.op("act", lambda h: h.activation(out=s[:], in_=pg[:], func=AF.Silu), reads=[pg], writes=[s])
                    if moe:
                        S.op("dve", lambda h: h.tensor_tensor(out=s[:], in0=s[:], in1=gbc_e[:, sl], op=ALU.mult), reads=[s, gbc_e], writes=[s])
                    S.op("dve", lambda h: h.tensor_tensor(out=act[i][:, sl], in0=s[:], in1=pu[:], op=ALU.mult), reads=[s, pu], writes=[act[i]])
            for m in range(8):
                for t in range(NTL):
                    sl = slice(t*TT, (t+1)*TT)
                    p = getps()
                    for i in range(n):
                        S.op("pe", lambda h: h.matmul(p[:], lhsT=wd_[:, i, m*128:(m+1)*128], rhs=act[i][:, sl], start=(i == 0), stop=(i == n-1)), reads=[wd_, act[i]], writes=[p])
                    S.op("dve", lambda h: h.tensor_tensor(out=xT[m][:, sl], in0=xT[m][:, sl], in1=p[:], op=ALU.add), reads=[xT[m], p], writes=[xT[m]])
    for k in range(8):
        S.dma("sp", G.xdst[l][k*128:(k+1)*128, :], xT[k][:], reads=[xT[k]], writes=[G.xdst[l]])

    S.phase_end(last=(l == 1))

def build_fused(NT=2048, stop_after=99):
    SEQ = 4 * NT; NTILE = SEQ // TT; NTL = NT // TT; NKT = (SEQ // 4) // 128
    nc = bass.Bass("TRN2", target_bir_lowering=False)
    G = NS(); G.NT = NT
    G.inp = {}
    def ein(name, shape, dt=F32):
        t = nc.dram_tensor(name, list(shape), dt, kind="ExternalInput")
        b = Buf(None, t.ap(), "dram"); G.inp[name] = b
        return b
    G.xT_d = ein("xT", [D, NT]); ein("pos", [1, NT], I32)
    ein("cstA", [128, 6, 128]); ein("cstB", [128, 8, 128]); ein("cstD", [128, 2, 128]); ein("maskC", [128, QT], BF16)
    ein("idxB", [128, 3, NTILE], I32); ein("idxBh", [128, 3, NTILE], I32); ein("idxCk", [128, 6, 4], I32)
    ein("idxCv", [128, NKT], I32); ein("idxDya", [128, 3, NTL], I32); ein("idxDo", [128, 24, NTL], I32)
    for l in range(2):
        ein("w_in%d" % l, [D, INC]); ein("pcolA%d" % l, [128, 32]); ein("wuq%d" % l, [256, 576]); ein("wukv%d" % l, [128, 768])
        ein("ws%d" % l, [4, 128, 128]); ein("bs%d" % l, [1, 512])
        ein("pcolB%d" % l, [128, 16]); ein("plo%d" % l, [64, 4]); ein("wup%d" % l, [32, 128]); ein("aup%d" % l, [32, 128])
        ein("gup%d" % l, [64, 128]); ein("w0row%d" % l, [1, 128]); ein("w_out%d" % l, [D, D]); ein("pcolD%d" % l, [128, 16])
    ein("vdown", [128, 3, 16]); ein("vup", [16, 128])
    ein("wg0", [1, D, 2816]); ein("wu0", [1, D, 2816]); ein("wd0", [1, 2816, D])
    ein("wg1", [8, D, 3584]); ein("wu1", [8, D, 3584]); ein("wd1", [8, 3584, D]); ein("router", [D, 8])
    xo = nc.dram_tensor("xoT", [D, NT], F32, kind="ExternalOutput")
    G.xo_d = Buf(None, xo.ap(), "dram")
    def idram(name, shape, dt=F32):
        t = nc.dram_tensor(name, list(shape), dt, kind="Internal")
        return Buf(None, t.ap(), "dram")
    G.sAf = [idram("sAf%d" % l, [1280, NT]) for l in range(2)]; G.gAf = [idram("gAf%d" % l, [4 * 1280, NT]) for l in range(2)]
    G.sAb = [idram("sAb%d" % l, [RAB, NT], BF16) for l in range(2)]; G.gAb = [idram("gAb%d" % l, [4 * RAB, NT], BF16) for l in range(2)]
    G.sAv = [idram("sAv%d" % l, [NT, 384], BF16) for l in range(2)]; G.gAv = [idram("gAv%d" % l, [4 * NT, 384], BF16) for l in range(2)]
    G.ycl = [idram("ycl%d" % l, [256, NT]) for l in range(2)]
    G.sP = [idram("sP%d" % l, [RP, NT]) for l in range(2)]; G.gP = [idram("gP%d" % l, [4 * RP, NT]) for l in range(2)]
    es = contextlib.ExitStack()
    G.xsp = idram("xsp", [D, NT])
    G.xsrc = [G.xT_d, G.xsp]; G.xdst = [G.xsp, G.xo_d]
    G.persist = list(G.inp.values()) + [G.xo_d, G.xsp] + G.sAf + G.gAf + G.sAb + G.gAb + G.sAv + G.gAv + G.ycl + G.sP + G.gP
    k_ = 0
    for l in range(2):
        for ph in (phase_A, phase_C, phase_B, phase_D):
            if k_ < stop_after:
                ph(nc, G, l)
            k_ += 1
    es.close()
    return nc

def fused_inputs(d, c, NT=2048):
    SEQ = 4 * NT; NTILE = SEQ // TT; NTL = NT // TT; NKT = (SEQ // 4) // 128; MS = NT // 512
    b, r = c // 4, c % 4
    p = r if r < 3 else 0; j = r; q = r
    m = {}
    x = np.asarray(d["x"])[:, :SEQ]
    m["xT"] = np.ascontiguousarray(x[b, q*NT:(q+1)*NT].T)
    m["pos"] = np.ascontiguousarray(np.asarray(d["positions"])[b:b+1, q*NT:(q+1)*NT]).astype(np.int32)
    m["cstA"] = a2_consts(); m["cstB"] = rwkv_consts(); m["cstD"] = d_consts(); m["maskC"] = np.ascontiguousarray(masks_C()[j])
    pp = np.arange(128)
    iB = np.zeros((128, 3, NTILE), np.int32); iBh = np.zeros((128, 3, NTILE), np.int32)
    for ti in range(NTILE):
        i, n = ti // NTL, ti % NTL
        for kind in range(3):
            row = kind * 384 + p * 128 + pp
            iB[:, kind, ti] = gaddr(CR_F, 1280, i, row) * NTL + n
            if ti > 0:
                if n > 0:
                    iBh[:, kind, ti] = gaddr(CR_F, 1280, i, row) * NT + n * TT - 1
                else:
                    iBh[:, kind, ti] = gaddr(CR_F, 1280, i - 1, row) * NT + NT - 1
    m["idxB"] = iB; m["idxBh"] = iBh
    iCk = np.zeros((128, 6, 4), np.int32)
    for h in range(6):
        for i in range(4):
            iCk[:96, h, i] = gaddr(CR_B, RAB, i, 576 + h * 96 + np.arange(96)) * 4 + j
    m["idxCk"] = iCk
    iCv = np.zeros((128, NKT), np.int32)
    for i in range(4):
        for ms in range(MS):
            iCv[:, i * MS + ms] = gaddr(min(CR_V, NT), NT, i, j * (NT // 4) + ms * 128 + pp)
    m["idxCv"] = iCv
    iDy = np.zeros((128, 3, NTL), np.int32); iDo = np.zeros((128, 24, NTL), np.int32)
    for t in range(NTL):
        for pr in range(3):
            iDy[:, pr, t] = gaddr(CR_F, RP, pr, q * 128 + pp) * NTL + t
        for jj in range(4):
            for h in range(6):
                iDo[:65, jj * 6 + h, t] = gaddr(CR_F, RP, jj, 512 + q * 390 + h * 65 + np.arange(65)) * NTL + t
    m["idxDya"] = iDy; m["idxDo"] = iDo
    for l in range(2):
        pc = np.zeros((128, 32), np.float32)
        pc[:, 0:8] = d["mix_norm_g"][l].reshape(8, 128).T
        pc[:, 8:10] = d["b_q_norm_g"][l].reshape(2, 128).T
        pc[:, 10] = d["b_kv_norm_g"][l]
        pc[0:96, 11] = d["b_q_head_g"][l]; pc[0:96, 12] = d["b_k_head_g"][l]
        pc[:, 13:15] = d["c_ln_g"][l].reshape(2, 128).T; pc[:, 15:17] = d["c_ln_b"][l].reshape(2, 128).T
        pc[:, 17:19] = d["c_out_g"][l].reshape(2, 128).T
        m["w_in%d" % l] = d["w_in"][l]; m["pcolA%d" % l] = pc; m["wuq%d" % l] = d["b_w_uq"][l]; m["wukv%d" % l] = d["b_w_ukv"][l]
        m["ws%d" % l] = d["c_w_s"][l]; m["bs%d" % l] = np.ascontiguousarray(d["c_b_s"][l].reshape(1, 512))
        mu = d["shift_mu"][l]; sl = slice(p * 128, (p + 1) * 128)
        pb = np.zeros((128, 16), np.float32)
        pb[:, 0] = mu[0:384][sl]; pb[:, 1] = mu[384:768][sl]; pb[:, 2] = mu[768:1152][sl]
        pb[:, 3] = d["a_w0"][l][sl]; pb[:, 4] = d["a_a0"][l][sl]; pb[:, 5] = d["a_k_k"][l][sl]; pb[:, 6] = d["a_k_a"][l][sl]
        pb[:, 7] = d["a_r_k"][l].reshape(-1)[sl]; pb[:, 8] = d["a_ln_g"][l][sl]; pb[:, 9] = d["a_ln_b"][l][sl]
        if l == 1:
            pb[:, 10] = d["a_v0"][0][sl]; pb[:, 11] = d["shift_mu"][0][768:1152][sl]
            for cc in range(3):
                pb[:, 12 + cc] = mu[768 + cc * 128:768 + (cc + 1) * 128]
        m["pcolB%d" % l] = pb
        pl = np.zeros((64, 4), np.float32)
        pl[0:32, 0] = mu[1152:1184]; pl[0:32, 1] = mu[1184:1216]; pl[0:64, 2] = mu[1216:1280]
        m["plo%d" % l] = pl
        m["wup%d" % l] = np.ascontiguousarray(d["a_w_up"][l][:, sl]); m["aup%d" % l] = np.ascontiguousarray(d["a_a_up"][l][:, sl])
        m["gup%d" % l] = np.ascontiguousarray(d["a_g_up"][l][:, sl]); m["w0row%d" % l] = np.ascontiguousarray(d["a_w0"][l][sl][None, :])
        pd = np.zeros((128, 16), np.float32)
        pd[:, 0:8] = d["ffn_norm_g"][l].reshape(8, 128).T
        pd[0:64, 8:14] = d["b_out_g"][l].reshape(6, 64).T
        m["w_out%d" % l] = d["w_out"][l]; m["pcolD%d" % l] = pd
    m["vdown"] = np.ascontiguousarray(d["a_v_down"][0].reshape(3, 128, 16).transpose(1, 0, 2))
    m["vup"] = np.ascontiguousarray(d["a_v_up"][0][:, p * 128:(p + 1) * 128])
    m["wg0"] = d["dense_w_gate"]; m["wu0"] = d["dense_w_up"]; m["wd0"] = d["dense_w_down"]
    m["wg1"] = d["moe_w_gate"][0]; m["wu1"] = d["moe_w_up"][0]; m["wd1"] = d["moe_w_down"][0]; m["router"] = d["moe_router"][0]
    return {k: np.ascontiguousarray(v) for k, v in m.items()}


_NC = []
def kernel(**inputs):
    d = {k: np.asarray(v) for k, v in inputs.items()}
    if not _NC:
        _NC.append(build_fused(2048))
    in_maps = [fused_inputs(d, c, 2048) for c in range(8)]
    res = run_bass_kernel_spmd(_NC[0], in_maps, core_ids=list(range(8)))
    outs = [np.asarray(res.results[c]["xoT"]) for c in range(8)]
    out = np.stack([o.T for o in outs]).reshape(2, 8192, 1024)
    return np.ascontiguousarray(out.astype(np.float32))
```

```python
import math, time, sys, contextlib
import numpy as np
import ml_dtypes
from concourse.bass_utils import run_bass_kernel_spmd
import contextlib
import numpy as np
import concourse.bass as bass
import concourse.mybir as mybir

F32 = mybir.dt.float32
BF16 = mybir.dt.bfloat16
I32 = mybir.dt.int32
AF = mybir.ActivationFunctionType
ALU = mybir.AluOpType
AX = mybir.AxisListType

EPOCH = 30000


class Buf:
    _n = 0

    def __init__(self, S, t, kind):
        self.S = S
        self.t = t
        self.kind = kind
        Buf._n += 1
        self.id = Buf._n
        self.lw = {}
        self.lr = {}
        self.dsem = None
        self.dcnt = 0

    def __getitem__(self, idx):
        return self.t[idx]

    def view(self, ap):
        b = Buf(self.S, ap, self.kind)
        b.lw = self.lw; b.lr = self.lr; b.id = self.id
        b.parent = self
        return b

    def reset(self):
        self.lw.clear(); self.lr.clear(); self.dsem = None; self.dcnt = 0


class Eng:
    def __init__(self, S, name, h):
        self.S = S
        self.name = name
        self.h = h
        self.seq = 0
        self.known = {}

    def cur_event(self):
        ep = (self.seq - 1) // EPOCH
        return (self.name, ep), (self.seq - 1) % EPOCH + 1


class Sched:
    _phase = 0

    def __init__(self, nc):
        self.nc = nc
        Sched._phase += 1
        self.pid = Sched._phase
        self.es = contextlib.ExitStack()
        self.E = {
            "pe": Eng(self, "pe", nc.tensor),
            "act": Eng(self, "act", nc.scalar),
            "dve": Eng(self, "dve", nc.vector),
            "pool": Eng(self, "pool", nc.gpsimd),
            "sp": Eng(self, "sp", nc.sync),
        }
        self.semtab = {}
        self.semh = []
        self.final = {}
        self.dram_out = []
        self.ncc = 0

    def sem(self, name):
        h = self.nc.alloc_semaphore("p%d_%s" % (self.pid, name))
        self.semh.append(h)
        return h

    def sbuf(self, name, shape, dt=F32):
        t = self.es.enter_context(self.nc.sbuf_tensor("sb%d_%s" % (self.pid, name), list(shape), dt))
        return Buf(self, t, "sbuf")

    def psum(self, name, shape, dt=F32):
        t = self.es.enter_context(self.nc.psum_tensor("ps%d_%s" % (self.pid, name), list(shape), dt))
        return Buf(self, t, "psum")

    def dram(self, name, shape, dt=F32, kind="Internal"):
        t = self.nc.dram_tensor(name, list(shape), dt, kind=kind)
        b = Buf(self, t.ap(), "dram")
        b.io = kind
        if kind == "ExternalOutput":
            self.dram_out.append(b)
        return b

    def _semfor(self, key):
        if key not in self.semtab:
            self.semtab[key] = self.sem("s_%s_%s" % (str(key[0]), str(key[1])))
        return self.semtab[key]

    def _wait(self, eng, key, val):
        if eng.known.get(key, 0) >= val:
            return
        eng.h.wait_ge(self._semfor(key), val)
        eng.known[key] = val
        if key[0] in self.E:
            for ep in range(key[1]):
                eng.known[(key[0], ep)] = EPOCH

    def _deps(self, eng, reads, writes, skipkey=None):
        for b in reads:
            for k, v in b.lw.items():
                if (k[0] == "pe" and eng.name == "pe"):
                    continue
                self._wait(eng, k, v)
        for b in writes:
            for k, v in b.lw.items():
                if (k[0] == "pe" and eng.name == "pe") or k == skipkey:
                    continue
                self._wait(eng, k, v)
            for k, v in b.lr.items():
                if (k[0] == "pe" and eng.name == "pe"):
                    continue
                self._wait(eng, k, v)

    def _commit(self, key, val, reads, writes):
        if self.final.get(key, 0) < val:
            self.final[key] = val
        for b in reads:
            if b.lr.get(key, 0) < val:
                b.lr[key] = val
        for b in writes:
            if b.kind == "dram":
                b.lw[key] = val
                continue
            b.lw.clear(); b.lw[key] = val
            b.lr.clear()

    def op(self, en, fn, reads=(), writes=()):
        eng = self.E[en]
        reads = [b for b in reads if b is not None]
        writes = [b for b in writes if b is not None]
        self._deps(eng, reads, writes)
        ins = fn(eng.h)
        eng.seq += 1
        key, val = eng.cur_event()
        ins.then_inc(self._semfor(key), 1)
        self._commit(key, val, reads, writes)
        return ins

    def _dma_common(self, qn, reads, writes, issue):
        eng = self.E[qn]
        reads = [b for b in reads if b is not None]
        writes = [b for b in writes if b is not None]
        cand = [b for b in writes if b.kind != "dram"] + [b for b in reads if b.kind != "dram"]
        owner = cand[0] if cand else (writes[0] if writes else reads[0])
        if owner.dsem is None:
            owner.dsem = {}
        key = ("ds" if qn == "pool" else "d", owner.id)
        owner.dsem[key] = owner.dsem.get(key, 0) + 16
        self._deps(eng, reads, [b for b in writes if b.kind != "dram"], skipkey=key)
        val = owner.dsem[key]
        ins = issue(eng.h)
        ins.then_inc(self._semfor(key), 16)
        self._commit(key, val, reads, writes)
        return ins

    def dma(self, qn, out, in_, reads=(), writes=(), **kw):
        return self._dma_common(qn, reads, writes, lambda h: h.dma_start(out=out, in_=in_, **kw))

    def idma(self, out, in_view, idx_ap, reads=(), writes=()):
        return self._dma_common("pool", reads, writes, lambda h: h.indirect_dma_start(
            out=out, out_offset=None, in_=in_view, in_offset=bass.IndirectOffsetOnAxis(ap=idx_ap, axis=0)))

    def allgather(self, in_buf, out_buf, groups):
        eng = self.E["pool"]
        for k, v in in_buf.lw.items():
            self._wait(eng, k, v)
        for k, v in list(out_buf.lw.items()) + list(out_buf.lr.items()):
            if k[0] == "cc":
                continue
            self._wait(eng, k, v)
        self.ncc += 1
        key = ("cc", self.ncc)
        ins = self.nc.gpsimd.collective_compute("AllGather", ALU.bypass, replica_groups=groups, ins=[in_buf.t], outs=[out_buf.t])
        ins.then_inc(self._semfor(key), 1)
        self._commit(key, 1, [in_buf], [])
        out_buf.lw[key] = 1
        return ins

    def drain(self):
        sp = self.E["sp"]
        for k, v in list(self.final.items()):
            self._wait(sp, k, v)

    def phase_end(self, last=False):
        self.drain()
        self.nc.all_engine_barrier()
        if not last:
            self.nc.clear_and_free_semaphores(self.semh)
            self.nc.all_engine_barrier()
        self.es.close()
BF = ml_dtypes.bfloat16
D = 1024; INC = 2208; EPS = 1e-6; TT = 512
CH = [(s, 128) for s in range(0, 1664, 128)] + [(1664, 32)] + [(s, 128) for s in range(1696, 2208, 128)]
TWO_PI = 2.0 * math.pi
T = 64; NCH = TT // T
LWS = -0.6065306597126334
GN_EPS = 64e-5
QT = 512
SCALE = 1.0 / math.sqrt(96.0)
GROUPS = [[0, 1, 2, 3], [4, 5, 6, 7]]
RAB = 1152
RP = 4 * 128 + 4 * 390

class NS:
    pass

CR_F = 128
CR_B = 192
CR_V = 1024

def gaddr(cr, R, i, r):
    m = r // cr
    nr = np.minimum(cr, R - m * cr)
    return 4 * cr * m + i * nr + (r - m * cr)

def allgather_rows(S, send, gath, R, cr, m0=0, m1=None):
    m = m0
    while m * cr < R and (m1 is None or m < m1):
        nr = min(cr, R - m * cr)
        S.allgather(send.view(send.t[m*cr:m*cr+nr, :]), gath.view(gath.t[4*cr*m:4*cr*m + 4*nr, :]), GROUPS)
        m += 1

def a2_consts():
    c = np.zeros((6, 128, 128), np.float32)
    c[0] = 1.0
    c[1] = np.eye(128)
    P = np.zeros((96, 96), np.float32)
    for i in range(16):
        P[64 + i, 80 + i] = -1.0
        P[80 + i, 64 + i] = 1.0
    c[2][:96, :96] = P.T
    s = np.arange(128)[:, None]; t = np.arange(128)[None, :]
    c[3] = (s <= t)
    inv = (10000.0 ** (-np.arange(0, 32, 2, dtype=np.float32) / 32)).astype(np.float32)
    c[4][64:80, 0] = inv; c[4][80:96, 0] = inv
    c[4][0:64, 1] = 1.0
    return np.ascontiguousarray(c.transpose(1, 0, 2))


def rwkv_consts():
    c = np.zeros((8, 128, 128), np.float32)
    bd = np.zeros((128, 128), np.float32); bd[:64, :64] = 1; bd[64:, 64:] = 1
    s = np.arange(128)[:, None]; t = np.arange(128)[None, :]
    c[0] = bd
    c[1] = bd * (s <= t)
    c[2] = bd * (s < t)
    c[3] = bd * (s > t)
    c[4] = bd * (s <= t)
    c[5] = -c[4]
    c[6] = np.eye(128)
    c[7] = bd / 64.0
    return np.ascontiguousarray(c.transpose(1, 0, 2))


def masks_C():
    m = np.zeros((4, 128, QT), np.float32)
    for d in range(4):
        kk = d * 128 + np.arange(128)[:, None]; qq = np.arange(QT)[None, :]
        m[d] = (kk // 64 <= qq // 64)
    return m.astype(BF)


def d_consts():
    c = np.zeros((2, 128, 128), np.float32)
    c[0] = 1.0; c[1] = np.eye(128)
    return np.ascontiguousarray(c.transpose(1, 0, 2))


def phase_A(nc, G, l):
    NT = G.NT
    S = Sched(nc)
    for b_ in G.persist:
        b_.reset()
    xT_d = G.xT_d; w_d = G.inp["w_in%d" % l]; pc_d = G.inp["pcolA%d" % l]; pos_d = G.inp["pos"]
    wuq_d = G.inp["wuq%d" % l]; wukv_d = G.inp["wukv%d" % l]; ws_d = G.inp["ws%d" % l]; bs_d = G.inp["bs%d" % l]; cst_d = G.inp["cstA"]
    sAf = G.sAf[l]; sAb = G.sAb[l]; sAv = G.sAv[l]; ycl = G.ycl[l]
    pc = S.sbuf("pc", [128, 32]); cst = S.sbuf("cst", [128, 6, 128])
    S.dma("sp", pc[:], pc_d[:], reads=[pc_d], writes=[pc])
    S.dma("sp", cst[:], cst_d[:], reads=[cst_d], writes=[cst])
    ONES = cst[:, 0, :]; IDENT = cst[:, 1, :]; PT = cst[0:96, 2, 0:96]; MASK = cst[:, 3, :]
    INVF = cst[0:96, 4, 0:1]; NOPE = cst[0:96, 4, 1:2]
    onesb = S.sbuf("onesb", [128, 128], BF16); identb = S.sbuf("identb", [128, 128], BF16); ptb = S.sbuf("ptb", [96, 96], BF16)
    onesrow = S.sbuf("onesrow", [1, 128], BF16)
    S.op("dve", lambda h: h.tensor_copy(out=onesb[:], in_=ONES), reads=[cst], writes=[onesb])
    S.op("dve", lambda h: h.tensor_copy(out=identb[:], in_=IDENT), reads=[cst], writes=[identb])
    S.op("dve", lambda h: h.tensor_copy(out=ptb[:], in_=PT), reads=[cst], writes=[ptb])
    S.op("pool", lambda h: h.memset(onesrow[:], 1.0), writes=[onesrow])
    epsc = S.sbuf("epsc", [128, 1]); S.op("pool", lambda h: h.memset(epsc[:], EPS), writes=[epsc])
    pic = S.sbuf("pic", [128, 1]); S.op("pool", lambda h: h.memset(pic[:], -math.pi), writes=[pic])

    NPS = 7
    slots = [S.psum(f"slot{i}", [128, TT]) for i in range(NPS)]
    ptr = S.psum("ptr", [128, 1024], BF16)
    psc = [0]
    def getps():
        psc[0] += 1
        return slots[psc[0] % NPS]
    tmpc = [0]
    tmps = [S.sbuf(f"tmp{i}", [128, TT]) for i in range(4)]
    def gettmp():
        tmpc[0] += 1
        return tmps[tmpc[0] % 4]

    xT = [S.sbuf(f"xT{k}", [128, NT], F32) for k in range(8)]
    wb = [S.sbuf(f"wb{k}", [128, INC], BF16) for k in range(8)]
    for k in range(8):
        S.dma("sp", xT[k][:], G.xsrc[l][k*128:(k+1)*128, :], reads=[G.xsrc[l]], writes=[xT[k]])
    for k in range(8):
        S.dma("pool", wb[k][:], w_d[k*128:(k+1)*128, :], reads=[w_d], writes=[wb[k]])
    wuq = [S.sbuf(f"wuq{k}", [128, 576], BF16) for k in range(2)]
    for k in range(2):
        S.dma("pool", wuq[k][:], wuq_d[k*128:(k+1)*128, :], reads=[wuq_d], writes=[wuq[k]])
    wukv = S.sbuf("wukv", [128, 768], BF16)
    S.dma("pool", wukv[:], wukv_d[:, :], reads=[wukv_d], writes=[wukv])
    wukv_v = S.sbuf("wukv_v", [128, 6, 64], BF16)
    S.op("pool", lambda h: h.tensor_copy(out=wukv_v[:], in_=wukv[:].rearrange("p (h c) -> p h c", c=128)[:, :, 64:128]), reads=[wukv], writes=[wukv_v])
    wsT = [S.sbuf(f"wsT{g}", [128, 128], BF16) for g in range(4)]
    wsl = S.sbuf("wsl", [128, 4, 128], F32)
    S.dma("sp", wsl[:], ws_d.t.rearrange("g t s -> t g s"), reads=[ws_d], writes=[wsl])
    for g in range(4):
        p = getps()
        S.op("pe", lambda h: h.transpose(p[:, 0:128], wsl[:, g, :], IDENT), reads=[wsl, cst], writes=[p])
        S.op("dve", lambda h: h.tensor_tensor(out=wsT[g][:], in0=p[:, 0:128], in1=MASK, op=ALU.mult), reads=[p, cst], writes=[wsT[g]])
    bsr = S.sbuf("bsr", [1, 512], BF16); bsf = S.sbuf("bsf", [1, 512], F32)
    S.dma("sp", bsf[:], bs_d[:, :], reads=[bs_d], writes=[bsf])
    S.op("dve", lambda h: h.tensor_copy(out=bsr[:], in_=bsf[:]), reads=[bsf], writes=[bsr])

    posi = S.sbuf("posi", [96, TT], I32); posf = S.sbuf("posf", [96, TT], F32)
    cosf = S.sbuf("cosf", [96, TT], F32); sinf = S.sbuf("sinf", [96, TT], F32)
    def rope_tables(sl):
        S.dma("sp", posi[:], pos_d.t[0:1, sl].partition_broadcast(96), reads=[pos_d], writes=[posi])
        S.op("dve", lambda h: h.tensor_copy(out=posf[:], in_=posi[:]), reads=[posi], writes=[posf])
        S.op("dve", lambda h: h.tensor_scalar(out=posf[:], in0=posf[:], scalar1=INVF, scalar2=None, op0=ALU.mult), reads=[posf, cst], writes=[posf])
        for dst, shift in [(sinf, 0.0), (cosf, 0.5 * math.pi)]:
            S.op("dve", lambda h: h.tensor_scalar(out=dst[:], in0=posf[:], scalar1=shift, scalar2=None, op0=ALU.add), reads=[posf], writes=[dst])
            S.op("dve", lambda h: h.tensor_scalar(out=rrf[:], in0=dst[:], scalar1=1.0 / TWO_PI, scalar2=None, op0=ALU.mult), reads=[dst], writes=[rrf])
            S.op("dve", lambda h: h.tensor_copy(out=rri[:], in_=rrf[:]), reads=[rrf], writes=[rri])
            S.op("dve", lambda h: h.tensor_copy(out=rrf[:], in_=rri[:]), reads=[rri], writes=[rrf])
            S.op("dve", lambda h: h.scalar_tensor_tensor(out=dst[:], in0=rrf[:], scalar=-6.28125, in1=dst[:], op0=ALU.mult, op1=ALU.add), reads=[rrf, dst], writes=[dst])
            S.op("dve", lambda h: h.scalar_tensor_tensor(out=dst[:], in0=rrf[:], scalar=-(TWO_PI - 6.28125), in1=dst[:], op0=ALU.mult, op1=ALU.add), reads=[rrf, dst], writes=[dst])
            S.op("dve", lambda h: h.tensor_scalar(out=rrf[:], in0=dst[:], scalar1=math.pi, scalar2=None, op0=ALU.is_gt), reads=[dst], writes=[rrf])
            S.op("dve", lambda h: h.scalar_tensor_tensor(out=dst[:], in0=rrf[:], scalar=-TWO_PI, in1=dst[:], op0=ALU.mult, op1=ALU.add), reads=[rrf, dst], writes=[dst])
            S.op("dve", lambda h: h.tensor_scalar(out=rrf[:], in0=dst[:], scalar1=-math.pi, scalar2=None, op0=ALU.is_lt), reads=[dst], writes=[rrf])
            S.op("dve", lambda h: h.scalar_tensor_tensor(out=dst[:], in0=rrf[:], scalar=TWO_PI, in1=dst[:], op0=ALU.mult, op1=ALU.add), reads=[rrf, dst], writes=[dst])
            S.op("act", lambda h: h.activation(out=dst[:], in_=dst[:], func=AF.Sin), reads=[dst], writes=[dst])
    rrf = S.sbuf("rrf", [96, TT], F32); rri = S.sbuf("rri", [96, TT], I32)

    sq = [S.sbuf(f"sq{i}", [128, TT], BF16) for i in range(2)]
    hT = [S.sbuf(f"hT{k}", [128, TT], BF16) for k in range(8)]
    rs = S.sbuf("rs", [128, TT], F32)
    zo = [S.sbuf(f"zo{i}", [128, TT], F32) for i in range(3)]
    zB = [S.sbuf(f"zB{i}", [128, TT], F32) for i in range(4)]
    zC = [S.sbuf(f"zC{i}", [128, TT], F32) for i in range(4)]
    cqn = [S.sbuf(f"cqn{i}", [128, TT], BF16) for i in range(2)]
    ckvn = S.sbuf("ckvn", [128, TT], BF16)
    kfull2 = [S.sbuf(f"kfull{i}", [96, TT], F32) for i in range(2)]
    sq96_2 = [S.sbuf(f"sq96_{i}", [96, TT], BF16) for i in range(2)]
    rs96_2 = [S.sbuf(f"rs96_{i}", [96, TT], F32) for i in range(2)]
    qn_2 = [S.sbuf(f"qn{i}", [96, TT], BF16) for i in range(2)]
    t1_2 = [S.sbuf(f"t1_{i}", [96, TT], F32) for i in range(2)]; t2_2 = [S.sbuf(f"t2_{i}", [96, TT], F32) for i in range(2)]
    hcnt = [0]
    qo = [S.sbuf(f"qo{i}", [96, TT], BF16) for i in range(3)]
    vo = [S.sbuf(f"vo{i}", [128, 384], BF16) for i in range(2)]
    vnb = [S.sbuf(f"vnb{i}", [128, TT], BF16) for i in range(2)]
    vtok = [S.sbuf(f"vtok{i}", [128, 128], BF16) for i in range(2)]
    yg = [S.sbuf(f"yg{i}", [128, TT], F32) for i in range(2)]
    yco = [S.sbuf(f"yco{i}", [128, TT], F32) for i in range(2)]
    cnt = [0]; qc = [0]

    def headnorm_rope(src_ps_or_sb, srcbuf, gcol, out_d, h, sl, t):
        hb_ = hcnt[0] % 2; hcnt[0] += 1
        sq96 = sq96_2[hb_]; rs96 = rs96_2[hb_]; qn = qn_2[hb_]; t1 = t1_2[hb_]; t2 = t2_2[hb_]
        S.op("act", lambda hh: hh.activation(out=sq96[:], in_=src_ps_or_sb, func=AF.Square), reads=[srcbuf], writes=[sq96])
        p = getps()
        S.op("pe", lambda hh: hh.matmul(p[0:96, :], lhsT=onesb[0:96, 0:96], rhs=sq96[:], start=True, stop=True), reads=[onesb, sq96], writes=[p])
        S.op("act", lambda hh: hh.activation(out=rs96[:], in_=p[0:96, :], func=AF.Sqrt, scale=1.0/96, bias=epsc[0:96, :]), reads=[p, epsc], writes=[rs96])
        S.op("dve", lambda hh: hh.reciprocal(out=rs96[:], in_=rs96[:]), reads=[rs96], writes=[rs96])
        S.op("dve", lambda hh: hh.scalar_tensor_tensor(out=qn[:], in0=src_ps_or_sb, scalar=gcol, in1=rs96[:], op0=ALU.mult, op1=ALU.mult), reads=[srcbuf, pc, rs96], writes=[qn])
        p2 = getps()
        S.op("pe", lambda hh: hh.matmul(p2[0:96, :], lhsT=ptb[:], rhs=qn[:], start=True, stop=True), reads=[ptb, qn], writes=[p2])
        S.op("pool", lambda hh: hh.tensor_tensor(out=t1[:], in0=qn[:], in1=cosf[:], op=ALU.mult), reads=[qn, cosf], writes=[t1])
        S.op("dve", lambda hh: hh.tensor_tensor(out=t2[:], in0=p2[0:96, :], in1=sinf[:], op=ALU.mult), reads=[p2, sinf], writes=[t2])
        o = qo[qc[0] % 3]; qc[0] += 1
        S.op("pool", lambda hh: hh.tensor_tensor(out=o[:], in0=t1[:], in1=t2[:], op=ALU.add), reads=[t1, t2], writes=[o])
        if out_d == "q":
            S.dma("sp", sAb[h*96:(h+1)*96, sl], o[:], reads=[o], writes=[sAb])
        else:
            dst = sAb.t[576 + h*96:576 + (h+1)*96, :].rearrange("d (j x) -> d j x", j=4)[:, :, t*128:(t+1)*128]
            S.dma("sp", dst, o[:].rearrange("d (j c) -> d j c", c=128), reads=[o], writes=[sAb])

    for t in range(NT // TT):
        sl = slice(t*TT, (t+1)*TT)
        rope_tables(sl)
        for k in range(8):
            s = sq[k % 2]
            S.op("act", lambda h: h.activation(out=s[:], in_=xT[k][:, sl], func=AF.Square), reads=[xT[k]], writes=[s])
            ps_ss = getps() if k == 0 else ps_ss
            S.op("pe", lambda h: h.matmul(ps_ss[:], lhsT=onesb[:], rhs=s[:], start=(k == 0), stop=(k == 7)), reads=[onesb, s], writes=[ps_ss])
        S.op("act", lambda h: h.activation(out=rs[:], in_=ps_ss[:], func=AF.Sqrt, scale=1.0/D, bias=epsc[:]), reads=[ps_ss, epsc], writes=[rs])
        S.op("dve", lambda h: h.reciprocal(out=rs[:], in_=rs[:]), reads=[rs], writes=[rs])
        for k in range(8):
            S.op("dve", lambda h: h.scalar_tensor_tensor(out=hT[k][:], in0=xT[k][:, sl], scalar=pc[:, k:k+1], in1=rs[:], op0=ALU.mult, op1=ALU.mult), reads=[xT[k], pc, rs], writes=[hT[k]])
        for m, (c0, mw) in enumerate(CH):
            pz = getps()
            for k in range(8):
                S.op("pe", lambda h: h.matmul(pz[:mw, :], lhsT=wb[k][:, c0:c0+mw], rhs=hT[k][:], start=(k == 0), stop=(k == 7)), reads=[wb[k], hT[k]], writes=[pz])
            if m < 10:
                o = zo[cnt[0] % 3]; cnt[0] += 1
                if m % 2 == 0:
                    S.op("act", lambda h: h.copy(out=o[:mw, :], in_=pz[:mw, :]), reads=[pz], writes=[o])
                else:
                    S.op("dve", lambda h: h.tensor_copy(out=o[:mw, :], in_=pz[:mw, :]), reads=[pz], writes=[o])
                S.dma("sp", sAf[c0:c0+mw, sl], o[:mw, :], reads=[o], writes=[sAf])
            elif m < 14:
                o = zB[m - 10]
                S.op("act", lambda h: h.copy(out=o[:mw, :], in_=pz[:mw, :]), reads=[pz], writes=[o])
            else:
                o = zC[m - 14]
                S.op("act", lambda h: h.activation(out=o[:], in_=pz[:], func=AF.Gelu), reads=[pz], writes=[o])
        ps1 = getps()
        for k in range(2):
            s = sq[k % 2]
            S.op("act", lambda h: h.activation(out=s[:], in_=zB[k][:], func=AF.Square), reads=[zB[k]], writes=[s])
            S.op("pe", lambda h: h.matmul(ps1[:], lhsT=onesb[:], rhs=s[:], start=(k == 0), stop=(k == 1)), reads=[onesb, s], writes=[ps1])
        S.op("act", lambda h: h.activation(out=rs[:], in_=ps1[:], func=AF.Sqrt, scale=1.0/256, bias=epsc[:]), reads=[ps1, epsc], writes=[rs])
        S.op("dve", lambda h: h.reciprocal(out=rs[:], in_=rs[:]), reads=[rs], writes=[rs])
        for k in range(2):
            S.op("dve", lambda h: h.scalar_tensor_tensor(out=cqn[k][:], in0=zB[k][:], scalar=pc[:, 8+k:9+k], in1=rs[:], op0=ALU.mult, op1=ALU.mult), reads=[zB[k], pc, rs], writes=[cqn[k]])
        for hd in range(6):
            pq = getps()
            for k in range(2):
                S.op("pe", lambda h: h.matmul(pq[0:96, :], lhsT=wuq[k][:, hd*96:(hd+1)*96], rhs=cqn[k][:], start=(k == 0), stop=(k == 1)), reads=[wuq[k], cqn[k]], writes=[pq])
            headnorm_rope(pq[0:96, :], pq, pc[0:96, 11:12], "q", hd, sl, t)
        s = sq[0]
        S.op("act", lambda h: h.activation(out=s[:], in_=zB[2][:], func=AF.Square), reads=[zB[2]], writes=[s])
        ps2 = getps()
        S.op("pe", lambda h: h.matmul(ps2[:], lhsT=onesb[:], rhs=s[:], start=True, stop=True), reads=[onesb, s], writes=[ps2])
        S.op("act", lambda h: h.activation(out=rs[:], in_=ps2[:], func=AF.Sqrt, scale=1.0/128, bias=epsc[:]), reads=[ps2, epsc], writes=[rs])
        S.op("dve", lambda h: h.reciprocal(out=rs[:], in_=rs[:]), reads=[rs], writes=[rs])
        S.op("dve", lambda h: h.scalar_tensor_tensor(out=ckvn[:], in0=zB[2][:], scalar=pc[:, 10:11], in1=rs[:], op0=ALU.mult, op1=ALU.mult), reads=[zB[2], pc, rs], writes=[ckvn])
        for kf_ in kfull2:
            S.op("pool", lambda h: h.tensor_copy(out=kf_[64:96, :], in_=zB[3][0:32, :]), reads=[zB[3]], writes=[kf_])
        for hd in range(6):
            pk = getps()
            S.op("pe", lambda h: h.matmul(pk[0:64, :], lhsT=wukv[:, hd*128:hd*128+64], rhs=ckvn[:], start=True, stop=True), reads=[wukv, ckvn], writes=[pk])
            kfull = kfull2[hd % 2]
            S.op("act", lambda h: h.copy(out=kfull[0:64, :], in_=pk[0:64, :]), reads=[pk], writes=[kfull])
            headnorm_rope(kfull[:], kfull, pc[0:96, 12:13], "k", hd, sl, t)
        for j in range(TT // 128):
            pv = getps()
            S.op("pe", lambda h: h.matmul(pv[:, 0:384], lhsT=ckvn[:, j*128:(j+1)*128], rhs=wukv_v[:].rearrange("p h c -> p (h c)"), start=True, stop=True), reads=[ckvn, wukv_v], writes=[pv])
            o = vo[j % 2]
            S.op("act", lambda h: h.copy(out=o[:], in_=pv[:, 0:384]), reads=[pv], writes=[o])
            S.dma("sp", sAv[j*(NT//4) + t*128: j*(NT//4) + (t+1)*128, :], o[:], reads=[o], writes=[sAv])
        pm = getps()
        for k in range(2):
            S.op("pe", lambda h: h.matmul(pm[:], lhsT=ONES, rhs=zC[2+k][:], start=(k == 0), stop=(k == 1)), reads=[cst, zC[2+k]], writes=[pm])
        vc = [gettmp(), gettmp()]
        for k in range(2):
            S.op("dve", lambda h: h.scalar_tensor_tensor(out=vc[k][:], in0=pm[:], scalar=-1.0/256, in1=zC[2+k][:], op0=ALU.mult, op1=ALU.add), reads=[pm, zC[2+k]], writes=[vc[k]])
        pvv = getps()
        for k in range(2):
            s2 = gettmp()
            S.op("pool", lambda h: h.tensor_tensor(out=s2[:], in0=vc[k][:], in1=vc[k][:], op=ALU.mult), reads=[vc[k]], writes=[s2])
            S.op("pe", lambda h: h.matmul(pvv[:], lhsT=ONES, rhs=s2[:], start=(k == 0), stop=(k == 1)), reads=[cst, s2], writes=[pvv])
        S.op("act", lambda h: h.activation(out=rs[:], in_=pvv[:], func=AF.Sqrt, scale=1.0/256, bias=epsc[:]), reads=[pvv, epsc], writes=[rs])
        S.op("dve", lambda h: h.reciprocal(out=rs[:], in_=rs[:]), reads=[rs], writes=[rs])
        for k in range(2):
            S.op("dve", lambda h: h.tensor_tensor(out=vc[k][:], in0=vc[k][:], in1=rs[:], op=ALU.mult), reads=[vc[k], rs], writes=[vc[k]])
            S.op("dve", lambda h: h.tensor_scalar(out=vnb[k][:], in0=vc[k][:], scalar1=pc[:, 13+k:14+k], scalar2=pc[:, 15+k:16+k], op0=ALU.mult, op1=ALU.add), reads=[vc[k], pc], writes=[vnb[k]])
        for j in range(TT // 128):
            bsl = slice(j*128, (j+1)*128)
            for k in range(2):
                S.op("pe", lambda h: h.transpose(ptr[:, k*128:(k+1)*128], vnb[k][:, bsl], identb[:]), reads=[vnb[k], identb], writes=[ptr])
                S.op("act", lambda h: h.copy(out=vtok[k][:], in_=ptr[:, k*128:(k+1)*128]), reads=[ptr], writes=[vtok[k]])
            for g in range(4):
                k = g // 2; hf = slice((g % 2)*64, (g % 2)*64 + 64)
                pg = getps()
                S.op("pe", lambda h: h.matmul(pg[:, 0:128], lhsT=vtok[k][:], rhs=wsT[g][:], start=True, stop=False), reads=[vtok[k], wsT[g]], writes=[pg])
                S.op("pe", lambda h: h.matmul(pg[:, 0:128], lhsT=onesrow[:], rhs=bsr[:, g*128:(g+1)*128], start=False, stop=True), reads=[onesrow, bsr], writes=[pg])
                S.op("dve", lambda h: h.tensor_tensor(out=yg[k][hf, bsl], in0=pg[hf, 0:128], in1=zC[k][hf, bsl], op=ALU.mult), reads=[pg, zC[k]], writes=[yg[k]])
        py = getps()
        for k in range(2):
            s2 = gettmp()
            S.op("pool", lambda h: h.tensor_tensor(out=s2[:], in0=yg[k][:], in1=yg[k][:], op=ALU.mult), reads=[yg[k]], writes=[s2])
            S.op("pe", lambda h: h.matmul(py[:], lhsT=ONES, rhs=s2[:], start=(k == 0), stop=(k == 1)), reads=[cst, s2], writes=[py])
        S.op("act", lambda h: h.activation(out=rs[:], in_=py[:], func=AF.Sqrt, scale=1.0/256, bias=epsc[:]), reads=[py, epsc], writes=[rs])
        S.op("dve", lambda h: h.reciprocal(out=rs[:], in_=rs[:]), reads=[rs], writes=[rs])
        for k in range(2):
            o = yco[k]
            S.op("dve", lambda h: h.scalar_tensor_tensor(out=o[:], in0=yg[k][:], scalar=pc[:, 17+k:18+k], in1=rs[:], op0=ALU.mult, op1=ALU.mult), reads=[yg[k], pc, rs], writes=[o])
            S.dma("sp", ycl[k*128:(k+1)*128, sl], o[:], reads=[o], writes=[ycl])

    allgather_rows(S, sAb, G.gAb[l], RAB, CR_B)
    allgather_rows(S, sAv, G.gAv[l], NT, min(CR_V, NT))
    S.phase_end()


def phase_B(nc, G, l):
    NT = G.NT; SEQ = 4 * NT; NTILE = SEQ // TT; NTL = NT // TT
    S = Sched(nc)
    for b_ in G.persist:
        b_.reset()
    gAf = G.gAf[l]; sP = G.sP[l]
    allgather_rows(S, sP, G.gP[l], RP, CR_F, m0=4)
    pcol_d = G.inp["pcolB%d" % l]; plo_d = G.inp["plo%d" % l]; wup_d = G.inp["wup%d" % l]; aup_d = G.inp["aup%d" % l]
    gup_d = G.inp["gup%d" % l]; w0row_d = G.inp["w0row%d" % l]; cst_d = G.inp["cstB"]
    if l == 1:
        vdown_d = G.inp["vdown"]; vup_d = G.inp["vup"]
    idxB = S.sbuf("idxB", [128, 3, NTILE], I32); idxBh = S.sbuf("idxBh", [128, 3, NTILE], I32)
    S.dma("sp", idxB[:], G.inp["idxB"].t, reads=[G.inp["idxB"]], writes=[idxB])
    S.dma("sp", idxBh[:], G.inp["idxBh"].t, reads=[G.inp["idxBh"]], writes=[idxBh])
    pcol = S.sbuf("pcol", [128, 16]); plo = S.sbuf("plo", [64, 4])
    wup = S.sbuf("wup", [32, 128]); aup = S.sbuf("aup", [32, 128]); gup = S.sbuf("gup", [64, 128])
    w0row = S.sbuf("w0row", [1, 128]); cst = S.sbuf("cst", [128, 8, 128])
    onesrow = S.sbuf("onesrow", [1, 128])
    omk = S.sbuf("omk", [128, 1])
    for sb, dr in [(pcol, pcol_d), (plo, plo_d), (wup, wup_d), (aup, aup_d), (gup, gup_d), (w0row, w0row_d)]:
        S.dma("sp", sb[:], dr[:], reads=[dr], writes=[sb])
    S.dma("sp", cst[:], cst_d[:], reads=[cst_d], writes=[cst])
    if l == 1:
        vdown = S.sbuf("vdown", [128, 3, 16]); vup = S.sbuf("vup", [16, 128])
        S.dma("sp", vdown[:], vdown_d[:], reads=[vdown_d], writes=[vdown])
        S.dma("sp", vup[:], vup_d[:], reads=[vup_d], writes=[vup])
    S.op("pool", lambda h: h.memset(onesrow[:], 1.0), writes=[onesrow])
    S.op("dve", lambda h: h.tensor_scalar(out=omk[:], in0=pcol[:, 6:7], scalar1=-1.0, scalar2=1.0, op0=ALU.mult, op1=ALU.add), reads=[pcol], writes=[omk])
    ONESB = cst[:, 0, :]; TRI = cst[:, 1, :]; TRIS = cst[:, 2, :]; MSU = cst[:, 2, :]; MSL = cst[:, 3, :]
    MUI = cst[:, 4, :]; MNUI = cst[:, 5, :]; IDENT = cst[:, 6, :]; MEANB = cst[:, 7, :]
    C_MUR, C_MUK, C_MUV, C_W0, C_A0, C_KK, C_KA, C_RK, C_LNG, C_LNB, C_V0, C_MU0V = range(12)

    def col(i, n=128):
        return pcol[0:n, i:i+1]

    NB = 2
    raw = {}
    names = [("zr", 128), ("zk", 128), ("zv", 128), ("zw", 32), ("za", 32), ("zg", 64)]
    if l == 1:
        names += [("zva0", 128), ("zva1", 128), ("zva2", 128), ("zv0", 128)]
    for nm, rows in names:
        raw[nm] = [S.sbuf(f"raw_{nm}{i}", [rows, TT + 1]) for i in range(1)] * NB
        S.op("pool", lambda h: h.memset(raw[nm][0][:, 0:1], 0.0), writes=[raw[nm][0]])
    tmp = [S.sbuf(f"tmp{i}", [128, TT]) for i in range(3)]
    tmpc = [0]
    def gettmp():
        tmpc[0] += 1
        return tmp[tmpc[0] % 3]
    sh = {nm: S.sbuf(f"sh_{nm}", [rows, TT]) for nm, rows in names}
    th = S.sbuf("th", [32, TT])
    sgd = S.sbuf("sgd", [64, TT])
    a_t = S.sbuf("a_t", [128, TT]); g_t = [S.sbuf(f"g_t{i}", [128, TT]) for i in range(NB)]
    kk = S.sbuf("kk", [128, TT]); kap = S.sbuf("kap", [128, TT]); kmod = S.sbuf("kmod", [128, TT])
    b_t = S.sbuf("b_t", [128, TT]); v_t = [S.sbuf(f"v_t{i}", [128, TT]) for i in range(NB)]
    bv = [S.sbuf(f"bv{i}", [128, TT]) for i in range(NB)]
    sq = S.sbuf("sq", [128, TT]); nrm = S.sbuf("nrm", [128, TT])
    sgtok = [S.sbuf(f"sgtok{i}", [128, 128]) for i in range(2)]
    eL = S.sbuf("eL", [128, TT]); eLx = S.sbuf("eLx", [128, TT]); enL = S.sbuf("enL", [128, TT])
    gam = [S.sbuf(f"gam{i}", [128, NCH]) for i in range(NB)]
    vd_sb = S.sbuf("vd_sb", [16, TT]); sv = S.sbuf("sv", [128, TT])
    bd = {nm: [S.sbuf(f"bd_{nm}{i}", [128, NCH, 128]) for i in range(NB)] for nm in ["RT", "KpT", "KT", "BT", "VT"]}
    for nm in bd:
        for i in range(NB):
            S.op("pool", lambda h: h.memset(bd[nm][i][:], 0.0), writes=[bd[nm][i]])
    yT = [S.sbuf(f"yT{i}", [128, TT]) for i in range(NB)]
    yo = [S.sbuf(f"yo{i}", [128, TT]) for i in range(1)] * NB
    pL = S.psum("pL", [128, TT]); pLx = S.psum("pLx", [128, TT])
    NPS = 6
    slots = [S.psum(f"slot{i}", [128, TT]) for i in range(NPS)]
    psc = [0]
    def getps():
        psc[0] += 1
        s_ = slots[psc[0] % NPS]
        return s_.view(s_.t[:, 0:128])
    def getpbig():
        psc[0] += 1
        return slots[psc[0] % NPS]
    def pool_of(name, n, shape=(128, 128)):
        bufs = [S.sbuf(f"{name}{i}", list(shape)) for i in range(n)]
        c = [0]
        def get():
            c[0] += 1
            return bufs[c[0] % n]
        return get
    NPIPE = 6
    g_N = pool_of("cN", NPIPE); g_Q = pool_of("cQ", NPIPE); g_Aak = pool_of("cAak", NPIPE)
    g_nArb = pool_of("cnArb", NPIPE); g_Ark = pool_of("cArk", NPIPE)
    g_nB = pool_of("cnB", NPIPE); g_Kb = pool_of("cKb", NPIPE); g_Vb = pool_of("cVb", NPIPE)
    g_X = pool_of("cX", 6); g_XT = pool_of("cXT", 6); g_WT = pool_of("cWT", 6); g_WTf = pool_of("cWTf", NPIPE + 1)
    g_rhs = pool_of("crhs", 2); g_U = pool_of("cU", 2)
    Mst = [S.sbuf(f"Mst{i}", [128, 128]) for i in range(2)]
    Mg = S.sbuf("Mg", [128, 128])
    S.op("pool", lambda h: h.memset(Mst[0][:], 0.0), writes=[Mst[0]])
    evc = [0]
    def evac_copy(dst, src, scale=None):
        evc[0] += 1
        if evc[0] % 2 == 0:
            if scale is None:
                S.op("act", lambda h: h.copy(out=dst[:], in_=src[:]), reads=[src], writes=[dst])
            else:
                S.op("act", lambda h: h.mul(out=dst[:], in_=src[:], mul=scale), reads=[src], writes=[dst])
        else:
            if scale is None:
                S.op("dve", lambda h: h.tensor_copy(out=dst[:], in_=src[:]), reads=[src], writes=[dst])
            else:
                S.op("dve", lambda h: h.tensor_scalar(out=dst[:], in0=src[:], scalar1=scale, scalar2=None, op0=ALU.mult), reads=[src], writes=[dst])

    def mm(ps, lhsT, rhs, rl, rr, start=True, stop=True, psl=None):
        o = ps[:] if psl is None else psl
        S.op("pe", lambda h: h.matmul(o, lhsT=lhsT, rhs=rhs, start=start, stop=stop), reads=[rl, rr], writes=[ps])

    def pre_steps(ti):
        bi = ti % NB
        t0 = ti * TT
        steps = []
        def loads():
            i_ = ti // NTL; n_ = ti % NTL
            gv = gAf.t.rearrange("a (n c) -> (a n) c", c=TT)
            gh = gAf.t.rearrange("a (c o) -> (a c) o", o=1)
            kinds = [("zr", 0, gAf, gv, gh), ("zk", 1, gAf, gv, gh), ("zv", 2, gAf, gv, gh)]
            if l == 1:
                gv0 = G.gAf[0].t.rearrange("a (n c) -> (a n) c", c=TT)
                gh0 = G.gAf[0].t.rearrange("a (c o) -> (a c) o", o=1)
                kinds.append(("zv0", 2, G.gAf[0], gv0, gh0))
            for nm, kd_, gb, v_, h_ in kinds:
                dst = raw[nm][bi]
                S.idma(dst[:, 1:TT+1], v_, idxB[:, kd_, ti:ti+1], reads=[gb, idxB], writes=[dst])
                if ti > 0:
                    S.idma(dst[:, 0:1], h_, idxBh[:, kd_, ti:ti+1], reads=[gb, idxBh], writes=[dst])
            stat = [("zw", 1152, 32), ("za", 1184, 32), ("zg", 1216, 64)]
            if l == 1:
                stat += [("zva0", 768, 128), ("zva1", 896, 128), ("zva2", 1024, 128)]
            for nm, r0, nr in stat:
                dst = raw[nm][bi]
                base = int(gaddr(CR_F, 1280, i_, r0))
                if n_ > 0:
                    S.dma("sp", dst[:, 0:TT+1], gAf[base:base+nr, n_*TT-1:(n_+1)*TT], reads=[gAf], writes=[dst])
                else:
                    S.dma("sp", dst[:, 1:TT+1], gAf[base:base+nr, 0:TT], reads=[gAf], writes=[dst])
                    if ti > 0:
                        pb = int(gaddr(CR_F, 1280, i_ - 1, r0))
                        S.dma("sp", dst[:, 0:1], gAf[pb:pb+nr, NT-1:NT], reads=[gAf], writes=[dst], allow_slow_non_contiguous=True)
        steps.append(loads)
        def shift(nm, mucol):
            X = raw[nm][bi]; o = sh[nm]; rows = X.t.shape[0]
            tp = gettmp()
            S.op("pool", lambda h: h.tensor_tensor(out=tp[0:rows, :], in0=X[:, 0:TT], in1=X[:, 1:TT+1], op=ALU.subtract), reads=[X], writes=[tp])
            S.op("dve", lambda h: h.scalar_tensor_tensor(out=o[:], in0=tp[0:rows, :], scalar=mucol, in1=X[:, 1:TT+1], op0=ALU.mult, op1=ALU.add), reads=[tp, X, pcol, plo], writes=[o])
        def shifts():
            shift("zr", col(C_MUR)); shift("zk", col(C_MUK)); shift("zv", col(C_MUV))
            shift("zw", plo[0:32, 0:1]); shift("za", plo[0:32, 1:2]); shift("zg", plo[0:64, 2:3])
            if l == 1:
                shift("zva0", col(12)); shift("zva1", col(13)); shift("zva2", col(14)); shift("zv0", col(C_MU0V))
        steps.append(shifts)
        def loras():
            S.op("act", lambda h: h.activation(out=th[:], in_=sh["zw"][:], func=AF.Tanh), reads=[sh["zw"]], writes=[th])
            for j in range(TT // 128):
                pt = getps()
                mm(pt, th[:, j*128:(j+1)*128], wup[:], th, wup, start=True, stop=False)
                mm(pt, onesrow[:], w0row[:], onesrow, w0row, start=False, stop=True)
                st = sgtok[j % 2]
                S.op("act", lambda h: h.activation(out=st[:], in_=pt[:], func=AF.Sigmoid), reads=[pt], writes=[st])
                mm(pL, st[:], TRI, st, cst, psl=pL[:, j*128:(j+1)*128])
                mm(pLx, st[:], TRIS, st, cst, psl=pLx[:, j*128:(j+1)*128])
            S.op("act", lambda h: h.activation(out=eL[:], in_=pL[:], func=AF.Exp, scale=LWS), reads=[pL], writes=[eL])
            S.op("act", lambda h: h.activation(out=enL[:], in_=pL[:], func=AF.Exp, scale=-LWS), reads=[pL], writes=[enL])
            S.op("act", lambda h: h.activation(out=eLx[:], in_=pLx[:], func=AF.Exp, scale=LWS), reads=[pLx], writes=[eLx])
            gm = gam[bi]
            S.op("dve", lambda h: h.tensor_copy(out=gm[:], in_=eL[:, T-1::T]), reads=[eL], writes=[gm])
            p = getpbig()
            mm(p, aup[:], sh["za"][:], aup, sh["za"])
            S.op("act", lambda h: h.activation(out=a_t[:], in_=p[:], func=AF.Sigmoid, bias=col(C_A0)), reads=[p, pcol], writes=[a_t])
            S.op("act", lambda h: h.activation(out=sgd[:], in_=sh["zg"][:], func=AF.Sigmoid), reads=[sh["zg"]], writes=[sgd])
            p2 = getpbig()
            mm(p2, gup[:], sgd[:], gup, sgd)
            S.op("act", lambda h: h.copy(out=g_t[bi][:], in_=p2[:]), reads=[p2], writes=[g_t[bi]])
        steps.append(loras)
        def vres():
            vt = v_t[bi]
            if l == 0:
                S.op("pool", lambda h: h.tensor_copy(out=vt[:], in_=sh["zv"][:]), reads=[sh["zv"]], writes=[vt])
                return
            p = getps()
            for c in range(3):
                mm(p, vdown[:, c, :], sh[f"zva{c}"][:], vdown, sh[f"zva{c}"], start=(c == 0), stop=(c == 2), psl=None) if False else \
                    S.op("pe", lambda h: h.matmul(pbig_v[0:16, :], lhsT=vdown[:, c, :], rhs=sh[f"zva{c}"][:], start=(c == 0), stop=(c == 2)), reads=[vdown, sh[f"zva{c}"]], writes=[pbig_vb])
            S.op("act", lambda h: h.copy(out=vd_sb[:], in_=pbig_v[0:16, :]), reads=[pbig_vb], writes=[vd_sb])
            p3 = getpbig()
            mm(p3, vup[:], vd_sb[:], vup, vd_sb)
            S.op("act", lambda h: h.activation(out=sv[:], in_=p3[:], func=AF.Sigmoid, bias=col(C_V0)), reads=[p3, pcol], writes=[sv])
            tp = gettmp()
            S.op("pool", lambda h: h.tensor_tensor(out=tp[:], in0=sh["zv0"][:], in1=sh["zv"][:], op=ALU.subtract), reads=[sh["zv0"], sh["zv"]], writes=[tp])
            S.op("dve", lambda h: h.tensor_tensor(out=tp[:], in0=tp[:], in1=sv[:], op=ALU.mult), reads=[tp, sv], writes=[tp])
            S.op("pool", lambda h: h.tensor_tensor(out=vt[:], in0=tp[:], in1=sh["zv"][:], op=ALU.add), reads=[tp, sh["zv"]], writes=[vt])
        if l == 1:
            pbig_vb = getpbig(); pbig_v = pbig_vb.t
        steps.append(vres)
        def kstuff():
            S.op("dve", lambda h: h.tensor_scalar(out=kk[:], in0=sh["zk"][:], scalar1=col(C_KK), scalar2=None, op0=ALU.mult), reads=[sh["zk"], pcol], writes=[kk])
            S.op("pool", lambda h: h.tensor_tensor(out=sq[:], in0=kk[:], in1=kk[:], op=ALU.mult), reads=[kk], writes=[sq])
            p = getpbig()
            mm(p, ONESB, sq[:], cst, sq)
            S.op("act", lambda h: h.activation(out=nrm[:], in_=p[:], func=AF.Sqrt), reads=[p], writes=[nrm])
            S.op("dve", lambda h: h.tensor_scalar(out=nrm[:], in0=nrm[:], scalar1=1e-12, scalar2=None, op0=ALU.max), reads=[nrm], writes=[nrm])
            S.op("dve", lambda h: h.reciprocal(out=nrm[:], in_=nrm[:]), reads=[nrm], writes=[nrm])
            S.op("dve", lambda h: h.tensor_tensor(out=kap[:], in0=kk[:], in1=nrm[:], op=ALU.mult), reads=[kk, nrm], writes=[kap])
            tp = gettmp()
            S.op("dve", lambda h: h.tensor_scalar(out=tp[:], in0=a_t[:], scalar1=col(C_KA), scalar2=omk[:, 0:1], op0=ALU.mult, op1=ALU.add), reads=[a_t, pcol, omk], writes=[tp])
            S.op("pool", lambda h: h.tensor_tensor(out=kmod[:], in0=sh["zk"][:], in1=tp[:], op=ALU.mult), reads=[sh["zk"], tp], writes=[kmod])
            S.op("pool", lambda h: h.tensor_tensor(out=b_t[:], in0=kap[:], in1=a_t[:], op=ALU.mult), reads=[kap, a_t], writes=[b_t])
            tp2 = gettmp()
            S.op("dve", lambda h: h.scalar_tensor_tensor(out=tp2[:], in0=sh["zr"][:], scalar=col(C_RK), in1=kmod[:], op0=ALU.mult, op1=ALU.mult), reads=[sh["zr"], pcol, kmod], writes=[tp2])
            p2 = getpbig()
            mm(p2, ONESB, tp2[:], cst, tp2)
            S.op("dve", lambda h: h.tensor_tensor(out=bv[bi][:], in0=p2[:], in1=v_t[bi][:], op=ALU.mult), reads=[p2, v_t[bi]], writes=[bv[bi]])
        steps.append(kstuff)
        def expand():
            for nm, src, ee in [("RT", sh["zr"], eL), ("KpT", kap, eLx), ("KT", kmod, enL), ("BT", b_t, enL), ("VT", v_t[bi], None)]:
                dst = bd[nm][bi]
                for hh in range(2):
                    ps_ = slice(hh*64, (hh+1)*64)
                    o = dst[ps_, :, hh*64:(hh+1)*64]
                    i0 = src[ps_, :].rearrange("p (c t) -> p c t", t=T)
                    eng = "dve" if hh == 0 else "pool"
                    if ee is None:
                        S.op(eng, lambda h: h.tensor_copy(out=o, in_=i0), reads=[src], writes=[dst])
                    else:
                        i1 = ee[ps_, :].rearrange("p (c t) -> p c t", t=T)
                        S.op(eng, lambda h: h.tensor_tensor(out=o, in0=i0, in1=i1, op=ALU.mult), reads=[src, ee], writes=[dst])
        steps.append(expand)
        return steps

    def chunk_par(ti, c):
        bi = ti % NB
        RT = bd["RT"][bi]; KpT = bd["KpT"][bi]; KT = bd["KT"][bi]; BT = bd["BT"][bi]; VT = bd["VT"][bi]
        rt = RT[:, c, :]; kpt = KpT[:, c, :]; kt = KT[:, c, :]; bt = BT[:, c, :]; vt = VT[:, c, :]
        st = {}
        steps = []
        def amats():
            N = g_N(); Q = g_Q(); Aak = g_Aak(); nArb = g_nArb(); Ark = g_Ark()
            for dst, l, ll, r, rr, mask in [(N, bt, BT, kpt, KpT, MSU), (Q, kpt, KpT, bt, BT, MSL), (Aak, kt, KT, kpt, KpT, MSU),
                                            (nArb, bt, BT, rt, RT, MNUI), (Ark, kt, KT, rt, RT, MUI)]:
                p = getps()
                mm(p, l, r, ll, rr)
                S.op("dve", lambda h: h.tensor_tensor(out=dst[:], in0=p[:], in1=mask, op=ALU.mult), reads=[p, cst], writes=[dst])
            st.update(N=N, Q=Q, Aak=Aak, nArb=nArb, Ark=Ark)
        steps.append(amats)
        def transposes():
            nB = g_nB(); Kb = g_Kb(); Vb = g_Vb()
            for dst, src, sb, scale in [(nB, bt, BT, -1.0), (Kb, kt, KT, None), (Vb, vt, VT, None)]:
                p = getps()
                S.op("pe", lambda h: h.transpose(p[:], src, IDENT), reads=[sb, cst], writes=[p])
                evac_copy(dst, p, scale)
            st.update(nB=nB, Kb=Kb, Vb=Vb)
        steps.append(transposes)
        def inv0():
            WT = g_WT()
            S.op("pool", lambda h: h.tensor_tensor(out=WT[:], in0=IDENT, in1=st["N"][:], op=ALU.subtract), reads=[cst, st["N"]], writes=[WT])
            st.update(WT=WT, X=st["N"], XT=st["Q"])
        steps.append(inv0)
        def invj(j):
            def f():
                X = st["X"]; XT = st["XT"]; WT = st["WT"]
                last = (j == 4)
                XTn = g_XT()
                p2 = getps(); mm(p2, X[:], XT[:], X, XT)
                if not last:
                    Xn = g_X()
                    p1 = getps(); mm(p1, XT[:], X[:], XT, X)
                    S.op("act", lambda h: h.copy(out=Xn[:], in_=p1[:]), reads=[p1], writes=[Xn])
                S.op("dve", lambda h: h.tensor_copy(out=XTn[:], in_=p2[:]), reads=[p2], writes=[XTn])
                p3 = getps(); mm(p3, XTn[:], WT[:], XTn, WT)
                WTn = g_WTf() if last else g_WT()
                S.op("dve", lambda h: h.tensor_tensor(out=WTn[:], in0=p3[:], in1=WT[:], op=ALU.add), reads=[p3, WT], writes=[WTn])
                st["XT"] = XTn; st["WT"] = WTn
                if not last:
                    st["X"] = Xn
            return f
        for j in range(5):
            steps.append(invj(j))
        return steps, st

    mcur = [0]
    def chunk_seq(ti, c, st):
        bi = ti % NB
        RT = bd["RT"][bi]; KpT = bd["KpT"][bi]
        rt = RT[:, c, :]; kpt = KpT[:, c, :]
        steps = []
        def s1():
            M0 = Mst[mcur[0] % 2]
            p = getps()
            mm(p, kpt, M0[:], KpT, M0, start=True, stop=False)
            mm(p, st["Aak"][:], st["Vb"][:], st["Aak"], st["Vb"], start=False, stop=True)
            rhs = g_rhs()
            S.op("act", lambda h: h.copy(out=rhs[:], in_=p[:]), reads=[p], writes=[rhs])
            st["rhs"] = rhs
            S.op("pool", lambda h: h.tensor_scalar(out=Mg[:], in0=M0[:], scalar1=gam[bi][:, c:c+1], scalar2=None, op0=ALU.mult), reads=[M0, gam[bi]], writes=[Mg])
        def s2():
            p = getps()
            mm(p, st["WT"][:], st["rhs"][:], st["WT"], st["rhs"])
            U = g_U()
            S.op("dve", lambda h: h.tensor_copy(out=U[:], in_=p[:]), reads=[p], writes=[U])
            st["U"] = U
        def s3():
            M0 = Mst[mcur[0] % 2]; M1 = Mst[(mcur[0] + 1) % 2]
            U = st["U"]
            pm = getps()
            mm(pm, st["Kb"][:], st["Vb"][:], st["Kb"], st["Vb"], start=True, stop=False)
            mm(pm, st["nB"][:], U[:], st["nB"], U, start=False, stop=True)
            S.op("dve", lambda h: h.scalar_tensor_tensor(out=M1[:], in0=pm[:], scalar=gam[bi][:, c:c+1], in1=Mg[:], op0=ALU.mult, op1=ALU.add), reads=[pm, gam[bi], Mg], writes=[M1])
            py = getps()
            mm(py, M0[:], rt, M0, RT, start=True, stop=False)
            mm(py, U[:], st["nArb"][:], U, st["nArb"], start=False, stop=False)
            mm(py, st["Vb"][:], st["Ark"][:], st["Vb"], st["Ark"], start=False, stop=True)
            y = yT[bi]
            S.op("act", lambda h: h.copy(out=y[0:64, c*T:(c+1)*T], in_=py[0:64, 0:64]), reads=[py], writes=[y])
            S.op("act", lambda h: h.copy(out=y[64:128, c*T:(c+1)*T], in_=py[64:128, 64:128]), reads=[py], writes=[y])
            mcur[0] += 1
        return [s1, s2, s3]

    def post_steps(ti):
        bi = ti % NB
        t0 = ti * TT
        def f():
            y = yT[bi]
            p = getpbig()
            mm(p, MEANB, y[:], cst, y)
            yc = gettmp()
            S.op("dve", lambda h: h.tensor_tensor(out=yc[:], in0=y[:], in1=p[:], op=ALU.subtract), reads=[y, p], writes=[yc])
            s2 = gettmp()
            S.op("pool", lambda h: h.tensor_tensor(out=s2[:], in0=yc[:], in1=yc[:], op=ALU.mult), reads=[yc], writes=[s2])
            p2 = getpbig()
            mm(p2, MEANB, s2[:], cst, s2)
            S.op("act", lambda h: h.activation(out=s2[:], in_=p2[:], func=AF.Sqrt, bias=epsc[:, 0:1]), reads=[p2, epsc], writes=[s2])
            S.op("dve", lambda h: h.reciprocal(out=s2[:], in_=s2[:]), reads=[s2], writes=[s2])
            S.op("dve", lambda h: h.tensor_tensor(out=yc[:], in0=yc[:], in1=s2[:], op=ALU.mult), reads=[yc, s2], writes=[yc])
            S.op("dve", lambda h: h.tensor_scalar(out=yc[:], in0=yc[:], scalar1=col(C_LNG), scalar2=col(C_LNB), op0=ALU.mult, op1=ALU.add), reads=[yc, pcol], writes=[yc])
            S.op("pool", lambda h: h.tensor_tensor(out=yc[:], in0=yc[:], in1=bv[bi][:], op=ALU.add), reads=[yc, bv[bi]], writes=[yc])
            o = yo[bi]
            S.op("dve", lambda h: h.tensor_tensor(out=o[:], in0=yc[:], in1=g_t[bi][:], op=ALU.mult), reads=[yc, g_t[bi]], writes=[o])
            S.dma("sp", sP[(ti // NTL)*128:(ti // NTL + 1)*128, (ti % NTL)*TT:(ti % NTL + 1)*TT], o[:], reads=[o], writes=[sP])
        return [f]
    epsc = S.sbuf("epsc", [128, 1])
    S.op("pool", lambda h: h.memset(epsc[:], GN_EPS), writes=[epsc])

    for s in pre_steps(0):
        s()
    LAG = 2
    pend = []
    nextpre = []
    for ti in range(NTILE):
        if ti + 1 < NTILE:
            nextpre = pre_steps(ti + 1)
        else:
            nextpre = []
        for c in range(0, NCH, 2):
            pa_, sta_ = chunk_par(ti, c)
            pb__, stb_ = chunk_par(ti, c + 1)
            psteps = [x_ for pr_ in zip(pa_, pb__) for x_ in pr_]
            seqs = []
            if len(pend) >= LAG:
                for _ in range(2):
                    pti, pc, pst = pend.pop(0)
                    seqs = seqs + chunk_seq(pti, pc, pst)
                    if pc == NCH - 1:
                        seqs = seqs + post_steps(pti)
            qi = 0
            for i in range(len(psteps)):
                psteps[i]()
                while qi < len(seqs) and (qi + 1) * len(psteps) <= (i + 1) * max(1, len(seqs)):
                    seqs[qi](); qi += 1
            while qi < len(seqs):
                seqs[qi](); qi += 1
            pend.append((ti, c, sta_)); pend.append((ti, c + 1, stb_))
            for cc_ in (c, c + 1):
                if nextpre and cc_ < len(nextpre):
                    nextpre[cc_]()
        for s in nextpre[NCH:]:
            s()
    while pend:
        pti, pc, pst = pend.pop(0)
        for s in chunk_seq(pti, pc, pst):
            s()
        if pc == NCH - 1:
            for s in post_steps(pti):
                s()

    allgather_rows(S, sP, G.gP[l], RP, CR_F, m0=0, m1=4)
    S.phase_end()


def phase_C(nc, G, l):
    NT = G.NT; SEQ = 4 * NT; NTL = NT // TT
    NQ = SEQ // QT; NKL = SEQ // 4
    S = Sched(nc)
    for b_ in G.persist:
        b_.reset()
    gAb = G.gAb[l]; gAv = G.gAv[l]; sP = G.sP[l]
    allgather_rows(S, G.sAf[l], G.gAf[l], 1280, CR_F)
    idxCk = S.sbuf("idxCk", [128, 6, 4], I32); idxCv = S.sbuf("idxCv", [128, NKL // 128], I32)
    S.dma("sp", idxCk[:], G.inp["idxCk"].t, reads=[G.inp["idxCk"]], writes=[idxCk])
    S.dma("sp", idxCv[:], G.inp["idxCv"].t, reads=[G.inp["idxCv"]], writes=[idxCv])
    kT = [S.sbuf(f"kT{h}", [96, NKL], BF16) for h in range(6)]
    gkv = gAb.t.rearrange("a (j x) -> (a j) x", j=4)
    for h in range(6):
        for i_ in range(4):
            S.idma(kT[h][:, i_*(NT//4):(i_+1)*(NT//4)], gkv, idxCk[0:96, h, i_:i_+1], reads=[gAb, idxCk], writes=[kT[h]])
    NKT = NKL // 128
    vaug = S.sbuf("vaug", [128, NKT, 6, 65], BF16)
    S.op("pool", lambda hh: hh.memset(vaug[:], 1.0), writes=[vaug])
    vst = S.sbuf("vst", [128, NKT, 384], BF16)
    for n_ in range(NKT):
        S.idma(vst[:, n_, :], gAv.t, idxCv[:, n_:n_+1], reads=[gAv, idxCv], writes=[vst])
    for n in range(NKT):
        S.op("pool", lambda hh: hh.tensor_copy(out=vaug[:, n, :, 0:64], in_=vst[:, n, :].rearrange("p (h c) -> p h c", c=64)), reads=[vst], writes=[vaug])
    mask = S.sbuf("mask", [128, QT], BF16)
    S.dma("sp", mask[:], G.inp["maskC"][:, :], reads=[G.inp["maskC"]], writes=[mask])
    qb = [[S.sbuf(f"q{i}_{h}", [96, QT], BF16) for h in range(6)] for i in range(2)]
    ps = [S.psum(f"ps{i}", [128, QT]) for i in range(4)]
    po = [S.psum(f"po{i}", [128, QT]) for i in range(2)]
    pT = [S.sbuf(f"pT{i}", [128, QT], BF16) for i in range(4)]
    oo = [S.sbuf(f"oo{i}", [65, QT], F32) for i in range(3)]
    steps_ = [(g, h, i) for g in range(NQ) for h in range(6) for i in range(g + 1)]
    oc = [0]
    def emit_qk(s_):
        g, h, i = steps_[s_]
        qs = qb[g % 2]
        if h == 0 and i == 0:
            for hh_ in range(6):
                S.dma("sp", qs[hh_][:], gAb[int(gaddr(CR_B, RAB, g // NTL, hh_*96)):int(gaddr(CR_B, RAB, g // NTL, hh_*96)) + 96, (g % NTL)*QT:(g % NTL + 1)*QT], reads=[gAb], writes=[qs[hh_]])
        p = ps[s_ % 4]
        S.op("pe", lambda hh: hh.matmul(p[:], lhsT=kT[h][:, i*128:(i+1)*128], rhs=qs[h][:], start=True, stop=True), reads=[kT[h], qs[h]], writes=[p])
    def emit_rest(s_):
        g, h, i = steps_[s_]
        p = ps[s_ % 4]; pt = pT[s_ % 4]
        acc = po[(g * 6 + h) % 2]
        S.op("act", lambda hh: hh.activation(out=pt[:], in_=p[:], func=AF.Exp, scale=SCALE), reads=[p], writes=[pt])
        if i == g:
            S.op("dve", lambda hh: hh.tensor_tensor(out=pt[:], in0=pt[:], in1=mask[:], op=ALU.mult), reads=[pt, mask], writes=[pt])
        S.op("pe", lambda hh: hh.matmul(acc[0:65, :], lhsT=vaug[:, i, h, :], rhs=pt[:], start=(i == 0), stop=(i == g)), reads=[vaug, pt], writes=[acc])
        if i == g:
            o = oo[oc[0] % 3]; oc[0] += 1
            S.op("dve", lambda hh: hh.tensor_copy(out=o[:], in_=acc[0:65, :]), reads=[acc], writes=[o])
            S.dma("sp", sP[512 + (g // NTL)*390 + h*65:512 + (g // NTL)*390 + (h+1)*65, (g % NTL)*QT:(g % NTL + 1)*QT], o[:], reads=[o], writes=[sP])
    AHEAD = 2
    for s_ in range(len(steps_) + AHEAD):
        if s_ < len(steps_):
            emit_qk(s_)
        if s_ - AHEAD >= 0:
            emit_rest(s_ - AHEAD)
    S.phase_end()


def phase_D(nc, G, l):
    NT = G.NT; NTL = NT // TT
    moe = (l % 2 == 1)
    G_ = 3 if moe else 4
    F = 3584 if moe else 2816
    NE = 8 if moe else 1
    NF = F // 128
    S = Sched(nc)
    for b_ in G.persist:
        b_.reset()
    gP = G.gP[l]; ycl = G.ycl[l]
    gPv = gP.t.rearrange("a (n c) -> (a n) c", c=TT)
    wo_d = G.inp["w_out%d" % l]; pc_d = G.inp["pcolD%d" % l]; cst_d = G.inp["cstD"]
    wg_d = G.inp["wg%d" % l]; wu_d = G.inp["wu%d" % l]; wd_d = G.inp["wd%d" % l]
    if moe:
        rt_d = G.inp["router"]
    idxDya = S.sbuf("idxDya", [128, 3, NTL], I32); idxDo = S.sbuf("idxDo", [128, 24, NTL], I32)
    S.dma("sp", idxDya[:], G.inp["idxDya"].t, reads=[G.inp["idxDya"]], writes=[idxDya])
    S.dma("sp", idxDo[:], G.inp["idxDo"].t, reads=[G.inp["idxDo"]], writes=[idxDo])
    pc = S.sbuf("pc", [128, 16]); cst = S.sbuf("cst", [128, 2, 128])
    S.dma("sp", pc[:], pc_d[:], reads=[pc_d], writes=[pc])
    S.dma("sp", cst[:], cst_d[:], reads=[cst_d], writes=[cst])
    ONES = cst[:, 0, :]; IDENT = cst[:, 1, :]
    onesb = S.sbuf("onesb", [128, 128], BF16)
    S.op("dve", lambda h: h.tensor_copy(out=onesb[:], in_=ONES), reads=[cst], writes=[onesb])
    epsc = S.sbuf("epsc", [128, 1]); S.op("pool", lambda h: h.memset(epsc[:], EPS), writes=[epsc])
    NPS = 8
    slots = [S.psum(f"slot{i}", [128, TT]) for i in range(NPS)]
    psc = [0]
    def getps():
        psc[0] += 1
        return slots[psc[0] % NPS]
    xT = [S.sbuf(f"xT{k}", [128, NT], F32) for k in range(8)]
    for k in range(8):
        S.dma("sp", xT[k][:], G.xsrc[l][k*128:(k+1)*128, :], reads=[G.xsrc[l]], writes=[xT[k]])
    hT = [S.sbuf(f"hT{k}", [128, NT], BF16) for k in range(8)]
    stg = []
    stc = [0]
    def getstg():
        stc[0] += 1
        return stg[stc[0] % 2]
    wgb = [S.sbuf(f"wgb{i}", [128, 8, G_*128], BF16) for i in range(2)]
    wub = [S.sbuf(f"wub{i}", [128, 8, G_*128], BF16) for i in range(2)]
    wdb = [S.sbuf(f"wdb{i}", [128, G_, D], BF16) for i in range(2)]
    _wob = [wgb[0], wgb[1], wub[0], wub[1]]
    def wo_view(i):
        b = _wob[i // G_]
        return b, b.t[:].rearrange("p k f -> p (k f)")[:, (i % G_) * D:(i % G_ + 1) * D]
    krows = [(i*128, 128) for i in range(3)] + [(384 + i*64, 64) for i in range(6)] + [(768 + i*128, 128) for i in range(2)]
    for i, (r0, n) in enumerate(krows):
        wob, wov = wo_view(i)
        S.dma("pool", wov[0:n, :], wo_d[r0:r0+n, :], reads=[wo_d], writes=[wob])
    mixb = [S.sbuf(f"mixb{i}", [128, TT], BF16) for i in range(11)]
    oh = [S.sbuf(f"oh{i}", [65, TT], F32) for i in range(6)]
    sqb = [S.sbuf(f"sqb{i}", [128, TT], BF16) for i in range(2)]
    rs = S.sbuf("rs", [128, TT], F32)
    ldt = [S.sbuf(f"ldt{i}", [128, TT], F32) for i in range(3)]
    ldc = [0]
    def getld():
        ldc[0] += 1
        return ldt[ldc[0] % 3]
    if moe:
        rtr = S.sbuf("rtr", [128, 8, 8], F32)
        S.dma("sp", rtr[:], rt_d.t.rearrange("(k p) e -> p k e", p=128), reads=[rt_d], writes=[rtr])
        hf = [S.sbuf(f"hf{k}", [128, TT], F32) for k in range(2)]
        gbc2 = [S.sbuf(f"gbc{e}", [128, NT], BF16) for e in range(2)]
        gfall = S.sbuf("gfall", [128, NT // 128, 8], F32)
        lg = S.sbuf("lg", [128, 8], F32); m1 = S.sbuf("m1", [128, 1], F32); m2 = S.sbuf("m2", [128, 1], F32)
        eq1 = S.sbuf("eq1", [128, 8], F32); eq2 = S.sbuf("eq2", [128, 8], F32); msk = S.sbuf("msk", [128, 8], F32)
        g1 = S.sbuf("g1", [128, 1], F32); g2 = S.sbuf("g2", [128, 1], F32); gf = S.sbuf("gf", [128, 8], F32)
        gexp = S.sbuf("gexp", [128, 128], F32)

    for t in range(NTL):
        sl = slice(t*TT, (t+1)*TT)
        for i in range(3):
            st = getld()
            S.idma(st[:], gPv, idxDya[:, i, t:t+1], reads=[gP, idxDya], writes=[st])
            S.op("act", lambda h: h.copy(out=mixb[i][:], in_=st[:]), reads=[st], writes=[mixb[i]])
        for i in range(2):
            st = getld()
            S.dma("sp", st[:], ycl[i*128:(i+1)*128, sl], reads=[ycl], writes=[st])
            S.op("act", lambda h: h.copy(out=mixb[9+i][:], in_=st[:]), reads=[st], writes=[mixb[9+i]])
        pss = getps()
        for hd in range(6):
            o = oh[hd]
            S.idma(o[:], gPv, idxDo[0:65, hd, t:t+1], reads=[gP, idxDo], writes=[o])
            for j in range(1, 4):
                st = getld()
                S.idma(st[0:65, :], gPv, idxDo[0:65, j*6 + hd, t:t+1], reads=[gP, idxDo], writes=[st])
                S.op("pool", lambda h: h.tensor_tensor(out=o[:], in0=o[:], in1=st[0:65, :], op=ALU.add), reads=[o, st], writes=[o])
            S.op("dve", lambda h: h.reciprocal(out=o[64:65, :], in_=o[64:65, :]), reads=[o], writes=[o])
            pb = getps()
            S.op("pe", lambda h: h.matmul(pb[0:64, :], lhsT=ONES[64:65, 0:64], rhs=o[64:65, :], start=True, stop=True), reads=[cst, o], writes=[pb])
            S.op("dve", lambda h: h.tensor_tensor(out=o[0:64, :], in0=o[0:64, :], in1=pb[0:64, :], op=ALU.mult), reads=[o, pb], writes=[o])
            s = sqb[hd % 2]
            S.op("act", lambda h: h.activation(out=s[0:64, :], in_=o[0:64, :], func=AF.Square), reads=[o], writes=[s])
            S.op("pe", lambda h: h.matmul(pss[:], lhsT=onesb[0:64, :], rhs=s[0:64, :], start=(hd == 0), stop=(hd == 5)), reads=[onesb, s], writes=[pss])
        S.op("act", lambda h: h.activation(out=rs[:], in_=pss[:], func=AF.Sqrt, scale=1.0/384, bias=epsc[:]), reads=[pss, epsc], writes=[rs])
        S.op("dve", lambda h: h.reciprocal(out=rs[:], in_=rs[:]), reads=[rs], writes=[rs])
        for hd in range(6):
            S.op("dve", lambda h: h.scalar_tensor_tensor(out=mixb[3+hd][0:64, :], in0=oh[hd][0:64, :], scalar=pc[0:64, 8+hd:9+hd], in1=rs[0:64, :], op0=ALU.mult, op1=ALU.mult), reads=[oh[hd], pc, rs], writes=[mixb[3+hd]])
        for m in range(8):
            p = getps()
            for i, (r0, n) in enumerate(krows):
                wob, wov = wo_view(i)
                S.op("pe", lambda h: h.matmul(p[:], lhsT=wov[0:n, m*128:(m+1)*128], rhs=mixb[i][0:n, :], start=(i == 0), stop=(i == 10)), reads=[wob, mixb[i]], writes=[p])
            S.op("dve", lambda h: h.tensor_tensor(out=xT[m][:, sl], in0=xT[m][:, sl], in1=p[:], op=ALU.add), reads=[xT[m], p], writes=[xT[m]])
        p = getps()
        for k in range(8):
            s = sqb[k % 2]
            S.op("act", lambda h: h.activation(out=s[:], in_=xT[k][:, sl], func=AF.Square), reads=[xT[k]], writes=[s])
            S.op("pe", lambda h: h.matmul(p[:], lhsT=onesb[:], rhs=s[:], start=(k == 0), stop=(k == 7)), reads=[onesb, s], writes=[p])
        S.op("act", lambda h: h.activation(out=rs[:], in_=p[:], func=AF.Sqrt, scale=1.0/D, bias=epsc[:]), reads=[p, epsc], writes=[rs])
        S.op("dve", lambda h: h.reciprocal(out=rs[:], in_=rs[:]), reads=[rs], writes=[rs])
        for k in range(8):
            S.op("dve", lambda h: h.scalar_tensor_tensor(out=hT[k][:, sl], in0=xT[k][:, sl], scalar=pc[:, k:k+1], in1=rs[:], op0=ALU.mult, op1=ALU.mult), reads=[xT[k], pc, rs], writes=[hT[k]])
        if moe:
            pls = [getps() for j in range(TT // 128)]
            for k in range(8):
                hk = hf[k % 2]
                S.op("dve", lambda h: h.scalar_tensor_tensor(out=hk[:], in0=xT[k][:, sl], scalar=pc[:, k:k+1], in1=rs[:], op0=ALU.mult, op1=ALU.mult), reads=[xT[k], pc, rs], writes=[hk])
                for j in range(TT // 128):
                    S.op("pe", lambda h: h.matmul(pls[j][:, 0:8], lhsT=hk[:, j*128:(j+1)*128], rhs=rtr[:, k, :], start=(k == 0), stop=(k == 7)), reads=[hk, rtr], writes=[pls[j]])
            for j in range(TT // 128):
                pl = pls[j]
                S.op("dve", lambda h: h.tensor_copy(out=lg[:], in_=pl[:, 0:8]), reads=[pl], writes=[lg])
                S.op("dve", lambda h: h.reduce_max(out=m1[:], in_=lg[:], axis=AX.X), reads=[lg], writes=[m1])
                S.op("dve", lambda h: h.tensor_scalar(out=eq1[:], in0=lg[:], scalar1=m1[:, 0:1], scalar2=None, op0=ALU.is_equal), reads=[lg, m1], writes=[eq1])
                S.op("dve", lambda h: h.scalar_tensor_tensor(out=msk[:], in0=eq1[:], scalar=-1e30, in1=lg[:], op0=ALU.mult, op1=ALU.add), reads=[eq1, lg], writes=[msk])
                S.op("dve", lambda h: h.reduce_max(out=m2[:], in_=msk[:], axis=AX.X), reads=[msk], writes=[m2])
                S.op("dve", lambda h: h.tensor_scalar(out=eq2[:], in0=msk[:], scalar1=m2[:, 0:1], scalar2=None, op0=ALU.is_equal), reads=[msk, m2], writes=[eq2])
                S.op("dve", lambda h: h.tensor_tensor(out=g1[:], in0=m1[:], in1=m2[:], op=ALU.subtract), reads=[m1, m2], writes=[g1])
                S.op("act", lambda h: h.activation(out=g1[:], in_=g1[:], func=AF.Sigmoid), reads=[g1], writes=[g1])
                S.op("dve", lambda h: h.tensor_scalar(out=g2[:], in0=g1[:], scalar1=-1.0, scalar2=1.0, op0=ALU.mult, op1=ALU.add), reads=[g1], writes=[g2])
                S.op("dve", lambda h: h.tensor_scalar(out=gf[:], in0=eq1[:], scalar1=g1[:, 0:1], scalar2=None, op0=ALU.mult), reads=[eq1, g1], writes=[gf])
                S.op("dve", lambda h: h.scalar_tensor_tensor(out=gf[:], in0=eq2[:], scalar=g2[:, 0:1], in1=gf[:], op0=ALU.mult, op1=ALU.add), reads=[eq2, g2, gf], writes=[gf])
                S.op("dve", lambda h: h.tensor_copy(out=gfall[:, t * (TT // 128) + j, :], in_=gf[:]), reads=[gf], writes=[gfall])

    act = [S.sbuf(f"act{i}", [128, NT], BF16) for i in range(G_)]
    sg = [S.sbuf(f"sg{i}", [128, TT], BF16) for i in range(3)]
    sgc = 0; gi = 0
    groups = []
    f0 = 0
    while f0 < NF:
        n = min(G_, NF - f0); groups.append((f0, n)); f0 += n
    for e in range(NE):
        if moe:
            gbc_e = gbc2[e % 2]
            for jj in range(NT // 128):
                S.op("dve", lambda h: h.tensor_scalar(out=gexp[:], in0=ONES, scalar1=gfall[:, jj, e:e+1], scalar2=None, op0=ALU.mult), reads=[cst, gfall], writes=[gexp])
                pg = getps()
                S.op("pe", lambda h: h.matmul(pg[:, 0:128], lhsT=gexp[:], rhs=IDENT, start=True, stop=True), reads=[gexp, cst], writes=[pg])
                S.op("act", lambda h: h.copy(out=gbc_e[:, jj*128:(jj+1)*128], in_=pg[:, 0:128]), reads=[pg], writes=[gbc_e])
        for (f0, n) in groups:
            bi = gi % 2; gi += 1
            wg_, wu_, wd_ = wgb[bi], wub[bi], wdb[bi]
            for (src, dst) in [(wg_d, wg_), (wu_d, wu_)]:
                for kh in range(2):
                    S.dma("pool", dst[:, kh*4:(kh+1)*4, 0:n*128], src.t[e, kh*512:(kh+1)*512, f0*128:(f0+n)*128].rearrange("(k p) f -> p k f", p=128), reads=[src], writes=[dst])
            S.dma("pool", wd_[:, 0:n, :], wd_d.t[e, f0*128:(f0+n)*128, :].rearrange("(i p) d -> p i d", p=128), reads=[wd_d], writes=[wd_])
            for i in range(n):
                for t in range(NTL):
                    sl = slice(t*TT, (t+1)*TT)
                    pg = getps(); pu = getps()
                    for k in range(8):
                        S.op("pe", lambda h: h.matmul(pg[:], lhsT=wg_[:, k, i*128:(i+1)*128], rhs=hT[k][:, sl], start=(k == 0), stop=(k == 7)), reads=[wg_, hT[k]], writes=[pg])
                    for k in range(8):
                        S.op("pe", lambda h: h.matmul(pu[:], lhsT=wu_[:, k, i*128:(i+1)*128], rhs=hT[k][:, sl], start=(k == 0), stop=(k == 7)), reads=[wu_, hT[k]], writes=[pu])
                    s = sg[sgc % 3]; sgc += 1
                    S.op("act", lambda h: h.activation(out=s[:], in_=pg[:], func=AF.Silu), reads=[pg], writes=[s])
                    if moe:
                        S.op("dve", lambda h: h.tensor_tensor(out=s[:], in0=s[:], in1=gbc_e[:, sl], op=ALU.mult), reads=[s, gbc_e], writes=[s])
                    S.op("dve", lambda h: h.tensor_tensor(out=act[i][:, sl], in0=s[:], in1=pu[:], op=ALU.mult), reads=[s, pu], writes=[act[i]])
            for m in range(8):
                for t in range(NTL):
                    sl = slice(t*TT, (t+1)*TT)
                    p = getps()
                    for i in range(n):
                        S.op("pe", lambda h: h.matmul(p[:], lhsT=wd_[:, i, m*128:(m+1)*128], rhs=act[i][:, sl], start=(i == 0), stop=(i == n-1)), reads=[wd_, act[i]], writes=[p])
                    S.op("dve", lambda h: h.tensor_tensor(out=xT[m][:, sl], in0=xT[m][:, sl], in1=p[:], op=ALU.add), reads=[xT[m], p], writes=[xT[m]])
    for k in range(8):
        S.dma("sp", G.xdst[l][k*128:(k+1)*128, :], xT[k][:], reads=[xT[k]], writes=[G.xdst[l]])

    S.phase_end(last=(l == 1))

def build_fused(NT=2048, stop_after=99):
    SEQ = 4 * NT; NTILE = SEQ // TT; NTL = NT // TT; NKT = (SEQ // 4) // 128
    nc = bass.Bass("TRN2", target_bir_lowering=False)
    G = NS(); G.NT = NT
    G.inp = {}
    def ein(name, shape, dt=F32):
        t = nc.dram_tensor(name, list(shape), dt, kind="ExternalInput")
        b = Buf(None, t.ap(), "dram"); G.inp[name] = b
        return b
    G.xT_d = ein("xT", [D, NT]); ein("pos", [1, NT], I32)
    ein("cstA", [128, 6, 128]); ein("cstB", [128, 8, 128]); ein("cstD", [128, 2, 128]); ein("maskC", [128, QT], BF16)
    ein("idxB", [128, 3, NTILE], I32); ein("idxBh", [128, 3, NTILE], I32); ein("idxCk", [128, 6, 4], I32)
    ein("idxCv", [128, NKT], I32); ein("idxDya", [128, 3, NTL], I32); ein("idxDo", [128, 24, NTL], I32)
    for l in range(2):
        ein("w_in%d" % l, [D, INC]); ein("pcolA%d" % l, [128, 32]); ein("wuq%d" % l, [256, 576]); ein("wukv%d" % l, [128, 768])
        ein("ws%d" % l, [4, 128, 128]); ein("bs%d" % l, [1, 512])
        ein("pcolB%d" % l, [128, 16]); ein("plo%d" % l, [64, 4]); ein("wup%d" % l, [32, 128]); ein("aup%d" % l, [32, 128])
        ein("gup%d" % l, [64, 128]); ein("w0row%d" % l, [1, 128]); ein("w_out%d" % l, [D, D]); ein("pcolD%d" % l, [128, 16])
    ein("vdown", [128, 3, 16]); ein("vup", [16, 128])
    ein("wg0", [1, D, 2816]); ein("wu0", [1, D, 2816]); ein("wd0", [1, 2816, D])
    ein("wg1", [8, D, 3584]); ein("wu1", [8, D, 3584]); ein("wd1", [8, 3584, D]); ein("router", [D, 8])
    xo = nc.dram_tensor("xoT", [D, NT], F32, kind="ExternalOutput")
    G.xo_d = Buf(None, xo.ap(), "dram")
    def idram(name, shape, dt=F32):
        t = nc.dram_tensor(name, list(shape), dt, kind="Internal")
        return Buf(None, t.ap(), "dram")
    G.sAf = [idram("sAf%d" % l, [1280, NT]) for l in range(2)]; G.gAf = [idram("gAf%d" % l, [4 * 1280, NT]) for l in range(2)]
    G.sAb = [idram("sAb%d" % l, [RAB, NT], BF16) for l in range(2)]; G.gAb = [idram("gAb%d" % l, [4 * RAB, NT], BF16) for l in range(2)]
    G.sAv = [idram("sAv%d" % l, [NT, 384], BF16) for l in range(2)]; G.gAv = [idram("gAv%d" % l, [4 * NT, 384], BF16) for l in range(2)]
    G.ycl = [idram("ycl%d" % l, [256, NT]) for l in range(2)]
    G.sP = [idram("sP%d" % l, [RP, NT]) for l in range(2)]; G.gP = [idram("gP%d" % l, [4 * RP, NT]) for l in range(2)]
    es = contextlib.ExitStack()
    G.xsp = idram("xsp", [D, NT])
    G.xsrc = [G.xT_d, G.xsp]; G.xdst = [G.xsp, G.xo_d]
    G.persist = list(G.inp.values()) + [G.xo_d, G.xsp] + G.sAf + G.gAf + G.sAb + G.gAb + G.sAv + G.gAv + G.ycl + G.sP + G.gP
    k_ = 0
    for l in range(2):
        for ph in (phase_A, phase_C, phase_B, phase_D):
            if k_ < stop_after:
                ph(nc, G, l)
            k_ += 1
    es.close()
    return nc

def fused_inputs(d, c, NT=2048):
    SEQ = 4 * NT; NTILE = SEQ // TT; NTL = NT // TT; NKT = (SEQ // 4) // 128; MS = NT // 512
    b, r = c // 4, c % 4
    p = r if r < 3 else 0; j = r; q = r
    m = {}
    x = np.asarray(d["x"])[:, :SEQ]
    m["xT"] = np.ascontiguousarray(x[b, q*NT:(q+1)*NT].T)
    m["pos"] = np.ascontiguousarray(np.asarray(d["positions"])[b:b+1, q*NT:(q+1)*NT]).astype(np.int32)
    m["cstA"] = a2_consts(); m["cstB"] = rwkv_consts(); m["cstD"] = d_consts(); m["maskC"] = np.ascontiguousarray(masks_C()[j])
    pp = np.arange(128)
    iB = np.zeros((128, 3, NTILE), np.int32); iBh = np.zeros((128, 3, NTILE), np.int32)
    for ti in range(NTILE):
        i, n = ti // NTL, ti % NTL
        for kind in range(3):
            row = kind * 384 + p * 128 + pp
            iB[:, kind, ti] = gaddr(CR_F, 1280, i, row) * NTL + n
            if ti > 0:
                if n > 0:
                    iBh[:, kind, ti] = gaddr(CR_F, 1280, i, row) * NT + n * TT - 1
                else:
                    iBh[:, kind, ti] = gaddr(CR_F, 1280, i - 1, row) * NT + NT - 1
    m["idxB"] = iB; m["idxBh"] = iBh
    iCk = np.zeros((128, 6, 4), np.int32)
    for h in range(6):
        for i in range(4):
            iCk[:96, h, i] = gaddr(CR_B, RAB, i, 576 + h * 96 + np.arange(96)) * 4 + j
    m["idxCk"] = iCk
    iCv = np.zeros((128, NKT), np.int32)
    for i in range(4):
        for ms in range(MS):
            iCv[:, i * MS + ms] = gaddr(min(CR_V, NT), NT, i, j * (NT // 4) + ms * 128 + pp)
    m["idxCv"] = iCv
    iDy = np.zeros((128, 3, NTL), np.int32); iDo = np.zeros((128, 24, NTL), np.int32)
    for t in range(NTL):
        for pr in range(3):
            iDy[:, pr, t] = gaddr(CR_F, RP, pr, q * 128 + pp) * NTL + t
        for jj in range(4):
            for h in range(6):
                iDo[:65, jj * 6 + h, t] = gaddr(CR_F, RP, jj, 512 + q * 390 + h * 65 + np.arange(65)) * NTL + t
    m["idxDya"] = iDy; m["idxDo"] = iDo
    for l in range(2):
        pc = np.zeros((128, 32), np.float32)
        pc[:, 0:8] = d["mix_norm_g"][l].reshape(8, 128).T
        pc[:, 8:10] = d["b_q_norm_g"][l].reshape(2, 128).T
        pc[:, 10] = d["b_kv_norm_g"][l]
        pc[0:96, 11] = d["b_q_head_g"][l]; pc[0:96, 12] = d["b_k_head_g"][l]
        pc[:, 13:15] = d["c_ln_g"][l].reshape(2, 128).T; pc[:, 15:17] = d["c_ln_b"][l].reshape(2, 128).T
        pc[:, 17:19] = d["c_out_g"][l].reshape(2, 128).T
        m["w_in%d" % l] = d["w_in"][l]; m["pcolA%d" % l] = pc; m["wuq%d" % l] = d["b_w_uq"][l]; m["wukv%d" % l] = d["b_w_ukv"][l]
        m["ws%d" % l] = d["c_w_s"][l]; m["bs%d" % l] = np.ascontiguousarray(d["c_b_s"][l].reshape(1, 512))
        mu = d["shift_mu"][l]; sl = slice(p * 128, (p + 1) * 128)
        pb = np.zeros((128, 16), np.float32)
        pb[:, 0] = mu[0:384][sl]; pb[:, 1] = mu[384:768][sl]; pb[:, 2] = mu[768:1152][sl]
        pb[:, 3] = d["a_w0"][l][sl]; pb[:, 4] = d["a_a0"][l][sl]; pb[:, 5] = d["a_k_k"][l][sl]; pb[:, 6] = d["a_k_a"][l][sl]
        pb[:, 7] = d["a_r_k"][l].reshape(-1)[sl]; pb[:, 8] = d["a_ln_g"][l][sl]; pb[:, 9] = d["a_ln_b"][l][sl]
        if l == 1:
            pb[:, 10] = d["a_v0"][0][sl]; pb[:, 11] = d["shift_mu"][0][768:1152][sl]
            for cc in range(3):
                pb[:, 12 + cc] = mu[768 + cc * 128:768 + (cc + 1) * 128]
        m["pcolB%d" % l] = pb
        pl = np.zeros((64, 4), np.float32)
        pl[0:32, 0] = mu[1152:1184]; pl[0:32, 1] = mu[1184:1216]; pl[0:64, 2] = mu[1216:1280]
        m["plo%d" % l] = pl
        m["wup%d" % l] = np.ascontiguousarray(d["a_w_up"][l][:, sl]); m["aup%d" % l] = np.ascontiguousarray(d["a_a_up"][l][:, sl])
        m["gup%d" % l] = np.ascontiguousarray(d["a_g_up"][l][:, sl]); m["w0row%d" % l] = np.ascontiguousarray(d["a_w0"][l][sl][None, :])
        pd = np.zeros((128, 16), np.float32)
        pd[:, 0:8] = d["ffn_norm_g"][l].reshape(8, 128).T
        pd[0:64, 8:14] = d["b_out_g"][l].reshape(6, 64).T
        m["w_out%d" % l] = d["w_out"][l]; m["pcolD%d" % l] = pd
    m["vdown"] = np.ascontiguousarray(d["a_v_down"][0].reshape(3, 128, 16).transpose(1, 0, 2))
    m["vup"] = np.ascontiguousarray(d["a_v_up"][0][:, p * 128:(p + 1) * 128])
    m["wg0"] = d["dense_w_gate"]; m["wu0"] = d["dense_w_up"]; m["wd0"] = d["dense_w_down"]
    m["wg1"] = d["moe_w_gate"][0]; m["wu1"] = d["moe_w_up"][0]; m["wd1"] = d["moe_w_down"][0]; m["router"] = d["moe_router"][0]
    return {k: np.ascontiguousarray(v) for k, v in m.items()}


_NC = []
def kernel(**inputs):
    d = {k: np.asarray(v) for k, v in inputs.items()}
    if not _NC:
        _NC.append(build_fused(2048))
    in_maps = [fused_inputs(d, c, 2048) for c in range(8)]
    res = run_bass_kernel_spmd(_NC[0], in_maps, core_ids=list(range(8)))
    outs = [np.asarray(res.results[c]["xoT"]) for c in range(8)]
    out = np.stack([o.T for o in outs]).reshape(2, 8192, 1024)
    return np.ascontiguousarray(out.astype(np.float32))
```
